# Optimizing a Trainium2 kernel written in Bass

```python
import jax, jax.numpy as jnp
from jax import lax
import numpy as np

D_MODEL = 1024
BATCH = 8
SEQ = 4096
DEPTH = 4

GRID_W = 64
Q_BLOCK = 128
ROPE_THETA = 10000.0
EPS = 1e-6

GQA_HEADS = 8
GQA_KV_HEADS = 2
GQA_GROUP = GQA_HEADS // GQA_KV_HEADS
GQA_HEAD_DIM = 64
GQA_WIDTH = GQA_HEADS * GQA_HEAD_DIM
GQA_KV_WIDTH = GQA_KV_HEADS * GQA_HEAD_DIM

MLA_HEADS = 8
MLA_Q_RANK = 256
MLA_KV_RANK = 128
MLA_NOPE_DIM = 64
MLA_ROPE_DIM = 32
MLA_V_DIM = 64
MLA_QK_DIM = MLA_NOPE_DIM + MLA_ROPE_DIM
MLA_WIDTH = MLA_HEADS * MLA_V_DIM

MIX_WIDTH = GQA_WIDTH + MLA_WIDTH
IN_SIZES = (GQA_WIDTH, GQA_KV_WIDTH, GQA_KV_WIDTH, MLA_Q_RANK, MLA_KV_RANK + MLA_ROPE_DIM)
IN_WIDTH = sum(IN_SIZES)
IN_OFFSETS = tuple(int(v) for v in np.cumsum(IN_SIZES)[:-1])

MEM_TOKENS = 256
MEM_HEADS = 4
MEM_HEAD_DIM = 128
MEM_WIDTH = MEM_HEADS * MEM_HEAD_DIM

N_EXPERTS = 16
EXPERT_FF = 512
EC_CAPACITY_FACTOR = 2

kernel_name = "hybrid_gqa_mla_memxattn_ecmoe_encoder"


def rms_norm(x, g):
    xf = x.astype(jnp.float32)
    y = xf * lax.rsqrt(jnp.mean(xf * xf, axis=-1, keepdims=True) + EPS)
    return (y * g.astype(jnp.float32)).astype(x.dtype)


def axial_rope_angles(seq_len, rot_dim):
    rows = seq_len // GRID_W
    row = jnp.repeat(jnp.arange(rows, dtype=jnp.float32), GRID_W)
    col = jnp.tile(jnp.arange(GRID_W, dtype=jnp.float32), rows)
    axis_dim = rot_dim // 2
    inv_freq = ROPE_THETA ** (-jnp.arange(0, axis_dim, 2, dtype=jnp.float32) / axis_dim)
    ang = jnp.concatenate([row[:, None] * inv_freq[None, :], col[:, None] * inv_freq[None, :]], axis=-1)
    return jnp.cos(ang), jnp.sin(ang)


def apply_rope(x, cos, sin):
    c = cos[None, :, None, :].astype(x.dtype)
    s = sin[None, :, None, :].astype(x.dtype)
    half = x.shape[-1] // 2
    x1, x2 = x[..., :half], x[..., half:]
    return jnp.concatenate([x1 * c - x2 * s, x2 * c + x1 * s], axis=-1)


def blocked_attention(q, k, v, scale):
    B, S, K, G, Dk = q.shape
    Dv = v.shape[-1]
    nblk = S // Q_BLOCK
    qb = q.reshape(B, nblk, Q_BLOCK, K, G, Dk).transpose(1, 0, 2, 3, 4, 5)

    def one_block(qblk):
        s = jnp.einsum('bqkgd,btkd->bkgqt', qblk, k).astype(jnp.float32) * scale
        p = jax.nn.softmax(s, axis=-1).astype(v.dtype)
        return jnp.einsum('bkgqt,btkd->bqkgd', p, v)

    o = lax.map(one_block, qb)
    return o.transpose(1, 0, 2, 3, 4, 5).reshape(B, S, K * G * Dv)


def parallel_mixer(h, w_in, gqa_q_norm, gqa_k_norm, mla_q_norm, mla_kv_norm, w_q_b, w_kv_b,
                   out_norm_gqa, out_norm_mla, w_o, cos_g, sin_g, cos_m, sin_m):
    B, S, _ = h.shape
    proj = h @ w_in
    q_g, k_g, v_g, q_lat, kv_lat = jnp.split(proj, IN_OFFSETS, axis=-1)

    q_g = apply_rope(rms_norm(q_g.reshape(B, S, GQA_HEADS, GQA_HEAD_DIM), gqa_q_norm), cos_g, sin_g)
    k_g = apply_rope(rms_norm(k_g.reshape(B, S, GQA_KV_HEADS, GQA_HEAD_DIM), gqa_k_norm), cos_g, sin_g)
    v_g = v_g.reshape(B, S, GQA_KV_HEADS, GQA_HEAD_DIM)
    o_g = blocked_attention(q_g.reshape(B, S, GQA_KV_HEADS, GQA_GROUP, GQA_HEAD_DIM), k_g, v_g,
                            GQA_HEAD_DIM ** -0.5)

    c_q = rms_norm(q_lat, mla_q_norm)
    q_m = (c_q @ w_q_b).reshape(B, S, MLA_HEADS, MLA_QK_DIM)
    q_m = jnp.concatenate([q_m[..., :MLA_NOPE_DIM], apply_rope(q_m[..., MLA_NOPE_DIM:], cos_m, sin_m)], axis=-1)
    c_kv = rms_norm(kv_lat[..., :MLA_KV_RANK], mla_kv_norm)
    k_rope = apply_rope(kv_lat[..., MLA_KV_RANK:].reshape(B, S, 1, MLA_ROPE_DIM), cos_m, sin_m)
    kv = (c_kv @ w_kv_b).reshape(B, S, MLA_HEADS, MLA_NOPE_DIM + MLA_V_DIM)
    k_m = jnp.concatenate([kv[..., :MLA_NOPE_DIM],
                           jnp.broadcast_to(k_rope, (B, S, MLA_HEADS, MLA_ROPE_DIM))], axis=-1)
    v_m = kv[..., MLA_NOPE_DIM:]
    o_m = blocked_attention(q_m[:, :, :, None, :], k_m, v_m, MLA_QK_DIM ** -0.5)

    merged = jnp.concatenate([rms_norm(o_g, out_norm_gqa), rms_norm(o_m, out_norm_mla)], axis=-1)
    return merged @ w_o


def memory_cross_attention(h, m, w_mem_q, w_mem_kv, w_mem_o):
    B, S, _ = h.shape
    M = m.shape[1]
    q = (h @ w_mem_q).reshape(B, S, MEM_HEADS, MEM_HEAD_DIM)
    kv = (m @ w_mem_kv).reshape(B, M, 2, MEM_HEADS, MEM_HEAD_DIM)
    k, v = kv[:, :, 0], kv[:, :, 1]
    s = jnp.einsum('bshd,bmhd->bhsm', q, k).astype(jnp.float32) * (MEM_HEAD_DIM ** -0.5)
    p = jax.nn.softmax(s, axis=-1).astype(v.dtype)
    o = jnp.einsum('bhsm,bmhd->bshd', p, v).reshape(B, S, MEM_WIDTH)
    return o @ w_mem_o


def expert_choice_moe(h, w_router, w_gate, w_up, w_down):
    B, S, D = h.shape
    cap = EC_CAPACITY_FACTOR * S // N_EXPERTS
    affinity = jax.nn.softmax((h @ w_router).astype(jnp.float32), axis=-1)
    gate, idx = lax.top_k(jnp.swapaxes(affinity, 1, 2), cap)
    x_in = jax.vmap(lambda hb, ib: hb[ib])(h, idx)
    a = jnp.einsum('becd,edf->becf', x_in, w_gate)
    u = jnp.einsum('becd,edf->becf', x_in, w_up)
    y = jnp.einsum('becf,efd->becd', jax.nn.silu(a) * u, w_down)
    y = y * gate[..., None].astype(y.dtype)
    return jax.vmap(lambda ib, yb: jnp.zeros((S, D), yb.dtype).at[ib.reshape(-1)].add(yb.reshape(-1, D)))(idx, y)


def setup_inputs(seed: int = 0) -> dict:
    key = jax.random.key(seed)
    ks = iter(jax.random.split(key, 32))
    L = DEPTH

    def w(shape, fan_in):
        return jax.random.normal(next(ks), shape, jnp.float32) * fan_in ** -0.5

    def gain(shape):
        return 1.0 + 0.02 * jax.random.normal(next(ks), shape, jnp.float32)

    return {
        "x": jax.random.normal(next(ks), (BATCH, SEQ, D_MODEL), jnp.float32),
        "mem": jax.random.normal(next(ks), (BATCH, MEM_TOKENS, D_MODEL), jnp.float32),
        "ln_mix": gain((L, D_MODEL)),
        "w_in": w((L, D_MODEL, IN_WIDTH), D_MODEL),
        "gqa_q_norm": gain((L, GQA_HEAD_DIM)),
        "gqa_k_norm": gain((L, GQA_HEAD_DIM)),
        "mla_q_norm": gain((L, MLA_Q_RANK)),
        "mla_kv_norm": gain((L, MLA_KV_RANK)),
        "w_q_b": w((L, MLA_Q_RANK, MLA_HEADS * MLA_QK_DIM), MLA_Q_RANK),
        "w_kv_b": w((L, MLA_KV_RANK, MLA_HEADS * (MLA_NOPE_DIM + MLA_V_DIM)), MLA_KV_RANK),
        "out_norm_gqa": gain((L, GQA_WIDTH)),
        "out_norm_mla": gain((L, MLA_WIDTH)),
        "w_o": w((L, MIX_WIDTH, D_MODEL), MIX_WIDTH),
        "ln_mem": gain((L, D_MODEL)),
        "ln_mem_kv": gain((L, D_MODEL)),
        "w_mem_q": w((L, D_MODEL, MEM_WIDTH), D_MODEL),
        "w_mem_kv": w((L, D_MODEL, 2 * MEM_WIDTH), D_MODEL),
        "w_mem_o": w((L, MEM_WIDTH, D_MODEL), MEM_WIDTH),
        "ln_ffn": gain((L, D_MODEL)),
        "w_router": w((L, D_MODEL, N_EXPERTS), D_MODEL),
        "w_gate": w((L, N_EXPERTS, D_MODEL, EXPERT_FF), D_MODEL),
        "w_up": w((L, N_EXPERTS, D_MODEL, EXPERT_FF), D_MODEL),
        "w_down": w((L, N_EXPERTS, EXPERT_FF, D_MODEL), EXPERT_FF),
        "ln_final": gain((D_MODEL,)),
    }


def reference(x, mem, ln_mix, w_in, gqa_q_norm, gqa_k_norm, mla_q_norm, mla_kv_norm, w_q_b, w_kv_b,
              out_norm_gqa, out_norm_mla, w_o, ln_mem, ln_mem_kv, w_mem_q, w_mem_kv, w_mem_o,
              ln_ffn, w_router, w_gate, w_up, w_down, ln_final):
    S = x.shape[1]
    cos_g, sin_g = axial_rope_angles(S, GQA_HEAD_DIM)
    cos_m, sin_m = axial_rope_angles(S, MLA_ROPE_DIM)
    for l in range(DEPTH):
        h = rms_norm(x, ln_mix[l])
        x = x + parallel_mixer(h, w_in[l], gqa_q_norm[l], gqa_k_norm[l], mla_q_norm[l], mla_kv_norm[l],
                               w_q_b[l], w_kv_b[l], out_norm_gqa[l], out_norm_mla[l], w_o[l],
                               cos_g, sin_g, cos_m, sin_m)
        h = rms_norm(x, ln_mem[l])
        x = x + memory_cross_attention(h, rms_norm(mem, ln_mem_kv[l]), w_mem_q[l], w_mem_kv[l], w_mem_o[l])
        h = rms_norm(x, ln_ffn[l])
        x = x + expert_choice_moe(h, w_router[l], w_gate[l], w_up[l], w_down[l])
    return rms_norm(x, ln_final)
```

```python
from contextlib import ExitStack
import numpy as np
import ml_dtypes
import concourse.bass as bass
import concourse.mybir as mybir
from concourse.bass_utils import run_bass_kernel_spmd

F32 = mybir.dt.float32
BF16 = mybir.dt.bfloat16
I32 = mybir.dt.int32
ALU = mybir.AluOpType
AF = mybir.ActivationFunctionType
AX = mybir.AxisListType

S_LEN = 4096
D = 1024
NT = 32
DEPTH = 4
EPS = 1e-6
IN_W = 1184
EPOCH = 30000


class Sched:
    def __init__(self, nc, es, dma_slots=8):
        self.nc = nc
        self.es = es
        self.engs = {"pe": nc.tensor, "act": nc.scalar, "dve": nc.vector, "pool": nc.gpsimd, "sp": nc.sync}
        self.cnt = {k: 0 for k in self.engs}
        self.esems = {k: [] for k in self.engs}
        self.waited = {k: {} for k in self.engs}
        self.lastw = {}
        self.readers = {}
        self.dma_pool = {}
        self.dma_slots = dma_slots
        self.nsem = 0
        self.ninstr = 0

    def _newsem(self, name):
        self.nsem += 1
        return self.es.enter_context(self.nc.semaphore(name))

    def _eng_event(self, eng):
        c = self.cnt[eng]
        ep, loc = divmod(c, EPOCH)
        while len(self.esems[eng]) <= ep:
            self.esems[eng].append(self._newsem(f"e_{eng}_{len(self.esems[eng])}"))
        self.cnt[eng] = c + 1
        return (self.esems[eng][ep], loc + 1, eng)

    def _dma_event(self, q):
        st = self.dma_pool.setdefault(q, {"sems": [], "uses": [], "i": 0})
        if len(st["sems"]) < self.dma_slots:
            st["sems"].append(self._newsem(f"d_{q}_{len(st['sems'])}"))
            st["uses"].append(0)
        i = st["i"] % self.dma_slots
        st["i"] += 1
        sem = st["sems"][i]
        if st["uses"][i] > 0:
            self._wait(q, (sem, 16 * st["uses"][i], None))
        st["uses"][i] += 1
        return (sem, 16 * st["uses"][i], None)

    def _wait(self, eng, ev):
        sem, val, src = ev
        if src == eng and eng == "pe":
            return
        w = self.waited[eng]
        k = id(sem)
        if w.get(k, 0) >= val:
            return
        w[k] = val
        self.engs[eng].wait_ge(sem, val)
        self.ninstr += 1

    def _deps(self, eng, reads, writes):
        for k in reads:
            ev = self.lastw.get(k)
            if ev is not None:
                self._wait(eng, ev)
        for k in writes:
            ev = self.lastw.get(k)
            if ev is not None:
                self._wait(eng, ev)
            for ev in self.readers.get(k, {}).values():
                self._wait(eng, ev)

    def _record(self, ev, reads, writes):
        for k in writes:
            self.lastw[k] = ev
            self.readers[k] = {}
        for k in reads:
            if k in writes:
                continue
            self.readers.setdefault(k, {})[id(ev[0])] = ev

    def op(self, eng, fn, reads=(), writes=()):
        self._deps(eng, reads, writes)
        ev = self._eng_event(eng)
        fn(self.engs[eng]).then_inc(ev[0], 1)
        self._record(ev, reads, writes)
        self.ninstr += 1
        return ev

    def dma(self, q, fn, reads=(), writes=()):
        self._deps(q, reads, writes)
        ev = self._dma_event(q)
        fn(self.engs[q]).then_inc(ev[0], 16)
        self._record(ev, reads, writes)
        self.ninstr += 1
        return ev

    def all_events(self):
        evs = []
        for eng in self.engs:
            c = self.cnt[eng]
            if c > 0:
                ep, loc = divmod(c - 1, EPOCH)
                evs.append((self.esems[eng][ep], loc + 1, eng))
        for q, st in self.dma_pool.items():
            for sem, u in zip(st["sems"], st["uses"]):
                if u:
                    evs.append((sem, 16 * u, None))
        return evs

    def barrier(self, engines=None):
        evs = self.all_events()
        for eng in (engines or list(self.engs)):
            for ev in evs:
                if ev[2] == eng:
                    continue
                self._wait(eng, ev)
        if engines is None:
            self.lastw = {}
            self.readers = {}


class Ring:
    def __init__(self, alloc, name, shape, dt, n):
        self.items = [(alloc(f"{name}{i}", shape, dt), f"{name}{i}") for i in range(n)]
        self.i = 0

    def next(self):
        it = self.items[self.i % len(self.items)]
        self.i += 1
        return it


def build_nc(nlayers=DEPTH, dbg=None, stop_after=None):
    dbg = dbg or {}
    nc = bass.Bass("TRN2", target_bir_lowering=False)
    L = DEPTH

    def din(name, shape, dt=F32):
        return nc.dram_tensor(name, list(shape), dt, kind="ExternalInput").ap()

    def dscr(name, shape, dt):
        return nc.dram_tensor(name, list(shape), dt, kind="Internal").ap()

    x_in = din("x", [S_LEN, D])
    mem_in = din("mem", [256, D])
    ln_mix = din("ln_mix", [L, D]); w_in = din("w_in", [L, D, IN_W])
    gqa_q_norm = din("gqa_q_norm", [L, 64]); gqa_k_norm = din("gqa_k_norm", [L, 64])
    mla_q_norm = din("mla_q_norm", [L, 256]); mla_kv_norm = din("mla_kv_norm", [L, 128])
    w_q_b = din("w_q_b", [L, 256, 768]); w_kv_b = din("w_kv_b", [L, 128, 1024])
    out_norm_gqa = din("out_norm_gqa", [L, 512]); out_norm_mla = din("out_norm_mla", [L, 512])
    w_o = din("w_o", [L, 1024, 1024])
    ln_mem = din("ln_mem", [L, D]); ln_mem_kv = din("ln_mem_kv", [L, D])
    w_mem_q = din("w_mem_q", [L, D, 512]); w_mem_kv = din("w_mem_kv", [L, D, 1024]); w_mem_o = din("w_mem_o", [L, 512, D])
    ln_ffn = din("ln_ffn", [L, D]); w_router = din("w_router", [L, D, 16])
    w_gate = din("w_gate", [L, 16, D, 512]); w_up = din("w_up", [L, 16, D, 512]); w_down = din("w_down", [L, 16, 512, D])
    ln_final = din("ln_final", [D])
    ropeg = din("ropeg", [S_LEN, 64])
    ropem = din("ropem", [S_LEN, 32])
    out = nc.dram_tensor("out", [S_LEN, D], F32, kind="ExternalOutput").ap()

    xres = dscr("xres", [S_LEN, D], F32)
    QgT = dscr("QgT", [128, 4, S_LEN], BF16)
    KgT = dscr("KgT", [128, 2, S_LEN], BF16)
    QmT = dscr("QmT", [96, 8, S_LEN], BF16)
    KmT = dscr("KmT", [96, 8, S_LEN], BF16)
    Odr = dscr("Odr", [S_LEN, 1024], F32)
    hdr = dscr("hdr", [S_LEN, D], BF16)
    affdr = dscr("affdr", [S_LEN, 16], F32)

    dbg_out = {}

    def dbgt(name, shape, dt=F32):
        t = nc.dram_tensor("dbg_" + name, list(shape), dt, kind="ExternalOutput").ap()
        dbg_out[name] = t
        return t

    with ExitStack() as es:
        S = Sched(nc, es)

        uid = [0]

        def sb(name, shape, dt, stack=es):
            uid[0] += 1
            return stack.enter_context(nc.sbuf_tensor(f"{name}_{uid[0]}", list(shape), dt))

        def ps(name, shape, dt, stack=es):
            return stack.enter_context(nc.psum_tensor(name, list(shape), dt))

        T0 = ps("T0", [128, 1024], BF16); T1 = ps("T1", [128, 1024], BF16)
        FB = [ps(f"F{i}", [128, 512], F32) for i in range(6)]
        FK = [f"F{i}" for i in range(6)]

        identb = sb("identb", [128, 128], BF16)
        identf = sb("identf", [128, 128], F32)
        for t_, k_ in ((identb, "identb"), (identf, "identf")):
            S.op("pool", lambda e, t_=t_: e.memset(t_[:], 0.0), writes=[k_])
            S.op("pool", lambda e, t_=t_: e.affine_select(out=t_[:], in_=t_[:], pattern=[[-1, 128]], compare_op=ALU.not_equal, fill=1.0, base=0, channel_multiplier=1), reads=[k_], writes=[k_])
        neghalf = sb("neghalf", [128, 16], F32)
        S.op("pool", lambda e: e.memset(neghalf[:], -0.5), writes=["neghalf"])
        ropeg_sb = sb("ropeg_sb", [128, NT, 64], F32)
        ropem_sb = sb("ropem_sb", [128, NT, 32], F32)
        S.dma("sp", lambda e: e.dma_start(out=ropeg_sb[:], in_=ropeg.rearrange("(j p) c -> p j c", p=128)), writes=["ropeg"])
        S.dma("sp", lambda e: e.dma_start(out=ropem_sb[:], in_=ropem.rearrange("(j p) c -> p j c", p=128)), writes=["ropem"])
        growA = sb("growA", [96, 128], F32); growB = sb("growB", [44, 128], F32)
        gcA = sb("gcA", [128, 96], F32); gcB = sb("gcB", [128, 44], F32)
        for r0, src in ((0, ln_mix), (32, ln_mem), (64, ln_mem_kv)):
            S.dma("sp", lambda e, r0=r0, src=src: e.dma_start(out=growA[r0:r0 + 32, :], in_=src.rearrange("l (kc p) -> (l kc) p", p=128)), writes=["growA"])
        for r0, n_, src in ((0, 8, mla_q_norm), (8, 4, mla_kv_norm), (12, 16, out_norm_gqa), (28, 16, out_norm_mla)):
            S.dma("sp", lambda e, r0=r0, n_=n_, src=src: e.dma_start(out=growB[r0:r0 + n_, :], in_=src.rearrange("l (kc p) -> (l kc) p", p=128)), writes=["growB"])
        S.op("pe", lambda e: e.transpose(out=FB[0][:, 0:96], in_=growA[:, :], identity=identf[0:96, 0:96]), reads=["growA", "identf"], writes=["F0"])
        S.op("pe", lambda e: e.transpose(out=FB[1][:, 0:44], in_=growB[:, :], identity=identf[0:44, 0:44]), reads=["growB", "identf"], writes=["F1"])
        S.op("dve", lambda e: e.tensor_copy(out=gcA[:, :], in_=FB[0][:, 0:96]), reads=["F0"], writes=["gcA"])
        S.op("dve", lambda e: e.tensor_copy(out=gcB[:, :], in_=FB[1][:, 0:44]), reads=["F1"], writes=["gcB"])
        gcAv = gcA[:, :].rearrange("p (g l k) -> p g l k", g=3, l=L)
        gc_mix, gc_mem, gc_memkv = gcAv[:, 0], gcAv[:, 1], gcAv[:, 2]
        gc_q = gcB[:, 0:8].rearrange("p (l k) -> p l k", l=L); gc_kv = gcB[:, 8:12].rearrange("p (l k) -> p l k", l=L)
        gc_og = gcB[:, 12:28].rearrange("p (l k) -> p l k", l=L); gc_om = gcB[:, 28:44].rearrange("p (l k) -> p l k", l=L)
        invn = sb("invn", [128, 12], F32)
        S.op("pool", lambda e: e.memset(invn[:, 0:10], 1.0 / 64), writes=["invn"])
        S.op("pool", lambda e: e.memset(invn[:, 10:11], 1.0 / 256), writes=["invn"])
        S.op("pool", lambda e: e.memset(invn[:, 11:12], 1.0 / 128), writes=["invn"])

        def rstd_from_ss(ss_ap, n, out_ap, key_in, key_out, ncols=1):
            S.op("dve", lambda e: e.tensor_scalar(out=out_ap, in0=ss_ap, scalar1=1.0 / n, scalar2=EPS, op0=ALU.mult, op1=ALU.add), reads=[key_in], writes=[key_out])
            S.op("pool", lambda e: e.tensor_tensor(out=out_ap, in0=out_ap, in1=neghalf[:, 0:ncols], op=ALU.pow), reads=[key_out, "neghalf"], writes=[key_out])

        wstage = Ring(sb, "wst", [128, IN_W], F32, 3)

        def load_weight(dst, dkey, src2d, KC, N, gain=None, gkey=None, q="sp"):
            for kc in range(KC):
                n0 = 0
                while n0 < N:
                    nn = min(IN_W, N - n0)
                    st, sk = wstage.next()
                    S.dma(q, lambda e, st=st, kc=kc, n0=n0, nn=nn: e.dma_start(out=st[:, 0:nn], in_=src2d[kc * 128:(kc + 1) * 128, n0:n0 + nn]), writes=[sk])
                    if gain is not None:
                        S.op("pool", lambda e, st=st, kc=kc, n0=n0, nn=nn: e.tensor_scalar(out=dst[:, kc, n0:n0 + nn], in0=st[:, 0:nn], scalar1=gain[:, kc:kc + 1], scalar2=None, op0=ALU.mult), reads=[sk, gkey], writes=[dkey])
                    else:
                        S.op("pool", lambda e, st=st, kc=kc, n0=n0, nn=nn: e.tensor_copy(out=dst[:, kc, n0:n0 + nn], in_=st[:, 0:nn]), reads=[sk], writes=[dkey])
                    n0 += nn

        def norm_transpose(xt, xkey, hn, hkey, XTdst, XTkey, tcol, rstd, rkey, scr, scrkey, gain_b=None, gbkey=None, halves=1):
            w = D // halves
            for hf in range(halves):
                S.op("dve", lambda e, hf=hf: e.scalar_tensor_tensor(out=scr[:, hf * w:(hf + 1) * w], in0=xt[:, hf * w:(hf + 1) * w], scalar=1.0, in1=xt[:, hf * w:(hf + 1) * w], op0=ALU.mult, op1=ALU.mult, accum_out=rstd[:, hf:hf + 1]),
                     reads=[xkey], writes=[scrkey, rkey])
            rstd_from_ss(rstd[:, 0:halves], w, rstd[:, 0:halves], rkey, rkey, ncols=halves)
            for hf in range(halves):
                if gain_b is None:
                    S.op("dve", lambda e, hf=hf: e.tensor_scalar(out=hn[:, hf * w:(hf + 1) * w], in0=xt[:, hf * w:(hf + 1) * w], scalar1=rstd[:, hf:hf + 1], scalar2=None, op0=ALU.mult), reads=[xkey, rkey], writes=[hkey])
                else:
                    S.op("dve", lambda e, hf=hf: e.scalar_tensor_tensor(out=hn[:, hf * w:(hf + 1) * w], in0=xt[:, hf * w:(hf + 1) * w], scalar=rstd[:, hf:hf + 1], in1=gain_b[:, hf * w:(hf + 1) * w], op0=ALU.mult, op1=ALU.mult), reads=[xkey, rkey, gbkey], writes=[hkey])
            if XTdst is None:
                return
            for kc in range(8):
                S.op("pe", lambda e, kc=kc: e.transpose(out=T0[:, kc * 128:(kc + 1) * 128], in_=hn[:, kc * 128:(kc + 1) * 128], identity=identb[:]), reads=[hkey, "identb"], writes=["T0"])
            S.op("dve", lambda e: e.tensor_copy(out=XTdst[:, :, tcol:tcol + 128], in_=T0[:, :].rearrange("p (k t) -> p k t", k=8)), reads=["T0"], writes=[XTkey])

        def attention(QT, qkey, KT, kkey, nk_tiles, Vfn, vkey, dv, scale, bias, out_cb, Sring, PTring, Oring_sets, nqc=8):
            per_bank = 512 // (dv + 1)
            per_bank = min(per_bank, 4)
            for qc in range(nqc):
                oset = Oring_sets[qc % len(Oring_sets)]
                okeys = [k for (_, k) in oset]
                first = True
                for kt in range(nk_tiles):
                    (sp_, skey) = Sring.next()
                    S.op("pe", lambda e, sp_=sp_, kt=kt, qc=qc: e.matmul(sp_[:, :], lhsT=KT[:, kt * 128:(kt + 1) * 128], rhs=QT[:, qc * 512:(qc + 1) * 512], start=True, stop=True), reads=[qkey, kkey], writes=[skey])
                    (pt_, pkey) = PTring.next()
                    if bias is None:
                        S.op("act", lambda e, sp_=sp_, pt_=pt_: e.activation(out=pt_[:, :], in_=sp_[:, :], func=AF.Exp, scale=scale), reads=[skey], writes=[pkey])
                    else:
                        S.op("act", lambda e, sp_=sp_, pt_=pt_: e.activation(out=pt_[:, :], in_=sp_[:, :], func=AF.Exp, scale=scale, bias=bias[0]), reads=[skey, bias[1]], writes=[pkey])
                    for qs in range(4):
                        ob, okey = oset[qs // per_bank]
                        o0 = (qs % per_bank) * (dv + 1)
                        st_flag = (kt == 0 and (qs % per_bank) == 0)
                        S.op("pe", lambda e, ob=ob, o0=o0, pt_=pt_, qs=qs, kt=kt, st_flag=st_flag: e.matmul(ob[:, o0:o0 + dv + 1], lhsT=pt_[:, qs * 128:(qs + 1) * 128], rhs=Vfn(kt), start=st_flag, stop=(kt == nk_tiles - 1), skip_group_check=True),
                             reads=[pkey, vkey], writes=[okey])
                out_cb(qc, oset, per_bank)

        def layer(l, x_src, xs_key):
            with ExitStack() as ph:
                a = lambda name, shape, dt: sb(name, shape, dt, ph)
                Win = a("Win", [128, 8, IN_W], BF16); Wqb = a("Wqb", [128, 2, 768], BF16); Wkvb = a("Wkvb", [128, 1, 1024], BF16)
                load_weight(Win, "Win", w_in[l], 8, IN_W, gc_mix[:, l, :], "gcA")
                load_weight(Wqb, "Wqb", w_q_b[l], 2, 768, gc_q[:, l, :], "gcB")
                load_weight(Wkvb, "Wkvb", w_kv_b[l], 1, 1024, gc_kv[:, l, :], "gcB")
                gq = a("gq", [128, 64], F32); gk = a("gk", [128, 64], F32)
                S.dma("sp", lambda e: e.dma_start(out=gq[:], in_=gqa_q_norm[l, :].partition_broadcast(128)), writes=["gq"])
                S.dma("sp", lambda e: e.dma_start(out=gk[:], in_=gqa_k_norm[l, :].partition_broadcast(128)), writes=["gk"])
                Vg = a("Vg", [128, NT, 2, 65], BF16); Vm = a("Vm", [128, NT, 8, 65], BF16)
                S.op("pool", lambda e: e.memset(Vg[:, :, :, 64:65], 1.0), writes=["Vg"])
                S.op("pool", lambda e: e.memset(Vm[:, :, :, 64:65], 1.0), writes=["Vm"])
                with ExitStack() as p1:
                    b = lambda name, shape, dt: sb(name, shape, dt, p1)
                    xtR = Ring(b, "xt", [128, D], F32, 2); hnR = Ring(b, "hn", [128, D], BF16, 2)
                    XTR = Ring(b, "XT", [128, 8, 128], BF16, 2)
                    scr = b("scr", [128, D], F32)
                    prR = Ring(b, "pr", [128, IN_W], F32, 2)
                    stR = Ring(b, "st", [128, 12], F32, 2)
                    rsx = Ring(b, "rsx", [128, 2], F32, 2)
                    qn = b("qn", [128, 10, 64], F32); tmpa = b("tmpa", [128, 10, 32], F32); tmpb = b("tmpb", [128, 10, 32], F32)
                    rot_f = b("rot", [128, 640], BF16); kdup_f = b("kdup", [128, 256], BF16)
                    rot = rot_f[:, :].rearrange("p (h d) -> p h d", h=10); kdup = kdup_f[:, :].rearrange("p (g u d) -> p g u d", g=2, u=2)
                    cb = b("cb", [128, 384], BF16); cT = b("cT", [128, 3, 128], BF16)
                    qm32_f = b("qm32", [128, 768], F32); qmb_f = b("qmb", [128, 768], BF16); km_f = b("km", [128, 768], BF16)
                    qm32 = qm32_f[:, :].rearrange("p (h d) -> p h d", h=8); qmb = qmb_f[:, :].rearrange("p (h d) -> p h d", h=8); km = km_f[:, :].rearrange("p (h d) -> p h d", h=8)
                    krot = b("krot", [128, 32], F32)
                    QKst = Ring(b, "QKst", [128, 6, 512], BF16, 2)
                    QmSt = Ring(b, "QmSt", [128, 8, 512], BF16, 1); KmSt = Ring(b, "KmSt", [128, 8, 512], BF16, 1)
                    for c in range(8):
                        qkst, qkk = QKst.next(); qmst, qmk = QmSt.next(); kmst, kmk = KmSt.next()
                        for t in range(4):
                            j = c * 4 + t
                            xt, xk = xtR.next(); hn, hk = hnR.next(); XT, XTk = XTR.next(); pr, prk = prR.next(); st, stk = stR.next(); rs, rsk = rsx.next()
                            S.dma("sp", lambda e, xt=xt, j=j: e.dma_start(out=xt[:], in_=x_src[j * 128:(j + 1) * 128, :]), reads=[xs_key], writes=[xk])
                            norm_transpose(xt, xk, hn, hk, XT, XTk, 0, rs, rsk, scr, "scr")
                            for nb, (n0, nn) in enumerate(((0, 512), (512, 512), (1024, 160))):
                                for kc in range(8):
                                    S.op("pe", lambda e, nb=nb, n0=n0, nn=nn, kc=kc, XT=XT: e.matmul(FB[nb][:, 0:nn], lhsT=XT[:, kc, :], rhs=Win[:, kc, n0:n0 + nn], start=(kc == 0), stop=(kc == 7)), reads=[XTk, "Win"], writes=[FK[nb]])
                                S.op("act", lambda e, nb=nb, n0=n0, nn=nn, pr=pr: e.activation(out=pr[:, n0:n0 + nn], in_=FB[nb][:, 0:nn], func=AF.Copy), reads=[FK[nb]], writes=[prk])
                            S.op("dve", lambda e, pr=pr: e.tensor_tensor(out=scr[:, 0:640], in0=pr[:, 0:640], in1=pr[:, 0:640], op=ALU.mult), reads=[prk], writes=["scr"])
                            S.op("dve", lambda e, st=st: e.tensor_reduce(out=st[:, 0:10], in_=scr[:, 0:640].rearrange("p (h d) -> p h d", h=10), axis=AX.X, op=ALU.add), reads=["scr"], writes=[stk])
                            S.op("dve", lambda e, pr=pr, st=st: e.scalar_tensor_tensor(out=scr[:, 0:256], in0=pr[:, 768:1024], scalar=1.0, in1=pr[:, 768:1024], op0=ALU.mult, op1=ALU.mult, accum_out=st[:, 10:11]), reads=[prk], writes=["scr", stk])
                            S.op("dve", lambda e, pr=pr, st=st: e.scalar_tensor_tensor(out=scr[:, 0:128], in0=pr[:, 1024:1152], scalar=1.0, in1=pr[:, 1024:1152], op0=ALU.mult, op1=ALU.mult, accum_out=st[:, 11:12]), reads=[prk], writes=["scr", stk])
                            S.op("dve", lambda e, st=st: e.tensor_tensor(out=st[:, :], in0=st[:, :], in1=invn[:, :], op=ALU.mult), reads=[stk, "invn"], writes=[stk])
                            S.op("dve", lambda e, st=st: e.tensor_scalar(out=st[:, :], in0=st[:, :], scalar1=EPS, scalar2=None, op0=ALU.add), reads=[stk], writes=[stk])
                            S.op("pool", lambda e, st=st: e.tensor_tensor(out=st[:, :], in0=st[:, :], in1=neghalf[:, 0:12], op=ALU.pow), reads=[stk, "neghalf"], writes=[stk])
                            prqk = pr[:, 0:640].rearrange("p (h d) -> p h d", h=10)
                            S.op("dve", lambda e, st=st, prqk=prqk: e.tensor_tensor(out=qn[:, :, :], in0=prqk, in1=st[:, 0:10].unsqueeze(2).to_broadcast([128, 10, 64]), op=ALU.mult), reads=[prk, stk], writes=["qn"])
                            S.op("dve", lambda e: e.tensor_tensor(out=qn[:, 0:8, :], in0=qn[:, 0:8, :], in1=gq[:, :].unsqueeze(1).to_broadcast([128, 8, 64]), op=ALU.mult), reads=["qn", "gq"], writes=["qn"])
                            S.op("dve", lambda e: e.tensor_tensor(out=qn[:, 8:10, :], in0=qn[:, 8:10, :], in1=gk[:, :].unsqueeze(1).to_broadcast([128, 2, 64]), op=ALU.mult), reads=["qn", "gk"], writes=["qn"])
                            cg = ropeg_sb[:, j, 0:32].unsqueeze(1).to_broadcast([128, 10, 32]); sg = ropeg_sb[:, j, 32:64].unsqueeze(1).to_broadcast([128, 10, 32])
                            S.op("dve", lambda e, cg=cg: e.tensor_tensor(out=tmpa[:, :, :], in0=qn[:, :, 0:32], in1=cg, op=ALU.mult), reads=["qn", "ropeg"], writes=["tmpa"])
                            S.op("dve", lambda e, sg=sg: e.tensor_tensor(out=tmpb[:, :, :], in0=qn[:, :, 32:64], in1=sg, op=ALU.mult), reads=["qn", "ropeg"], writes=["tmpb"])
                            S.op("dve", lambda e: e.tensor_tensor(out=rot[:, :, 0:32], in0=tmpa[:, :, :], in1=tmpb[:, :, :], op=ALU.subtract), reads=["tmpa", "tmpb"], writes=["rot"])
                            S.op("dve", lambda e, cg=cg: e.tensor_tensor(out=tmpa[:, :, :], in0=qn[:, :, 32:64], in1=cg, op=ALU.mult), reads=["qn", "ropeg"], writes=["tmpa"])
                            S.op("dve", lambda e, sg=sg: e.tensor_tensor(out=tmpb[:, :, :], in0=qn[:, :, 0:32], in1=sg, op=ALU.mult), reads=["qn", "ropeg"], writes=["tmpb"])
                            S.op("dve", lambda e: e.tensor_tensor(out=rot[:, :, 32:64], in0=tmpa[:, :, :], in1=tmpb[:, :, :], op=ALU.add), reads=["tmpa", "tmpb"], writes=["rot"])
                            S.op("dve", lambda e: e.tensor_copy(out=kdup[:, :, :, :], in_=rot[:, 8:10, :].unsqueeze(2).to_broadcast([128, 2, 2, 64])), reads=["rot"], writes=["kdup"])
                            for p_ in range(4):
                                S.op("pe", lambda e, p_=p_: e.transpose(out=T1[:, p_ * 128:(p_ + 1) * 128], in_=rot_f[:, p_ * 128:(p_ + 1) * 128], identity=identb[:]), reads=["rot", "identb"], writes=["T1"])
                            for g_ in range(2):
                                S.op("pe", lambda e, g_=g_: e.transpose(out=T1[:, (4 + g_) * 128:(5 + g_) * 128], in_=kdup_f[:, g_ * 128:(g_ + 1) * 128], identity=identb[:]), reads=["kdup", "identb"], writes=["T1"])
                            S.op("dve", lambda e, qkst=qkst, t=t: e.tensor_copy(out=qkst[:, :, t * 128:(t + 1) * 128], in_=T1[:, 0:768].rearrange("p (k t) -> p k t", k=6)), reads=["T1"], writes=[qkk])
                            S.op("act", lambda e, pr=pr, j=j: e.activation(out=Vg[:, j, :, 0:64], in_=pr[:, 640:768].rearrange("p (g d) -> p g d", g=2), func=AF.Copy), reads=[prk], writes=["Vg"])
                            S.op("dve", lambda e, pr=pr, st=st: e.tensor_scalar(out=cb[:, 0:256], in0=pr[:, 768:1024], scalar1=st[:, 10:11], scalar2=None, op0=ALU.mult), reads=[prk, stk], writes=["cb"])
                            S.op("dve", lambda e, pr=pr, st=st: e.tensor_scalar(out=cb[:, 256:384], in0=pr[:, 1024:1152], scalar1=st[:, 11:12], scalar2=None, op0=ALU.mult), reads=[prk, stk], writes=["cb"])
                            for i_ in range(3):
                                S.op("pe", lambda e, i_=i_: e.transpose(out=T1[:, i_ * 128:(i_ + 1) * 128], in_=cb[:, i_ * 128:(i_ + 1) * 128], identity=identb[:]), reads=["cb", "identb"], writes=["T1"])
                            S.op("dve", lambda e: e.tensor_copy(out=cT[:, :, :], in_=T1[:, 0:384].rearrange("p (k t) -> p k t", k=3)), reads=["T1"], writes=["cT"])
                            for nb, (n0, nn) in ((3, (0, 512)), (4, (512, 256))):
                                for kc in range(2):
                                    S.op("pe", lambda e, nb=nb, n0=n0, nn=nn, kc=kc: e.matmul(FB[nb][:, 0:nn], lhsT=cT[:, kc, :], rhs=Wqb[:, kc, n0:n0 + nn], start=(kc == 0), stop=(kc == 1)), reads=["cT", "Wqb"], writes=[FK[nb]])
                            qm32f = qm32_f
                            S.op("act", lambda e: e.activation(out=qm32f[:, 0:512], in_=FB[3][:, 0:512], func=AF.Copy), reads=[FK[3]], writes=["qm32"])
                            S.op("act", lambda e: e.activation(out=qm32f[:, 512:768], in_=FB[4][:, 0:256], func=AF.Copy), reads=[FK[4]], writes=["qm32"])
                            for nb in range(2):
                                S.op("pe", lambda e, nb=nb: e.matmul(FB[nb][:, 0:512], lhsT=cT[:, 2, :], rhs=Wkvb[:, 0, nb * 512:(nb + 1) * 512], start=True, stop=True), reads=["cT", "Wkvb"], writes=[FK[nb]])
                                kvv = FB[nb][:, 0:512].rearrange("p (h d) -> p h d", h=4)
                                S.op("act", lambda e, kvv=kvv, nb=nb, j=j: e.activation(out=Vm[:, j, nb * 4:(nb + 1) * 4, 0:64], in_=kvv[:, :, 64:128], func=AF.Copy), reads=[FK[nb]], writes=["Vm"])
                                S.op("dve", lambda e, kvv=kvv, nb=nb: e.tensor_copy(out=km[:, nb * 4:(nb + 1) * 4, 0:64], in_=kvv[:, :, 0:64]), reads=[FK[nb]], writes=["km"])
                            cm = ropem_sb[:, j, 0:16]; sm = ropem_sb[:, j, 16:32]
                            cm8 = cm.unsqueeze(1).to_broadcast([128, 8, 16]); sm8 = sm.unsqueeze(1).to_broadcast([128, 8, 16])
                            ta = tmpa[:, 0:8, 0:16]; tb = tmpb[:, 0:8, 0:16]
                            S.op("dve", lambda e: e.tensor_copy(out=qmb[:, :, 0:64], in_=qm32[:, :, 0:64]), reads=["qm32"], writes=["qmb"])
                            S.op("dve", lambda e, cm8=cm8, ta=ta: e.tensor_tensor(out=ta, in0=qm32[:, :, 64:80], in1=cm8, op=ALU.mult), reads=["qm32", "ropem"], writes=["tmpa"])
                            S.op("dve", lambda e, sm8=sm8, tb=tb: e.tensor_tensor(out=tb, in0=qm32[:, :, 80:96], in1=sm8, op=ALU.mult), reads=["qm32", "ropem"], writes=["tmpb"])
                            S.op("dve", lambda e, ta=ta, tb=tb: e.tensor_tensor(out=qmb[:, :, 64:80], in0=ta, in1=tb, op=ALU.subtract), reads=["tmpa", "tmpb"], writes=["qmb"])
                            S.op("dve", lambda e, cm8=cm8, ta=ta: e.tensor_tensor(out=ta, in0=qm32[:, :, 80:96], in1=cm8, op=ALU.mult), reads=["qm32", "ropem"], writes=["tmpa"])
                            S.op("dve", lambda e, sm8=sm8, tb=tb: e.tensor_tensor(out=tb, in0=qm32[:, :, 64:80], in1=sm8, op=ALU.mult), reads=["qm32", "ropem"], writes=["tmpb"])
                            S.op("dve", lambda e, ta=ta, tb=tb: e.tensor_tensor(out=qmb[:, :, 80:96], in0=ta, in1=tb, op=ALU.add), reads=["tmpa", "tmpb"], writes=["qmb"])
                            ta1 = tmpa[:, 8, 0:16]; tb1 = tmpb[:, 8, 0:16]
                            S.op("dve", lambda e, pr=pr, cm=cm, ta1=ta1: e.tensor_tensor(out=ta1, in0=pr[:, 1152:1168], in1=cm, op=ALU.mult), reads=[prk, "ropem"], writes=["tmpa"])
                            S.op("dve", lambda e, pr=pr, sm=sm, tb1=tb1: e.tensor_tensor(out=tb1, in0=pr[:, 1168:1184], in1=sm, op=ALU.mult), reads=[prk, "ropem"], writes=["tmpb"])
                            S.op("dve", lambda e, ta1=ta1, tb1=tb1: e.tensor_tensor(out=krot[:, 0:16], in0=ta1, in1=tb1, op=ALU.subtract), reads=["tmpa", "tmpb"], writes=["krot"])
                            S.op("dve", lambda e, pr=pr, cm=cm, ta1=ta1: e.tensor_tensor(out=ta1, in0=pr[:, 1168:1184], in1=cm, op=ALU.mult), reads=[prk, "ropem"], writes=["tmpa"])
                            S.op("dve", lambda e, pr=pr, sm=sm, tb1=tb1: e.tensor_tensor(out=tb1, in0=pr[:, 1152:1168], in1=sm, op=ALU.mult), reads=[prk, "ropem"], writes=["tmpb"])
                            S.op("dve", lambda e, ta1=ta1, tb1=tb1: e.tensor_tensor(out=krot[:, 16:32], in0=ta1, in1=tb1, op=ALU.add), reads=["tmpa", "tmpb"], writes=["krot"])
                            S.op("dve", lambda e: e.tensor_copy(out=km[:, :, 64:96], in_=krot[:, :].unsqueeze(1).to_broadcast([128, 8, 32])), reads=["krot"], writes=["km"])
                            for h in range(8):
                                S.op("pe", lambda e, h=h: e.transpose(out=T0[0:96, h * 128:(h + 1) * 128], in_=qmb_f[:, h * 96:(h + 1) * 96], identity=identb[:]), reads=["qmb", "identb"], writes=["T0"])
                            S.op("dve", lambda e, qmst=qmst, t=t: e.tensor_copy(out=qmst[0:96, :, t * 128:(t + 1) * 128], in_=T0[0:96, :].rearrange("p (k t) -> p k t", k=8)), reads=["T0"], writes=[qmk])
                            for h in range(8):
                                S.op("pe", lambda e, h=h: e.transpose(out=T1[0:96, h * 128:(h + 1) * 128], in_=km_f[:, h * 96:(h + 1) * 96], identity=identb[:]), reads=["km", "identb"], writes=["T1"])
                            S.op("dve", lambda e, kmst=kmst, t=t: e.tensor_copy(out=kmst[0:96, :, t * 128:(t + 1) * 128], in_=T1[0:96, :].rearrange("p (k t) -> p k t", k=8)), reads=["T1"], writes=[kmk])
                        csl = slice(c * 512, (c + 1) * 512)
                        S.dma("sp", lambda e, qkst=qkst, csl=csl: e.dma_start(out=QgT[:, :, csl], in_=qkst[:, 0:4, :]), reads=[qkk], writes=["QgT"])
                        S.dma("sp", lambda e, qkst=qkst, csl=csl: e.dma_start(out=KgT[:, :, csl], in_=qkst[:, 4:6, :]), reads=[qkk], writes=["KgT"])
                        S.dma("sp", lambda e, qmst=qmst, csl=csl: e.dma_start(out=QmT[:, :, csl], in_=qmst[0:96, :, :]), reads=[qmk], writes=["QmT"])
                        S.dma("sp", lambda e, kmst=kmst, csl=csl: e.dma_start(out=KmT[:, :, csl], in_=kmst[0:96, :, :]), reads=[kmk], writes=["KmT"])
                S.barrier()
                if stop_after == "P1":
                    for nm, t_ in (("QgT", QgT), ("KgT", KgT), ("QmT", QmT), ("KmT", KmT)):
                        d_ = dbgt(nm, t_.shape, BF16)
                        S.dma("sp", lambda e, d_=d_, t_=t_: e.dma_start(out=d_, in_=t_), reads=[nm], writes=["dbg"])
                    return False
                with ExitStack() as p2:
                    b = lambda name, shape, dt: sb(name, shape, dt, p2)
                    QTR = Ring(b, "QTb", [128, S_LEN], BF16, 2); KTR = Ring(b, "KTb", [128, S_LEN], BF16, 2)
                    PTR = Ring(b, "PT", [128, 512], BF16, 4)
                    OoR = Ring(b, "Oo", [128, 4, 64], F32, 2); rcR = Ring(b, "rc", [128, 4], F32, 2)
                    Sring = Ring(lambda n, s, d: FB[int(n[-1])], "F", None, None, 3)
                    Osets = [[(FB[3], "F3")], [(FB[4], "F4")]]

                    def make_cb(hcol):
                        def cbk(qc, oset, per_bank):
                            ob, okey = oset[0]
                            ov = ob[:, 0:260].rearrange("p (q d) -> p q d", q=4)
                            rc, rck = rcR.next(); Oo, Ook = OoR.next()
                            S.op("dve", lambda e: e.reciprocal(out=rc[:, :], in_=ov[:, :, 64]), reads=[okey], writes=[rck])
                            S.op("dve", lambda e: e.tensor_tensor(out=Oo[:, :, :], in0=ov[:, :, 0:64], in1=rc[:, :].unsqueeze(2).to_broadcast([128, 4, 64]), op=ALU.mult), reads=[okey, rck], writes=[Ook])
                            S.dma("sp", lambda e: e.dma_start(out=Odr[qc * 512:(qc + 1) * 512, hcol:hcol + 64].rearrange("(q p) d -> p q d", p=128), in_=Oo[:, :, :]), reads=[Ook], writes=["Odr"])
                        return cbk

                    for pr_ in range(4):
                        g_ = pr_ // 2
                        QTb, qk_ = QTR.next(); KTb, kk_ = KTR.next()
                        S.dma("sp", lambda e, QTb=QTb, pr_=pr_: e.dma_start(out=QTb[:, :], in_=QgT[:, pr_, :]), reads=["QgT"], writes=[qk_])
                        S.dma("sp", lambda e, KTb=KTb, g_=g_: e.dma_start(out=KTb[:, :], in_=KgT[:, g_, :]), reads=["KgT"], writes=[kk_])
                        for hh in range(2):
                            h = pr_ * 2 + hh
                            r0 = hh * 64
                            attention(QTb[r0:r0 + 64, :], qk_, KTb[r0:r0 + 64, :], kk_, NT, lambda kt, g_=g_: Vg[:, kt, g_, :], "Vg", 64, 64 ** -0.5, None, make_cb(h * 64), Sring, PTR, Osets)
                    for h in range(8):
                        QTb, qk_ = QTR.next(); KTb, kk_ = KTR.next()
                        S.dma("sp", lambda e, QTb=QTb, h=h: e.dma_start(out=QTb[0:96, :], in_=QmT[:, h, :]), reads=["QmT"], writes=[qk_])
                        S.dma("sp", lambda e, KTb=KTb, h=h: e.dma_start(out=KTb[0:96, :], in_=KmT[:, h, :]), reads=["KmT"], writes=[kk_])
                        attention(QTb[0:96, :], qk_, KTb[0:96, :], kk_, NT, lambda kt, h=h: Vm[:, kt, h, :], "Vm", 64, 96 ** -0.5, None, make_cb(512 + h * 64), Sring, PTR, Osets)
                S.barrier()
            if stop_after == "P2":
                d_ = dbgt("Odr", [S_LEN, 1024], F32)
                S.dma("sp", lambda e: e.dma_start(out=d_, in_=Odr), reads=["Odr"], writes=["dbg"])
                return False
            with ExitStack() as p3:
                b = lambda name, shape, dt: sb(name, shape, dt, p3)
                Wo = b("Wo", [128, 8, 1024], BF16)
                load_weight(Wo[:, 0:4, :], "Wo", w_o[l, 0:512, :], 4, 1024, gc_og[:, l, :], "gcB")
                load_weight(Wo[:, 4:8, :], "Wo", w_o[l, 512:1024, :], 4, 1024, gc_om[:, l, :], "gcB")
                otR = Ring(b, "ot", [128, D], F32, 2); hnR = Ring(b, "hn3", [128, D], BF16, 2); XTR = Ring(b, "XT3", [128, 8, 128], BF16, 2)
                xtR = Ring(b, "xt3", [128, D], F32, 2); yR = Ring(b, "y3", [128, D], F32, 2)
                scr = b("scr3", [128, D], F32); rsx = Ring(b, "rs3", [128, 2], F32, 2)
                for j in range(NT):
                    ot, otk = otR.next(); hn, hk = hnR.next(); XT, XTk = XTR.next(); xt, xk = xtR.next(); y, yk = yR.next(); rs, rsk = rsx.next()
                    S.dma("sp", lambda e, ot=ot, j=j: e.dma_start(out=ot[:], in_=Odr[j * 128:(j + 1) * 128, :]), reads=["Odr"], writes=[otk])
                    S.dma("sp", lambda e, xt=xt, j=j: e.dma_start(out=xt[:], in_=x_src[j * 128:(j + 1) * 128, :]), reads=[xs_key], writes=[xk])
                    norm_transpose(ot, otk, hn, hk, XT, XTk, 0, rs, rsk, scr, "scr3", halves=2)
                    for nb in range(2):
                        for kc in range(8):
                            S.op("pe", lambda e, nb=nb, kc=kc, XT=XT: e.matmul(FB[nb][:, :], lhsT=XT[:, kc, :], rhs=Wo[:, kc, nb * 512:(nb + 1) * 512], start=(kc == 0), stop=(kc == 7)), reads=[XTk, "Wo"], writes=[FK[nb]])
                        S.op("dve", lambda e, nb=nb, y=y, xt=xt: e.tensor_tensor(out=y[:, nb * 512:(nb + 1) * 512], in0=FB[nb][:, :], in1=xt[:, nb * 512:(nb + 1) * 512], op=ALU.add), reads=[FK[nb], xk], writes=[yk])
                    S.dma("sp", lambda e, y=y, j=j: e.dma_start(out=xres[j * 128:(j + 1) * 128, :], in_=y[:]), reads=[yk], writes=["xres"])
            S.barrier()
            if stop_after == "P3":
                return False

            with ExitStack() as p4:
                b = lambda name, shape, dt: sb(name, shape, dt, p4)
                Wq = b("Wmq", [128, 8, 512], BF16); Wkv = b("Wmkv", [128, 8, 1024], BF16); Wmo = b("Wmo", [128, 4, 1024], BF16)
                load_weight(Wq, "Wmq", w_mem_q[l], 8, 512, gc_mem[:, l, :], "gcA")
                load_weight(Wkv, "Wmkv", w_mem_kv[l], 8, 1024, gc_memkv[:, l, :], "gcA")
                load_weight(Wmo, "Wmo", w_mem_o[l], 4, 1024)
                xtR = Ring(b, "xt4", [128, D], F32, 8); hnR = Ring(b, "hn4", [128, D], BF16, 2)
                scr = b("scr4", [128, D], F32); rsx = Ring(b, "rs4", [128, 2], F32, 2)
                XTm = b("XTm", [128, 8, 256], BF16)
                KTm = b("KTm", [128, 4, 256], BF16); Vme = b("Vme", [128, 2, 4, 129], BF16)
                S.op("pool", lambda e: e.memset(Vme[:, :, :, 128:129], 1.0), writes=["Vme"])
                for mt in range(2):
                    xt, xk = xtR.next(); hn, hk = hnR.next(); rs, rsk = rsx.next()
                    S.dma("sp", lambda e, xt=xt, mt=mt: e.dma_start(out=xt[:], in_=mem_in[mt * 128:(mt + 1) * 128, :]), writes=[xk])
                    norm_transpose(xt, xk, hn, hk, XTm, "XTm", mt * 128, rs, rsk, scr, "scr4")
                for h in range(4):
                    for kc in range(8):
                        S.op("pe", lambda e, h=h, kc=kc: e.matmul(FB[2][:, 0:256], lhsT=Wkv[:, kc, h * 128:(h + 1) * 128], rhs=XTm[:, kc, :], start=(kc == 0), stop=(kc == 7)), reads=["Wmkv", "XTm"], writes=["F2"])
                    S.op("dve", lambda e, h=h: e.tensor_copy(out=KTm[:, h, :], in_=FB[2][:, 0:256]), reads=["F2"], writes=["KTm"])
                for mt in range(2):
                    for kc in range(8):
                        S.op("pe", lambda e, mt=mt, kc=kc: e.matmul(FB[3][:, :], lhsT=XTm[:, kc, mt * 128:(mt + 1) * 128], rhs=Wkv[:, kc, 512:1024], start=(kc == 0), stop=(kc == 7)), reads=["Wmkv", "XTm"], writes=["F3"])
                    S.op("dve", lambda e, mt=mt: e.tensor_copy(out=Vme[:, mt, :, 0:128], in_=FB[3][:, :].rearrange("p (h d) -> p h d", h=4)), reads=["F3"], writes=["Vme"])
                XTR = Ring(b, "XT4", [128, 8, 512], BF16, 2)
                QTR = Ring(b, "QTm", [128, 512], BF16, 2); PTR = Ring(b, "PT4", [128, 512], BF16, 4)
                ObR = Ring(b, "Obf", [128, 4, 512], BF16, 2); OTR = Ring(b, "OT4", [128, 4, 128], BF16, 2)
                rcR = Ring(b, "rc4", [128, 4], F32, 2); yR = Ring(b, "y4", [128, D], F32, 2)
                Sring = Ring(lambda n, s_, d: FB[int(n[-1])], "F", None, None, 2)
                Osets = [[(FB[2], "F2"), (FB[3], "F3")]]
                for c in range(8):
                    XT, XTk = XTR.next()
                    xts = []
                    for t in range(4):
                        j = c * 4 + t
                        xt, xk = xtR.next(); hn, hk = hnR.next(); rs, rsk = rsx.next()
                        xts.append((xt, xk))
                        S.dma("sp", lambda e, xt=xt, j=j: e.dma_start(out=xt[:], in_=xres[j * 128:(j + 1) * 128, :]), reads=["xres"], writes=[xk])
                        norm_transpose(xt, xk, hn, hk, XT, XTk, t * 128, rs, rsk, scr, "scr4")
                    Obf, Obk = ObR.next()
                    for h in range(4):
                        QTm, QTk = QTR.next()
                        for kc in range(8):
                            S.op("pe", lambda e, h=h, kc=kc, XT=XT: e.matmul(FB[4][:, :], lhsT=Wq[:, kc, h * 128:(h + 1) * 128], rhs=XT[:, kc, :], start=(kc == 0), stop=(kc == 7)), reads=["Wmq", XTk], writes=["F4"])
                        S.op("dve", lambda e, QTm=QTm: e.tensor_copy(out=QTm[:, :], in_=FB[4][:, :]), reads=["F4"], writes=[QTk])

                        def cbk(qc, oset, per_bank, h=h, Obf=Obf, Obk=Obk):
                            rc, rck = rcR.next()
                            for qs in range(4):
                                ob, okey = oset[qs // per_bank]
                                o0 = (qs % per_bank) * 129
                                S.op("dve", lambda e, ob=ob, o0=o0, qs=qs: e.reciprocal(out=rc[:, qs:qs + 1], in_=ob[:, o0 + 128:o0 + 129]), reads=[okey], writes=[rck])
                                S.op("dve", lambda e, ob=ob, o0=o0, qs=qs: e.tensor_scalar(out=Obf[:, qs, h * 128:(h + 1) * 128], in0=ob[:, o0:o0 + 128], scalar1=rc[:, qs:qs + 1], scalar2=None, op0=ALU.mult), reads=[okey, rck], writes=[Obk])
                        attention(QTm[:, :], QTk, KTm[:, h, :], "KTm", 2, lambda kt, h=h: Vme[:, kt, h, :], "Vme", 128, 128 ** -0.5, None, cbk, Sring, PTR, Osets, nqc=1)
                    for qs in range(4):
                        j = c * 4 + qs
                        xt, xk = xts[qs]
                        OT, OTk = OTR.next(); y, yk = yR.next()
                        for h in range(4):
                            S.op("pe", lambda e, h=h, qs=qs, Obf=Obf: e.transpose(out=T1[:, h * 128:(h + 1) * 128], in_=Obf[:, qs, h * 128:(h + 1) * 128], identity=identb[:]), reads=[Obk, "identb"], writes=["T1"])
                        S.op("dve", lambda e, OT=OT: e.tensor_copy(out=OT[:, :, :], in_=T1[:, 0:512].rearrange("p (k t) -> p k t", k=4)), reads=["T1"], writes=[OTk])
                        for nb in range(2):
                            for h in range(4):
                                S.op("pe", lambda e, nb=nb, h=h, OT=OT: e.matmul(FB[4 + nb][:, :], lhsT=OT[:, h, :], rhs=Wmo[:, h, nb * 512:(nb + 1) * 512], start=(h == 0), stop=(h == 3)), reads=[OTk, "Wmo"], writes=[FK[4 + nb]])
                            S.op("dve", lambda e, nb=nb, y=y, xt=xt: e.tensor_tensor(out=y[:, nb * 512:(nb + 1) * 512], in0=FB[4 + nb][:, :], in1=xt[:, nb * 512:(nb + 1) * 512], op=ALU.add), reads=[FK[4 + nb], xk], writes=[yk])
                        S.dma("sp", lambda e, y=y, j=j: e.dma_start(out=xres[j * 128:(j + 1) * 128, :], in_=y[:]), reads=[yk], writes=["xres"])
            S.barrier()
            if stop_after == "P4":
                return False
            with ExitStack() as p5:
                bo = lambda name, shape, dt: sb(name, shape, dt, p5)
                idx_i = bo("idx_i", [128, 64], I32)
                with ExitStack() as p5a:
                    b = lambda name, shape, dt: sb(name, shape, dt, p5a)
                    gfb = b("gfb", [128, D], F32)
                    S.dma("sp", lambda e: e.dma_start(out=gfb[:], in_=ln_ffn[l, :].partition_broadcast(128)), writes=["gfb"])
                    Wr = b("Wr", [128, 8, 16], F32)
                    for kc in range(8):
                        S.dma("sp", lambda e, kc=kc: e.dma_start(out=Wr[:, kc, :], in_=w_router[l, kc * 128:(kc + 1) * 128, :]), writes=["Wr"])
                    aff = b("aff", [128, NT, 16], F32)
                    xtR = Ring(b, "xt5", [128, D], F32, 2); h32R = Ring(b, "h32", [128, D], F32, 2); hbR = Ring(b, "hb5", [128, D], BF16, 2)
                    scr = b("scr5", [128, D], F32); rsx = Ring(b, "rs5", [128, 2], F32, 2)
                    XT32R = Ring(b, "XT32", [128, 8, 128], F32, 2)
                    smR = Ring(b, "sm5", [128, 4], F32, 2); exR = Ring(b, "ex5", [128, 16], F32, 2)
                    for j in range(NT):
                        xt, xk = xtR.next(); h32, h32k = h32R.next(); hb, hbk = hbR.next(); rs, rsk = rsx.next(); XT32, X32k = XT32R.next()
                        sm, smk = smR.next(); ex, exk = exR.next()
                        S.dma("sp", lambda e, xt=xt, j=j: e.dma_start(out=xt[:], in_=xres[j * 128:(j + 1) * 128, :]), reads=["xres"], writes=[xk])
                        norm_transpose(xt, xk, h32, h32k, None, None, 0, rs, rsk, scr, "scr5", gain_b=gfb, gbkey="gfb")
                        S.op("pool", lambda e, hb=hb, h32=h32: e.tensor_copy(out=hb[:, :], in_=h32[:, :]), reads=[h32k], writes=[hbk])
                        S.dma("sp", lambda e, hb=hb, j=j: e.dma_start(out=hdr[j * 128:(j + 1) * 128, :], in_=hb[:]), reads=[hbk], writes=["hdr"])
                        for kc in range(8):
                            S.op("pe", lambda e, kc=kc, h32=h32: e.transpose(out=FB[kc // 4][:, (kc % 4) * 128:(kc % 4 + 1) * 128], in_=h32[:, kc * 128:(kc + 1) * 128], identity=identf[:]), reads=[h32k, "identf"], writes=[FK[kc // 4]])
                        for hf in range(2):
                            S.op("dve", lambda e, hf=hf, XT32=XT32: e.tensor_copy(out=XT32[:, hf * 4:(hf + 1) * 4, :], in_=FB[hf][:, :].rearrange("p (k t) -> p k t", k=4)), reads=[FK[hf]], writes=[X32k])
                        for kc in range(8):
                            S.op("pe", lambda e, kc=kc, XT32=XT32: e.matmul(FB[2][:, 0:16], lhsT=XT32[:, kc, :], rhs=Wr[:, kc, :], start=(kc == 0), stop=(kc == 7)), reads=[X32k, "Wr"], writes=["F2"])
                        S.op("dve", lambda e, sm=sm: e.tensor_reduce(out=sm[:, 0:1], in_=FB[2][:, 0:16], axis=AX.X, op=ALU.max, negate=True), reads=["F2"], writes=[smk])
                        S.op("act", lambda e, sm=sm, ex=ex: e.activation(out=ex[:, :], in_=FB[2][:, 0:16], func=AF.Exp, bias=sm[:, 0:1], scale=1.0, accum_out=sm[:, 1:2]), reads=["F2", smk], writes=[exk, smk])
                        S.op("dve", lambda e, sm=sm: e.reciprocal(out=sm[:, 2:3], in_=sm[:, 1:2]), reads=[smk], writes=[smk])
                        S.op("dve", lambda e, sm=sm, ex=ex, j=j: e.tensor_scalar(out=aff[:, j, :], in0=ex[:, :], scalar1=sm[:, 2:3], scalar2=None, op0=ALU.mult), reads=[exk, smk], writes=["aff"])
                        S.dma("sp", lambda e, j=j: e.dma_start(out=affdr[j * 128:(j + 1) * 128, :], in_=aff[:, j, :]), reads=["aff"], writes=["affdr"])
                    affT = b("affT", [16, S_LEN], F32); junk = b("junk", [16, S_LEN], BF16); maskT = b("maskT", [16, S_LEN], BF16)
                    for rnd in range(2):
                        for jj in range(16):
                            j = rnd * 16 + jj
                            S.op("pe", lambda e, j=j, jj=jj: e.transpose(out=FB[jj // 4][0:16, (jj % 4) * 128:(jj % 4 + 1) * 128], in_=aff[:, j, :], identity=identf[:]), reads=["aff", "identf"], writes=[FK[jj // 4]])
                        for q4 in range(4):
                            S.op("dve", lambda e, rnd=rnd, q4=q4: e.tensor_copy(out=affT[:, (rnd * 4 + q4) * 512:(rnd * 4 + q4 + 1) * 512], in_=FB[q4][0:16, :]), reads=[FK[q4]], writes=["affT"])
                    bs = b("bs", [16, 8], F32)
                    S.op("dve", lambda e: e.memset(bs[:, 0:1], 0.0), writes=["bs"])
                    S.op("dve", lambda e: e.memset(bs[:, 1:2], 2.0), writes=["bs"])
                    for it in range(33):
                        S.op("dve", lambda e: e.tensor_tensor(out=bs[:, 2:3], in0=bs[:, 0:1], in1=bs[:, 1:2], op=ALU.add), reads=["bs"], writes=["bs"])
                        S.op("dve", lambda e: e.tensor_scalar(out=bs[:, 2:3], in0=bs[:, 2:3], scalar1=0.5, scalar2=None, op0=ALU.mult), reads=["bs"], writes=["bs"])
                        S.op("dve", lambda e: e.tensor_scalar(out=junk[:, :], in0=affT[:, :], scalar1=bs[:, 2:3], scalar2=0.0, op0=ALU.is_ge, op1=ALU.add, accum_out=bs[:, 3:4]), reads=["affT", "bs"], writes=["junk", "bs"])
                        S.op("dve", lambda e: e.tensor_scalar(out=bs[:, 4:5], in0=bs[:, 3:4], scalar1=511.5, scalar2=None, op0=ALU.is_ge), reads=["bs"], writes=["bs"])
                        S.op("dve", lambda e: e.tensor_tensor(out=bs[:, 5:6], in0=bs[:, 2:3], in1=bs[:, 0:1], op=ALU.subtract), reads=["bs"], writes=["bs"])
                        S.op("dve", lambda e: e.scalar_tensor_tensor(out=bs[:, 0:1], in0=bs[:, 5:6], scalar=bs[:, 4:5], in1=bs[:, 0:1], op0=ALU.mult, op1=ALU.add), reads=["bs"], writes=["bs"])
                        S.op("dve", lambda e: e.tensor_tensor(out=bs[:, 5:6], in0=bs[:, 1:2], in1=bs[:, 2:3], op=ALU.subtract), reads=["bs"], writes=["bs"])
                        S.op("dve", lambda e: e.scalar_tensor_tensor(out=bs[:, 1:2], in0=bs[:, 5:6], scalar=bs[:, 4:5], in1=bs[:, 2:3], op0=ALU.mult, op1=ALU.add), reads=["bs"], writes=["bs"])
                    S.op("dve", lambda e: e.tensor_scalar(out=maskT[:, :], in0=affT[:, :], scalar1=bs[:, 0:1], scalar2=None, op0=ALU.is_ge), reads=["affT", "bs"], writes=["maskT"])
                    mask_tok = b("mask_tok", [128, NT, 16], BF16)
                    for j in range(NT):
                        S.op("pe", lambda e, j=j: e.transpose(out=T0[:, j * 16:(j + 1) * 16], in_=maskT[:, j * 128:(j + 1) * 128], identity=identb[0:16, 0:16]), reads=["maskT", "identb"], writes=["T0"])
                    S.op("dve", lambda e: e.tensor_copy(out=mask_tok[:, :, :], in_=T0[:, 0:512].rearrange("p (j e) -> p j e", j=NT)), reads=["T0"], writes=["mask_tok"])
                    onesb = b("onesb", [128, 128], BF16); ustr = b("ustr", [128, 128], BF16)
                    S.op("pool", lambda e: e.memset(onesb[:], 1.0), writes=["onesb"])
                    S.op("pool", lambda e: e.memset(ustr[:], 1.0), writes=["ustr"])
                    S.op("pool", lambda e: e.affine_select(out=ustr[:], in_=ustr[:], pattern=[[1, 128]], compare_op=ALU.is_gt, fill=0.0, base=0, channel_multiplier=-1), reads=["ustr"], writes=["ustr"])
                    firstmm = True
                    for j in range(NT):
                        for i in range(j + 1):
                            lhs = ustr if i == j else onesb
                            S.op("pe", lambda e, j=j, i=i, lhs=lhs, firstmm=firstmm: e.matmul(FB[0][:, j * 16:(j + 1) * 16], lhsT=lhs[:, :], rhs=mask_tok[:, i, :], start=firstmm, stop=(i == j), skip_group_check=True), reads=["mask_tok", "onesb", "ustr"], writes=["F0"])
                            firstmm = False
                    posm = b("posm", [128, NT, 16], F32)
                    S.op("dve", lambda e: e.scalar_tensor_tensor(out=posm[:, :, :], in0=FB[0][:, :].rearrange("p (j e) -> p j e", j=NT), scalar=1.0, in1=mask_tok[:, :, :], op0=ALU.add, op1=ALU.mult), reads=["F0", "mask_tok"], writes=["posm"])
                    S.op("dve", lambda e: e.tensor_scalar(out=posm[:, :, :], in0=posm[:, :, :], scalar1=-1.0, scalar2=None, op0=ALU.add), reads=["posm"], writes=["posm"])
                    iota_s = b("iota_s", [128, 512], F32)
                    S.op("pool", lambda e: e.iota(iota_s[:, :], pattern=[[1, 512]], base=0, channel_multiplier=0, allow_small_or_imprecise_dtypes=True), writes=["iota_s"])
                    tv32 = b("tv32", [128, NT, 2], F32); tokval = b("tokval", [128, NT, 2], BF16)
                    S.op("pool", lambda e: e.iota(tv32[:, :, :], pattern=[[0, NT], [0, 2]], base=0, channel_multiplier=1, allow_small_or_imprecise_dtypes=True), writes=["tv32"])
                    S.op("pool", lambda e: e.iota(tv32[:, :, 1], pattern=[[1, NT]], base=0, channel_multiplier=0, allow_small_or_imprecise_dtypes=True), reads=["tv32"], writes=["tv32"])
                    S.op("pool", lambda e: e.tensor_copy(out=tokval[:, :, :], in_=tv32[:, :, :]), reads=["tv32"], writes=["tokval"])
                    OhR = Ring(b, "Oh", [128, 16, 512], BF16, 2)
                    firstmm = True
                    for j in range(NT):
                        Oh, Ohk = OhR.next()
                        eng = "dve"
                        S.op(eng, lambda e, Oh=Oh, j=j: e.tensor_tensor(out=Oh[:, :, :], in0=iota_s[:, :].unsqueeze(1).to_broadcast([128, 16, 512]), in1=posm[:, j, :].unsqueeze(2).to_broadcast([128, 16, 512]), op=ALU.is_equal), reads=["iota_s", "posm"], writes=[Ohk])
                        for ee in range(16):
                            for c in range(4):
                                col = (ee * 4 + c) * 2
                                S.op("pe", lambda e, Oh=Oh, ee=ee, c=c, col=col, j=j, firstmm=firstmm: e.matmul(FB[5][:, col:col + 2], lhsT=Oh[:, ee, c * 128:(c + 1) * 128], rhs=tokval[:, j, :], start=firstmm, stop=(j == NT - 1), skip_group_check=True), reads=[Ohk, "tokval"], writes=["F5"])
                                firstmm = False
                    idxf = b("idxf", [128, 64], F32)
                    f5s = b("f5s", [128, 128], F32)
                    S.op("dve", lambda e: e.tensor_copy(out=f5s[:, :], in_=FB[5][:, 0:128]), reads=["F5"], writes=["f5s"])
                    f5v = f5s[:, :].rearrange("p (s two) -> p s two", two=2)
                    S.op("dve", lambda e: e.scalar_tensor_tensor(out=idxf[:, :], in0=f5v[:, :, 1], scalar=128.0, in1=f5v[:, :, 0], op0=ALU.mult, op1=ALU.add), reads=["f5s"], writes=["idxf"])
                    S.op("dve", lambda e: e.tensor_copy(out=idx_i[:, :], in_=idxf[:, :]), reads=["idxf"], writes=["idx_i"])
                    if stop_after == "P5a":
                        d1 = dbgt("idx", [128, 64], I32); d2 = dbgt("aff", [S_LEN, 16], F32); d3 = dbgt("bs", [16, 8], F32)
                        S.dma("sp", lambda e: e.dma_start(out=d1, in_=idx_i[:, :]), reads=["idx_i"], writes=["dbg"])
                        S.dma("sp", lambda e: e.dma_start(out=d2, in_=affdr), reads=["affdr"], writes=["dbg"])
                        S.dma("sp", lambda e: e.dma_start(out=d3, in_=bs[:, :]), reads=["bs"], writes=["dbg"])
                S.barrier()
                if stop_after == "P5a":
                    return False
                with ExitStack() as p5b:
                    b = lambda name, shape, dt: sb(name, shape, dt, p5b)
                    WgR = Ring(b, "Wg", [128, 8, 512], BF16, 2); WuR = Ring(b, "Wu", [128, 8, 512], BF16, 2); WdR = Ring(b, "Wd", [128, 4, 1024], BF16, 2)
                    XgTR = Ring(b, "XgT", [128, 8, 512], BF16, 2); xgR = Ring(b, "xg", [128, D], BF16, 4); gaR = Ring(b, "ga", [128, 16], F32, 8)
                    GTR = Ring(b, "GT", [128, 4, 512], BF16, 2); sgR = Ring(b, "sg", [128, 512], F32, 2); yR = Ring(b, "y5", [128, D], F32, 2)
                    for ee in range(16):
                        Wg, Wgk = WgR.next(); Wu, Wuk = WuR.next(); Wd, Wdk = WdR.next()
                        load_weight(Wg, Wgk, w_gate[l, ee], 8, 512)
                        load_weight(Wu, Wuk, w_up[l, ee], 8, 512)
                        load_weight(Wd, Wdk, w_down[l, ee], 4, 1024)
                        XgT, XgTk = XgTR.next(); GT, GTk = GTR.next()
                        gas = []
                        for c in range(4):
                            xg, xgk = xgR.next(); ga, gak = gaR.next()
                            gas.append((ga, gak))
                            col = ee * 4 + c
                            S.dma("pool", lambda e, xg=xg, col=col: e.indirect_dma_start(out=xg[:, :], out_offset=None, in_=hdr[:, :], in_offset=bass.IndirectOffsetOnAxis(ap=idx_i[:, col:col + 1], axis=0)), reads=["idx_i", "hdr"], writes=[xgk])
                            S.dma("pool", lambda e, ga=ga, col=col: e.indirect_dma_start(out=ga[:, :], out_offset=None, in_=affdr[:, :], in_offset=bass.IndirectOffsetOnAxis(ap=idx_i[:, col:col + 1], axis=0)), reads=["idx_i", "affdr"], writes=[gak])
                            for kc in range(8):
                                S.op("pe", lambda e, kc=kc, xg=xg: e.transpose(out=T0[:, kc * 128:(kc + 1) * 128], in_=xg[:, kc * 128:(kc + 1) * 128], identity=identb[:]), reads=[xgk, "identb"], writes=["T0"])
                            S.op("dve", lambda e, XgT=XgT, c=c: e.tensor_copy(out=XgT[:, :, c * 128:(c + 1) * 128], in_=T0[:, :].rearrange("p (k t) -> p k t", k=8)), reads=["T0"], writes=[XgTk])
                        for fc in range(4):
                            pa = 2 * (fc % 2); pu = pa + 1
                            for kc in range(8):
                                S.op("pe", lambda e, fc=fc, kc=kc, pa=pa, Wg=Wg, XgT=XgT: e.matmul(FB[pa][:, :], lhsT=Wg[:, kc, fc * 128:(fc + 1) * 128], rhs=XgT[:, kc, :], start=(kc == 0), stop=(kc == 7)), reads=[Wgk, XgTk], writes=[FK[pa]])
                            for kc in range(8):
                                S.op("pe", lambda e, fc=fc, kc=kc, pu=pu, Wu=Wu, XgT=XgT: e.matmul(FB[pu][:, :], lhsT=Wu[:, kc, fc * 128:(fc + 1) * 128], rhs=XgT[:, kc, :], start=(kc == 0), stop=(kc == 7)), reads=[Wuk, XgTk], writes=[FK[pu]])
                            sg, sgk = sgR.next()
                            S.op("act", lambda e, sg=sg, pa=pa: e.activation(out=sg[:, :], in_=FB[pa][:, :], func=AF.Silu), reads=[FK[pa]], writes=[sgk])
                            S.op("dve", lambda e, sg=sg, pu=pu, GT=GT, fc=fc: e.tensor_tensor(out=GT[:, fc, :], in0=sg[:, :], in1=FB[pu][:, :], op=ALU.mult), reads=[sgk, FK[pu]], writes=[GTk])
                        for st_ in range(4):
                            y, yk = yR.next(); ga, gak = gas[st_]
                            col = ee * 4 + st_
                            for half in range(2):
                                for fc in range(4):
                                    S.op("pe", lambda e, half=half, fc=fc, GT=GT, Wd=Wd, st_=st_: e.matmul(FB[4 + half][:, :], lhsT=GT[:, fc, st_ * 128:(st_ + 1) * 128], rhs=Wd[:, fc, half * 512:(half + 1) * 512], start=(fc == 0), stop=(fc == 3)), reads=[GTk, Wdk], writes=[FK[4 + half]])
                                S.op("act", lambda e, half=half, y=y, ga=ga, ee=ee: e.activation(out=y[:, half * 512:(half + 1) * 512], in_=FB[4 + half][:, :], func=AF.Copy, scale=ga[:, ee:ee + 1]), reads=[FK[4 + half], gak], writes=[yk])
                            S.dma("pool", lambda e, y=y, col=col: e.indirect_dma_start(out=xres[:, :], out_offset=bass.IndirectOffsetOnAxis(ap=idx_i[:, col:col + 1], axis=0), in_=y[:, :], in_offset=None, compute_op=ALU.add), reads=[yk, "idx_i"], writes=["xres"])
            S.barrier()
            return True

        x_src, xs_key = x_in, "x_in"
        final_norm = (stop_after is None)
        for l in range(nlayers):
            cont = layer(l, x_src, xs_key)
            x_src, xs_key = xres, "xres"
            if not cont:
                final_norm = False
                break
        with ExitStack() as pf:
            b = lambda name, shape, dt: sb(name, shape, dt, pf)
            xtR = Ring(b, "xtf", [128, D], F32, 2); yR = Ring(b, "yf", [128, D], F32, 2)
            scr = b("scrf", [128, D], F32); rsx = Ring(b, "rsf", [128, 2], F32, 2)
            gfin = b("gfin", [128, D], F32)
            S.dma("sp", lambda e: e.dma_start(out=gfin[:], in_=ln_final.partition_broadcast(128)), writes=["gfin"])
            for j in range(NT):
                xt, xk = xtR.next(); y, yk = yR.next(); rs, rsk = rsx.next()
                S.dma("sp", lambda e, xt=xt, j=j: e.dma_start(out=xt[:], in_=xres[j * 128:(j + 1) * 128, :]), reads=["xres"], writes=[xk])
                if final_norm:
                    norm_transpose(xt, xk, y, yk, None, None, 0, rs, rsk, scr, "scrf", gain_b=gfin, gbkey="gfin")
                    S.dma("sp", lambda e, y=y, j=j: e.dma_start(out=out[j * 128:(j + 1) * 128, :], in_=y[:]), reads=[yk], writes=["out"])
                else:
                    S.dma("sp", lambda e, xt=xt, j=j: e.dma_start(out=out[j * 128:(j + 1) * 128, :], in_=xt[:]), reads=[xk], writes=["out"])
        S.barrier(engines=["sp"])
        print("ninstr", S.ninstr, "nsem", S.nsem, {k: v for k, v in S.cnt.items()})
    return nc, dbg_out


def rope_tables():
    def ang(rot_dim):
        rows = S_LEN // 64
        row = np.repeat(np.arange(rows, dtype=np.float32), 64)
        col = np.tile(np.arange(64, dtype=np.float32), rows)
        axis_dim = rot_dim // 2
        inv = (10000.0 ** (-np.arange(0, axis_dim, 2, dtype=np.float32) / axis_dim)).astype(np.float32)
        a = np.concatenate([row[:, None] * inv[None, :], col[:, None] * inv[None, :]], axis=-1).astype(np.float32)
        return np.concatenate([np.cos(a), np.sin(a)], axis=-1).astype(np.float32)
    return ang(64), ang(32)


def make_in_maps(inputs, ncores=8):
    rg, rm = rope_tables()
    maps = []
    for c in range(ncores):
        m = {k: np.ascontiguousarray(np.asarray(v, dtype=np.float32)) for k, v in inputs.items() if k not in ("x", "mem")}
        m["x"] = np.ascontiguousarray(np.asarray(inputs["x"][c], dtype=np.float32))
        m["mem"] = np.ascontiguousarray(np.asarray(inputs["mem"][c], dtype=np.float32))
        m["ropeg"] = rg
        m["ropem"] = rm
        maps.append(m)
    return maps


def kernel(**inputs):
    nc, _ = build_nc()
    maps = make_in_maps(inputs, 8)
    res = run_bass_kernel_spmd(nc, maps, core_ids=list(range(8)))
    return np.stack([np.asarray(r["out"], dtype=np.float32) for r in res.results], axis=0)
```

```python
from contextlib import ExitStack
import numpy as np
import ml_dtypes
import concourse.bass as bass
import concourse.mybir as mybir
from concourse.bass_utils import run_bass_kernel_spmd

F32 = mybir.dt.float32
BF16 = mybir.dt.bfloat16
I32 = mybir.dt.int32
ALU = mybir.AluOpType
AF = mybir.ActivationFunctionType
AX = mybir.AxisListType

S_LEN = 4096
D = 1024
NT = 32
DEPTH = 4
EPS = 1e-6
IN_W = 1184
EPOCH = 30000
import os
ATT_LA = int(os.environ.get('ATT_LA', '3'))


class Sched:
    def __init__(self, nc, es, dma_slots=8):
        self.nc = nc
        self.es = es
        self.engs = {"pe": nc.tensor, "act": nc.scalar, "dve": nc.vector, "pool": nc.gpsimd, "sp": nc.sync}
        self.cnt = {k: 0 for k in self.engs}
        self.esems = {k: [] for k in self.engs}
        self.waited = {k: {} for k in self.engs}
        self.lastw = {}
        self.readers = {}
        self.dma_pool = {}
        self.dma_slots = dma_slots
        self.nsem = 0
        self.ninstr = 0

    def _newsem(self, name):
        self.nsem += 1
        return self.es.enter_context(self.nc.semaphore(name))

    def _eng_event(self, eng):
        c = self.cnt[eng]
        ep, loc = divmod(c, EPOCH)
        while len(self.esems[eng]) <= ep:
            self.esems[eng].append(self._newsem(f"e_{eng}_{len(self.esems[eng])}"))
        self.cnt[eng] = c + 1
        return (self.esems[eng][ep], loc + 1, eng)

    def _dma_event(self, q):
        st = self.dma_pool.setdefault(q, {"sems": [], "uses": [], "i": 0})
        if len(st["sems"]) < self.dma_slots:
            st["sems"].append(self._newsem(f"d_{q}_{len(st['sems'])}"))
            st["uses"].append(0)
        i = st["i"] % self.dma_slots
        st["i"] += 1
        sem = st["sems"][i]
        if st["uses"][i] > 0:
            self._wait(q, (sem, 16 * st["uses"][i], None))
        st["uses"][i] += 1
        return (sem, 16 * st["uses"][i], None)

    def _wait(self, eng, ev):
        sem, val, src = ev
        if src == eng and eng == "pe":
            return
        w = self.waited[eng]
        k = id(sem)
        if w.get(k, 0) >= val:
            return
        w[k] = val
        self.engs[eng].wait_ge(sem, val)
        self.ninstr += 1

    def _deps(self, eng, reads, writes):
        for k in reads:
            ev = self.lastw.get(k)
            if ev is not None:
                self._wait(eng, ev)
        for k in writes:
            ev = self.lastw.get(k)
            if ev is not None:
                self._wait(eng, ev)
            for ev in self.readers.get(k, {}).values():
                self._wait(eng, ev)

    def _record(self, ev, reads, writes):
        for k in writes:
            self.lastw[k] = ev
            self.readers[k] = {}
        for k in reads:
            if k in writes:
                continue
            self.readers.setdefault(k, {})[id(ev[0])] = ev

    def op(self, eng, fn, reads=(), writes=()):
        self._deps(eng, reads, writes)
        ev = self._eng_event(eng)
        fn(self.engs[eng]).then_inc(ev[0], 1)
        self._record(ev, reads, writes)
        self.ninstr += 1
        return ev

    def dma(self, q, fn, reads=(), writes=()):
        self._deps(q, reads, writes)
        ev = self._dma_event(q)
        fn(self.engs[q]).then_inc(ev[0], 16)
        self._record(ev, reads, writes)
        self.ninstr += 1
        return ev

    def all_events(self):
        evs = []
        for eng in self.engs:
            c = self.cnt[eng]
            if c > 0:
                ep, loc = divmod(c - 1, EPOCH)
                evs.append((self.esems[eng][ep], loc + 1, eng))
        for q, st in self.dma_pool.items():
            for sem, u in zip(st["sems"], st["uses"]):
                if u:
                    evs.append((sem, 16 * u, None))
        return evs

    def barrier(self, engines=None):
        evs = self.all_events()
        for eng in (engines or list(self.engs)):
            for ev in evs:
                if ev[2] == eng:
                    continue
                self._wait(eng, ev)
        if engines is None:
            self.lastw = {}
            self.readers = {}


class Ring:
    def __init__(self, alloc, name, shape, dt, n):
        self.items = [(alloc(f"{name}{i}", shape, dt), f"{name}{i}") for i in range(n)]
        self.i = 0

    def next(self):
        it = self.items[self.i % len(self.items)]
        self.i += 1
        return it


def build_nc(nlayers=DEPTH, dbg=None, stop_after=None):
    dbg = dbg or {}
    nc = bass.Bass("TRN2", target_bir_lowering=False)
    L = DEPTH

    def din(name, shape, dt=F32):
        return nc.dram_tensor(name, list(shape), dt, kind="ExternalInput").ap()

    def dscr(name, shape, dt):
        return nc.dram_tensor(name, list(shape), dt, kind="Internal").ap()

    x_in = din("x", [S_LEN, D])
    mem_in = din("mem", [256, D])
    ln_mix = din("ln_mix", [L, D]); w_in = din("w_in", [L, D, IN_W])
    gqa_q_norm = din("gqa_q_norm", [L, 64]); gqa_k_norm = din("gqa_k_norm", [L, 64])
    mla_q_norm = din("mla_q_norm", [L, 256]); mla_kv_norm = din("mla_kv_norm", [L, 128])
    w_q_b = din("w_q_b", [L, 256, 768]); w_kv_b = din("w_kv_b", [L, 128, 1024])
    out_norm_gqa = din("out_norm_gqa", [L, 512]); out_norm_mla = din("out_norm_mla", [L, 512])
    w_o = din("w_o", [L, 1024, 1024])
    ln_mem = din("ln_mem", [L, D]); ln_mem_kv = din("ln_mem_kv", [L, D])
    w_mem_q = din("w_mem_q", [L, D, 512]); w_mem_kv = din("w_mem_kv", [L, D, 1024]); w_mem_o = din("w_mem_o", [L, 512, D])
    ln_ffn = din("ln_ffn", [L, D]); w_router = din("w_router", [L, D, 16])
    w_gate = din("w_gate", [L, 16, D, 512]); w_up = din("w_up", [L, 16, D, 512]); w_down = din("w_down", [L, 16, 512, D])
    ln_final = din("ln_final", [D])
    ropeg = din("ropeg", [S_LEN, 64])
    ropem = din("ropem", [S_LEN, 32])
    out = nc.dram_tensor("out", [S_LEN, D], F32, kind="ExternalOutput").ap()

    xres = dscr("xres", [S_LEN, D], F32)
    QgT = dscr("QgT", [128, 4, S_LEN], BF16)
    KgT = dscr("KgT", [128, 2, S_LEN], BF16)
    QmT = dscr("QmT", [96, 8, S_LEN], BF16)
    KmT = dscr("KmT", [96, 8, S_LEN], BF16)
    Odr = dscr("Odr", [S_LEN, 1024], F32)
    hdr = dscr("hdr", [S_LEN, D], BF16)
    affdr = dscr("affdr", [S_LEN, 16], F32)

    dbg_out = {}

    def dbgt(name, shape, dt=F32):
        t = nc.dram_tensor("dbg_" + name, list(shape), dt, kind="ExternalOutput").ap()
        dbg_out[name] = t
        return t

    with ExitStack() as es:
        S = Sched(nc, es)

        uid = [0]

        def sb(name, shape, dt, stack=es):
            uid[0] += 1
            return stack.enter_context(nc.sbuf_tensor(f"{name}_{uid[0]}", list(shape), dt))

        def ps(name, shape, dt, stack=es):
            return stack.enter_context(nc.psum_tensor(name, list(shape), dt))

        T0 = ps("T0", [128, 1024], BF16); T1 = ps("T1", [128, 1024], BF16)
        FB = [ps(f"F{i}", [128, 512], F32) for i in range(6)]
        FK = [f"F{i}" for i in range(6)]

        identb = sb("identb", [128, 128], BF16)
        identf = sb("identf", [128, 128], F32)
        for t_, k_ in ((identb, "identb"), (identf, "identf")):
            S.op("pool", lambda e, t_=t_: e.memset(t_[:], 0.0), writes=[k_])
            S.op("pool", lambda e, t_=t_: e.affine_select(out=t_[:], in_=t_[:], pattern=[[-1, 128]], compare_op=ALU.not_equal, fill=1.0, base=0, channel_multiplier=1), reads=[k_], writes=[k_])
        neghalf = sb("neghalf", [128, 16], F32)
        S.op("pool", lambda e: e.memset(neghalf[:], -0.5), writes=["neghalf"])
        ropeg_sb = sb("ropeg_sb", [128, NT, 64], F32)
        ropem_sb = sb("ropem_sb", [128, NT, 32], F32)
        S.dma("sp", lambda e: e.dma_start(out=ropeg_sb[:], in_=ropeg.rearrange("(j p) c -> p j c", p=128)), writes=["ropeg"])
        S.dma("sp", lambda e: e.dma_start(out=ropem_sb[:], in_=ropem.rearrange("(j p) c -> p j c", p=128)), writes=["ropem"])
        growA = sb("growA", [96, 128], F32); growB = sb("growB", [44, 128], F32)
        gcA = sb("gcA", [128, 96], F32); gcB = sb("gcB", [128, 44], F32)
        for r0, src in ((0, ln_mix), (32, ln_mem), (64, ln_mem_kv)):
            S.dma("sp", lambda e, r0=r0, src=src: e.dma_start(out=growA[r0:r0 + 32, :], in_=src.rearrange("l (kc p) -> (l kc) p", p=128)), writes=["growA"])
        for r0, n_, src in ((0, 8, mla_q_norm), (8, 4, mla_kv_norm), (12, 16, out_norm_gqa), (28, 16, out_norm_mla)):
            S.dma("sp", lambda e, r0=r0, n_=n_, src=src: e.dma_start(out=growB[r0:r0 + n_, :], in_=src.rearrange("l (kc p) -> (l kc) p", p=128)), writes=["growB"])
        S.op("pe", lambda e: e.transpose(out=FB[0][:, 0:96], in_=growA[:, :], identity=identf[0:96, 0:96]), reads=["growA", "identf"], writes=["F0"])
        S.op("pe", lambda e: e.transpose(out=FB[1][:, 0:44], in_=growB[:, :], identity=identf[0:44, 0:44]), reads=["growB", "identf"], writes=["F1"])
        S.op("dve", lambda e: e.tensor_copy(out=gcA[:, :], in_=FB[0][:, 0:96]), reads=["F0"], writes=["gcA"])
        S.op("dve", lambda e: e.tensor_copy(out=gcB[:, :], in_=FB[1][:, 0:44]), reads=["F1"], writes=["gcB"])
        gcAv = gcA[:, :].rearrange("p (g l k) -> p g l k", g=3, l=L)
        gc_mix, gc_mem, gc_memkv = gcAv[:, 0], gcAv[:, 1], gcAv[:, 2]
        gc_q = gcB[:, 0:8].rearrange("p (l k) -> p l k", l=L); gc_kv = gcB[:, 8:12].rearrange("p (l k) -> p l k", l=L)
        gc_og = gcB[:, 12:28].rearrange("p (l k) -> p l k", l=L); gc_om = gcB[:, 28:44].rearrange("p (l k) -> p l k", l=L)
        invn = sb("invn", [128, 12], F32)
        S.op("pool", lambda e: e.memset(invn[:, 0:10], 1.0 / 64), writes=["invn"])
        S.op("pool", lambda e: e.memset(invn[:, 10:11], 1.0 / 256), writes=["invn"])
        S.op("pool", lambda e: e.memset(invn[:, 11:12], 1.0 / 128), writes=["invn"])

        def rstd_from_ss(ss_ap, n, out_ap, key_in, key_out, ncols=1):
            S.op("dve", lambda e: e.tensor_scalar(out=out_ap, in0=ss_ap, scalar1=1.0 / n, scalar2=EPS, op0=ALU.mult, op1=ALU.add), reads=[key_in], writes=[key_out])
            S.op("pool", lambda e: e.tensor_tensor(out=out_ap, in0=out_ap, in1=neghalf[:, 0:ncols], op=ALU.pow), reads=[key_out, "neghalf"], writes=[key_out])

        wstage = Ring(sb, "wst", [128, IN_W], F32, 3)

        cast_rr = [0]

        def load_weight(dst, dkey, src2d, KC, N, gain=None, gkey=None, q="sp", engs=("pool",)):
            for kc in range(KC):
                n0 = 0
                while n0 < N:
                    nn = min(IN_W, N - n0)
                    st, sk = wstage.next()
                    S.dma(q, lambda e, st=st, kc=kc, n0=n0, nn=nn: e.dma_start(out=st[:, 0:nn], in_=src2d[kc * 128:(kc + 1) * 128, n0:n0 + nn]), writes=[sk])
                    eng = engs[cast_rr[0] % len(engs)]
                    cast_rr[0] += 1
                    rd = [sk] + ([gkey] if gain is not None else [])
                    if eng == "act":
                        if gain is not None:
                            S.op("act", lambda e, st=st, kc=kc, n0=n0, nn=nn: e.activation(out=dst[:, kc, n0:n0 + nn], in_=st[:, 0:nn], func=AF.Copy, scale=gain[:, kc:kc + 1]), reads=rd, writes=[dkey])
                        else:
                            S.op("act", lambda e, st=st, kc=kc, n0=n0, nn=nn: e.activation(out=dst[:, kc, n0:n0 + nn], in_=st[:, 0:nn], func=AF.Copy), reads=rd, writes=[dkey])
                    elif gain is not None:
                        S.op(eng, lambda e, st=st, kc=kc, n0=n0, nn=nn: e.tensor_scalar(out=dst[:, kc, n0:n0 + nn], in0=st[:, 0:nn], scalar1=gain[:, kc:kc + 1], scalar2=None, op0=ALU.mult), reads=rd, writes=[dkey])
                    else:
                        S.op(eng, lambda e, st=st, kc=kc, n0=n0, nn=nn: e.tensor_copy(out=dst[:, kc, n0:n0 + nn], in_=st[:, 0:nn]), reads=rd, writes=[dkey])
                    n0 += nn

        def norm_transpose(xt, xkey, hn, hkey, XTdst, XTkey, tcol, rstd, rkey, scr, scrkey, gain_b=None, gbkey=None, halves=1):
            w = D // halves
            for hf in range(halves):
                S.op("dve", lambda e, hf=hf: e.scalar_tensor_tensor(out=scr[:, hf * w:(hf + 1) * w], in0=xt[:, hf * w:(hf + 1) * w], scalar=1.0, in1=xt[:, hf * w:(hf + 1) * w], op0=ALU.mult, op1=ALU.mult, accum_out=rstd[:, hf:hf + 1]),
                     reads=[xkey], writes=[scrkey, rkey])
            rstd_from_ss(rstd[:, 0:halves], w, rstd[:, 0:halves], rkey, rkey, ncols=halves)
            for hf in range(halves):
                if gain_b is None:
                    S.op("dve", lambda e, hf=hf: e.tensor_scalar(out=hn[:, hf * w:(hf + 1) * w], in0=xt[:, hf * w:(hf + 1) * w], scalar1=rstd[:, hf:hf + 1], scalar2=None, op0=ALU.mult), reads=[xkey, rkey], writes=[hkey])
                else:
                    S.op("dve", lambda e, hf=hf: e.scalar_tensor_tensor(out=hn[:, hf * w:(hf + 1) * w], in0=xt[:, hf * w:(hf + 1) * w], scalar=rstd[:, hf:hf + 1], in1=gain_b[:, hf * w:(hf + 1) * w], op0=ALU.mult, op1=ALU.mult), reads=[xkey, rkey, gbkey], writes=[hkey])
            if XTdst is None:
                return
            for kc in range(8):
                S.op("pe", lambda e, kc=kc: e.transpose(out=T0[:, kc * 128:(kc + 1) * 128], in_=hn[:, kc * 128:(kc + 1) * 128], identity=identb[:]), reads=[hkey, "identb"], writes=["T0"])
            S.op("dve", lambda e: e.tensor_copy(out=XTdst[:, :, tcol:tcol + 128], in_=T0[:, :].rearrange("p (k t) -> p k t", k=8)), reads=["T0"], writes=[XTkey])

        def attn_steps(QT, qkey, KT, kkey, nk_tiles, Vfn, vkey, dv, scale, bias, out_cb, Sring, PTring, Oring_sets, nqc=8):
            per_bank = min(512 // (dv + 1), 4)
            steps = []
            for qc in range(nqc):
                oset = Oring_sets[qc % len(Oring_sets)]
                for kt in range(nk_tiles):
                    st = {}

                    def fS(st=st, kt=kt, qc=qc):
                        sp_, skey = Sring.next()
                        st["s"] = (sp_, skey)
                        S.op("pe", lambda e: e.matmul(sp_[:, :], lhsT=KT[:, kt * 128:(kt + 1) * 128], rhs=QT[:, qc * 512:(qc + 1) * 512], start=True, stop=True), reads=[qkey, kkey], writes=[skey])

                    def fE(st=st):
                        sp_, skey = st["s"]
                        pt_, pkey = PTring.next()
                        st["p"] = (pt_, pkey)
                        if bias is None:
                            S.op("act", lambda e: e.activation(out=pt_[:, :], in_=sp_[:, :], func=AF.Exp, scale=scale), reads=[skey], writes=[pkey])
                        else:
                            S.op("act", lambda e: e.activation(out=pt_[:, :], in_=sp_[:, :], func=AF.Exp, scale=scale, bias=bias[0]), reads=[skey, bias[1]], writes=[pkey])

                    def fPV(st=st, kt=kt, oset=oset):
                        pt_, pkey = st["p"]
                        for qs in range(4):
                            ob, okey = oset[qs // per_bank]
                            o0 = (qs % per_bank) * (dv + 1)
                            st_flag = (kt == 0 and (qs % per_bank) == 0)
                            S.op("pe", lambda e, ob=ob, o0=o0, qs=qs, st_flag=st_flag: e.matmul(ob[:, o0:o0 + dv + 1], lhsT=pt_[:, qs * 128:(qs + 1) * 128], rhs=Vfn(kt), start=st_flag, stop=(kt == nk_tiles - 1), skip_group_check=True),
                                 reads=[pkey, vkey], writes=[okey])

                    post = (lambda qc=qc, oset=oset: out_cb(qc, oset, per_bank)) if kt == nk_tiles - 1 else None
                    steps.append({"S": fS, "E": fE, "PV": fPV, "post": post, "pre": None})
            return steps

        def run_steps(steps, LA):
            n = len(steps)
            for i in range(n + LA):
                if i < n:
                    if steps[i]["pre"] is not None:
                        steps[i]["pre"]()
                    steps[i]["S"]()
                m = i - LA
                if m >= 0:
                    steps[m]["E"]()
                    steps[m]["PV"]()
                    if steps[m]["post"] is not None:
                        steps[m]["post"]()

        def attention(*args, LA=1, **kw):
            run_steps(attn_steps(*args, **kw), LA)

        def layer(l, x_src, xs_key):
            with ExitStack() as ph:
                a = lambda name, shape, dt: sb(name, shape, dt, ph)
                Win = a("Win", [128, 8, IN_W], BF16); Wqb = a("Wqb", [128, 2, 768], BF16); Wkvb = a("Wkvb", [128, 1, 1024], BF16)
                load_weight(Win, "Win", w_in[l], 8, IN_W, gc_mix[:, l, :], "gcA")
                load_weight(Wqb, "Wqb", w_q_b[l], 2, 768, gc_q[:, l, :], "gcB")
                load_weight(Wkvb, "Wkvb", w_kv_b[l], 1, 1024, gc_kv[:, l, :], "gcB")
                gq = a("gq", [128, 64], F32); gk = a("gk", [128, 64], F32)
                S.dma("sp", lambda e: e.dma_start(out=gq[:], in_=gqa_q_norm[l, :].partition_broadcast(128)), writes=["gq"])
                S.dma("sp", lambda e: e.dma_start(out=gk[:], in_=gqa_k_norm[l, :].partition_broadcast(128)), writes=["gk"])
                Vg = a("Vg", [128, NT, 2, 65], BF16); Vm = a("Vm", [128, NT, 8, 65], BF16)
                S.op("pool", lambda e: e.memset(Vg[:, :, :, 64:65], 1.0), writes=["Vg"])
                S.op("pool", lambda e: e.memset(Vm[:, :, :, 64:65], 1.0), writes=["Vm"])
                with ExitStack() as p1:
                    b = lambda name, shape, dt: sb(name, shape, dt, p1)
                    xtR = Ring(b, "xt", [128, D], F32, 2); hnR = Ring(b, "hn", [128, D], BF16, 2)
                    XTR = Ring(b, "XT", [128, 8, 128], BF16, 2)
                    scr = b("scr", [128, D], F32)
                    prR = Ring(b, "pr", [128, IN_W], F32, 2)
                    stR = Ring(b, "st", [128, 12], F32, 2)
                    rsx = Ring(b, "rsx", [128, 2], F32, 2)
                    qn = b("qn", [128, 10, 64], F32); tmpa = b("tmpa", [128, 10, 32], F32); tmpb = b("tmpb", [128, 10, 32], F32)
                    rot_f = b("rot", [128, 640], BF16); kdup_f = b("kdup", [128, 256], BF16)
                    rot = rot_f[:, :].rearrange("p (h d) -> p h d", h=10); kdup = kdup_f[:, :].rearrange("p (g u d) -> p g u d", g=2, u=2)
                    cb = b("cb", [128, 384], BF16); cT = b("cT", [128, 3, 128], BF16)
                    qm32_f = b("qm32", [128, 768], F32); qmb_f = b("qmb", [128, 768], BF16); km_f = b("km", [128, 768], BF16)
                    qm32 = qm32_f[:, :].rearrange("p (h d) -> p h d", h=8); qmb = qmb_f[:, :].rearrange("p (h d) -> p h d", h=8); km = km_f[:, :].rearrange("p (h d) -> p h d", h=8)
                    krot = b("krot", [128, 32], F32)
                    QKst = Ring(b, "QKst", [128, 6, 512], BF16, 2)
                    QmSt = Ring(b, "QmSt", [128, 8, 512], BF16, 1); KmSt = Ring(b, "KmSt", [128, 8, 512], BF16, 1)
                    for c in range(8):
                        qkst, qkk = QKst.next(); qmst, qmk = QmSt.next(); kmst, kmk = KmSt.next()
                        for t in range(4):
                            j = c * 4 + t
                            xt, xk = xtR.next(); hn, hk = hnR.next(); XT, XTk = XTR.next(); pr, prk = prR.next(); st, stk = stR.next(); rs, rsk = rsx.next()
                            S.dma("sp", lambda e, xt=xt, j=j: e.dma_start(out=xt[:], in_=x_src[j * 128:(j + 1) * 128, :]), reads=[xs_key], writes=[xk])
                            norm_transpose(xt, xk, hn, hk, XT, XTk, 0, rs, rsk, scr, "scr")
                            for nb, (n0, nn) in enumerate(((0, 512), (512, 512), (1024, 160))):
                                for kc in range(8):
                                    S.op("pe", lambda e, nb=nb, n0=n0, nn=nn, kc=kc, XT=XT: e.matmul(FB[nb][:, 0:nn], lhsT=XT[:, kc, :], rhs=Win[:, kc, n0:n0 + nn], start=(kc == 0), stop=(kc == 7)), reads=[XTk, "Win"], writes=[FK[nb]])
                                S.op("act", lambda e, nb=nb, n0=n0, nn=nn, pr=pr: e.activation(out=pr[:, n0:n0 + nn], in_=FB[nb][:, 0:nn], func=AF.Copy), reads=[FK[nb]], writes=[prk])
                            S.op("dve", lambda e, pr=pr: e.tensor_tensor(out=scr[:, 0:640], in0=pr[:, 0:640], in1=pr[:, 0:640], op=ALU.mult), reads=[prk], writes=["scr"])
                            S.op("dve", lambda e, st=st: e.tensor_reduce(out=st[:, 0:10], in_=scr[:, 0:640].rearrange("p (h d) -> p h d", h=10), axis=AX.X, op=ALU.add), reads=["scr"], writes=[stk])
                            S.op("dve", lambda e, pr=pr, st=st: e.scalar_tensor_tensor(out=scr[:, 0:256], in0=pr[:, 768:1024], scalar=1.0, in1=pr[:, 768:1024], op0=ALU.mult, op1=ALU.mult, accum_out=st[:, 10:11]), reads=[prk], writes=["scr", stk])
                            S.op("dve", lambda e, pr=pr, st=st: e.scalar_tensor_tensor(out=scr[:, 0:128], in0=pr[:, 1024:1152], scalar=1.0, in1=pr[:, 1024:1152], op0=ALU.mult, op1=ALU.mult, accum_out=st[:, 11:12]), reads=[prk], writes=["scr", stk])
                            S.op("dve", lambda e, st=st: e.tensor_tensor(out=st[:, :], in0=st[:, :], in1=invn[:, :], op=ALU.mult), reads=[stk, "invn"], writes=[stk])
                            S.op("dve", lambda e, st=st: e.tensor_scalar(out=st[:, :], in0=st[:, :], scalar1=EPS, scalar2=None, op0=ALU.add), reads=[stk], writes=[stk])
                            S.op("pool", lambda e, st=st: e.tensor_tensor(out=st[:, :], in0=st[:, :], in1=neghalf[:, 0:12], op=ALU.pow), reads=[stk, "neghalf"], writes=[stk])
                            prqk = pr[:, 0:640].rearrange("p (h d) -> p h d", h=10)
                            S.op("dve", lambda e, st=st, prqk=prqk: e.tensor_tensor(out=qn[:, :, :], in0=prqk, in1=st[:, 0:10].unsqueeze(2).to_broadcast([128, 10, 64]), op=ALU.mult), reads=[prk, stk], writes=["qn"])
                            S.op("dve", lambda e: e.tensor_tensor(out=qn[:, 0:8, :], in0=qn[:, 0:8, :], in1=gq[:, :].unsqueeze(1).to_broadcast([128, 8, 64]), op=ALU.mult), reads=["qn", "gq"], writes=["qn"])
                            S.op("dve", lambda e: e.tensor_tensor(out=qn[:, 8:10, :], in0=qn[:, 8:10, :], in1=gk[:, :].unsqueeze(1).to_broadcast([128, 2, 64]), op=ALU.mult), reads=["qn", "gk"], writes=["qn"])
                            cg = ropeg_sb[:, j, 0:32].unsqueeze(1).to_broadcast([128, 10, 32]); sg = ropeg_sb[:, j, 32:64].unsqueeze(1).to_broadcast([128, 10, 32])
                            S.op("dve", lambda e, cg=cg: e.tensor_tensor(out=tmpa[:, :, :], in0=qn[:, :, 0:32], in1=cg, op=ALU.mult), reads=["qn", "ropeg"], writes=["tmpa"])
                            S.op("dve", lambda e, sg=sg: e.tensor_tensor(out=tmpb[:, :, :], in0=qn[:, :, 32:64], in1=sg, op=ALU.mult), reads=["qn", "ropeg"], writes=["tmpb"])
                            S.op("dve", lambda e: e.tensor_tensor(out=rot[:, :, 0:32], in0=tmpa[:, :, :], in1=tmpb[:, :, :], op=ALU.subtract), reads=["tmpa", "tmpb"], writes=["rot"])
                            S.op("dve", lambda e, cg=cg: e.tensor_tensor(out=tmpa[:, :, :], in0=qn[:, :, 32:64], in1=cg, op=ALU.mult), reads=["qn", "ropeg"], writes=["tmpa"])
                            S.op("dve", lambda e, sg=sg: e.tensor_tensor(out=tmpb[:, :, :], in0=qn[:, :, 0:32], in1=sg, op=ALU.mult), reads=["qn", "ropeg"], writes=["tmpb"])
                            S.op("dve", lambda e: e.tensor_tensor(out=rot[:, :, 32:64], in0=tmpa[:, :, :], in1=tmpb[:, :, :], op=ALU.add), reads=["tmpa", "tmpb"], writes=["rot"])
                            S.op("dve", lambda e: e.tensor_copy(out=kdup[:, :, :, :], in_=rot[:, 8:10, :].unsqueeze(2).to_broadcast([128, 2, 2, 64])), reads=["rot"], writes=["kdup"])
                            for p_ in range(4):
                                S.op("pe", lambda e, p_=p_: e.transpose(out=T1[:, p_ * 128:(p_ + 1) * 128], in_=rot_f[:, p_ * 128:(p_ + 1) * 128], identity=identb[:]), reads=["rot", "identb"], writes=["T1"])
                            for g_ in range(2):
                                S.op("pe", lambda e, g_=g_: e.transpose(out=T1[:, (4 + g_) * 128:(5 + g_) * 128], in_=kdup_f[:, g_ * 128:(g_ + 1) * 128], identity=identb[:]), reads=["kdup", "identb"], writes=["T1"])
                            S.op("dve", lambda e, qkst=qkst, t=t: e.tensor_copy(out=qkst[:, :, t * 128:(t + 1) * 128], in_=T1[:, 0:768].rearrange("p (k t) -> p k t", k=6)), reads=["T1"], writes=[qkk])
                            S.op("act", lambda e, pr=pr, j=j: e.activation(out=Vg[:, j, :, 0:64], in_=pr[:, 640:768].rearrange("p (g d) -> p g d", g=2), func=AF.Copy), reads=[prk], writes=["Vg"])
                            S.op("dve", lambda e, pr=pr, st=st: e.tensor_scalar(out=cb[:, 0:256], in0=pr[:, 768:1024], scalar1=st[:, 10:11], scalar2=None, op0=ALU.mult), reads=[prk, stk], writes=["cb"])
                            S.op("dve", lambda e, pr=pr, st=st: e.tensor_scalar(out=cb[:, 256:384], in0=pr[:, 1024:1152], scalar1=st[:, 11:12], scalar2=None, op0=ALU.mult), reads=[prk, stk], writes=["cb"])
                            for i_ in range(3):
                                S.op("pe", lambda e, i_=i_: e.transpose(out=T1[:, i_ * 128:(i_ + 1) * 128], in_=cb[:, i_ * 128:(i_ + 1) * 128], identity=identb[:]), reads=["cb", "identb"], writes=["T1"])
                            S.op("dve", lambda e: e.tensor_copy(out=cT[:, :, :], in_=T1[:, 0:384].rearrange("p (k t) -> p k t", k=3)), reads=["T1"], writes=["cT"])
                            for nb, (n0, nn) in ((3, (0, 512)), (4, (512, 256))):
                                for kc in range(2):
                                    S.op("pe", lambda e, nb=nb, n0=n0, nn=nn, kc=kc: e.matmul(FB[nb][:, 0:nn], lhsT=cT[:, kc, :], rhs=Wqb[:, kc, n0:n0 + nn], start=(kc == 0), stop=(kc == 1)), reads=["cT", "Wqb"], writes=[FK[nb]])
                            qm32f = qm32_f
                            S.op("act", lambda e: e.activation(out=qm32f[:, 0:512], in_=FB[3][:, 0:512], func=AF.Copy), reads=[FK[3]], writes=["qm32"])
                            S.op("act", lambda e: e.activation(out=qm32f[:, 512:768], in_=FB[4][:, 0:256], func=AF.Copy), reads=[FK[4]], writes=["qm32"])
                            for nb in range(2):
                                S.op("pe", lambda e, nb=nb: e.matmul(FB[nb][:, 0:512], lhsT=cT[:, 2, :], rhs=Wkvb[:, 0, nb * 512:(nb + 1) * 512], start=True, stop=True), reads=["cT", "Wkvb"], writes=[FK[nb]])
                                kvv = FB[nb][:, 0:512].rearrange("p (h d) -> p h d", h=4)
                                S.op("act", lambda e, kvv=kvv, nb=nb, j=j: e.activation(out=Vm[:, j, nb * 4:(nb + 1) * 4, 0:64], in_=kvv[:, :, 64:128], func=AF.Copy), reads=[FK[nb]], writes=["Vm"])
                                S.op("dve", lambda e, kvv=kvv, nb=nb: e.tensor_copy(out=km[:, nb * 4:(nb + 1) * 4, 0:64], in_=kvv[:, :, 0:64]), reads=[FK[nb]], writes=["km"])
                            cm = ropem_sb[:, j, 0:16]; sm = ropem_sb[:, j, 16:32]
                            cm8 = cm.unsqueeze(1).to_broadcast([128, 8, 16]); sm8 = sm.unsqueeze(1).to_broadcast([128, 8, 16])
                            ta = tmpa[:, 0:8, 0:16]; tb = tmpb[:, 0:8, 0:16]
                            S.op("dve", lambda e: e.tensor_copy(out=qmb[:, :, 0:64], in_=qm32[:, :, 0:64]), reads=["qm32"], writes=["qmb"])
                            S.op("dve", lambda e, cm8=cm8, ta=ta: e.tensor_tensor(out=ta, in0=qm32[:, :, 64:80], in1=cm8, op=ALU.mult), reads=["qm32", "ropem"], writes=["tmpa"])
                            S.op("dve", lambda e, sm8=sm8, tb=tb: e.tensor_tensor(out=tb, in0=qm32[:, :, 80:96], in1=sm8, op=ALU.mult), reads=["qm32", "ropem"], writes=["tmpb"])
                            S.op("dve", lambda e, ta=ta, tb=tb: e.tensor_tensor(out=qmb[:, :, 64:80], in0=ta, in1=tb, op=ALU.subtract), reads=["tmpa", "tmpb"], writes=["qmb"])
                            S.op("dve", lambda e, cm8=cm8, ta=ta: e.tensor_tensor(out=ta, in0=qm32[:, :, 80:96], in1=cm8, op=ALU.mult), reads=["qm32", "ropem"], writes=["tmpa"])
                            S.op("dve", lambda e, sm8=sm8, tb=tb: e.tensor_tensor(out=tb, in0=qm32[:, :, 64:80], in1=sm8, op=ALU.mult), reads=["qm32", "ropem"], writes=["tmpb"])
                            S.op("dve", lambda e, ta=ta, tb=tb: e.tensor_tensor(out=qmb[:, :, 80:96], in0=ta, in1=tb, op=ALU.add), reads=["tmpa", "tmpb"], writes=["qmb"])
                            ta1 = tmpa[:, 8, 0:16]; tb1 = tmpb[:, 8, 0:16]
                            S.op("dve", lambda e, pr=pr, cm=cm, ta1=ta1: e.tensor_tensor(out=ta1, in0=pr[:, 1152:1168], in1=cm, op=ALU.mult), reads=[prk, "ropem"], writes=["tmpa"])
                            S.op("dve", lambda e, pr=pr, sm=sm, tb1=tb1: e.tensor_tensor(out=tb1, in0=pr[:, 1168:1184], in1=sm, op=ALU.mult), reads=[prk, "ropem"], writes=["tmpb"])
                            S.op("dve", lambda e, ta1=ta1, tb1=tb1: e.tensor_tensor(out=krot[:, 0:16], in0=ta1, in1=tb1, op=ALU.subtract), reads=["tmpa", "tmpb"], writes=["krot"])
                            S.op("dve", lambda e, pr=pr, cm=cm, ta1=ta1: e.tensor_tensor(out=ta1, in0=pr[:, 1168:1184], in1=cm, op=ALU.mult), reads=[prk, "ropem"], writes=["tmpa"])
                            S.op("dve", lambda e, pr=pr, sm=sm, tb1=tb1: e.tensor_tensor(out=tb1, in0=pr[:, 1152:1168], in1=sm, op=ALU.mult), reads=[prk, "ropem"], writes=["tmpb"])
                            S.op("dve", lambda e, ta1=ta1, tb1=tb1: e.tensor_tensor(out=krot[:, 16:32], in0=ta1, in1=tb1, op=ALU.add), reads=["tmpa", "tmpb"], writes=["krot"])
                            S.op("dve", lambda e: e.tensor_copy(out=km[:, :, 64:96], in_=krot[:, :].unsqueeze(1).to_broadcast([128, 8, 32])), reads=["krot"], writes=["km"])
                            for h in range(8):
                                S.op("pe", lambda e, h=h: e.transpose(out=T0[0:96, h * 128:(h + 1) * 128], in_=qmb_f[:, h * 96:(h + 1) * 96], identity=identb[:]), reads=["qmb", "identb"], writes=["T0"])
                            S.op("dve", lambda e, qmst=qmst, t=t: e.tensor_copy(out=qmst[0:96, :, t * 128:(t + 1) * 128], in_=T0[0:96, :].rearrange("p (k t) -> p k t", k=8)), reads=["T0"], writes=[qmk])
                            for h in range(8):
                                S.op("pe", lambda e, h=h: e.transpose(out=T1[0:96, h * 128:(h + 1) * 128], in_=km_f[:, h * 96:(h + 1) * 96], identity=identb[:]), reads=["km", "identb"], writes=["T1"])
                            S.op("dve", lambda e, kmst=kmst, t=t: e.tensor_copy(out=kmst[0:96, :, t * 128:(t + 1) * 128], in_=T1[0:96, :].rearrange("p (k t) -> p k t", k=8)), reads=["T1"], writes=[kmk])
                        csl = slice(c * 512, (c + 1) * 512)
                        S.dma("sp", lambda e, qkst=qkst, csl=csl: e.dma_start(out=QgT[:, :, csl], in_=qkst[:, 0:4, :]), reads=[qkk], writes=["QgT"])
                        S.dma("sp", lambda e, qkst=qkst, csl=csl: e.dma_start(out=KgT[:, :, csl], in_=qkst[:, 4:6, :]), reads=[qkk], writes=["KgT"])
                        S.dma("sp", lambda e, qmst=qmst, csl=csl: e.dma_start(out=QmT[:, :, csl], in_=qmst[0:96, :, :]), reads=[qmk], writes=["QmT"])
                        S.dma("sp", lambda e, kmst=kmst, csl=csl: e.dma_start(out=KmT[:, :, csl], in_=kmst[0:96, :, :]), reads=[kmk], writes=["KmT"])
                S.barrier()
                if stop_after == "P1":
                    for nm, t_ in (("QgT", QgT), ("KgT", KgT), ("QmT", QmT), ("KmT", KmT)):
                        d_ = dbgt(nm, t_.shape, BF16)
                        S.dma("sp", lambda e, d_=d_, t_=t_: e.dma_start(out=d_, in_=t_), reads=[nm], writes=["dbg"])
                    return False
                with ExitStack() as p2:
                    b = lambda name, shape, dt: sb(name, shape, dt, p2)
                    QTR = Ring(b, "QTb", [128, S_LEN], BF16, 2); KTR = Ring(b, "KTb", [128, S_LEN], BF16, 2)
                    PTR = Ring(b, "PT", [128, 512], BF16, 4)
                    OoR = Ring(b, "Oo", [128, 4, 64], F32, 2); rcR = Ring(b, "rc", [128, 4], F32, 2)
                    Sring = Ring(lambda n, s, d: FB[(0, 1, 2, 5)[int(n[-1])]], "SR", None, None, 4)
                    Sring.items = [(FB[i], FK[i]) for i in (0, 1, 2, 5)]
                    Osets = [[(FB[3], "F3")], [(FB[4], "F4")]]

                    def make_cb(hcol):
                        def cbk(qc, oset, per_bank):
                            ob, okey = oset[0]
                            ov = ob[:, 0:260].rearrange("p (q d) -> p q d", q=4)
                            rc, rck = rcR.next(); Oo, Ook = OoR.next()
                            S.op("dve", lambda e: e.reciprocal(out=rc[:, :], in_=ov[:, :, 64]), reads=[okey], writes=[rck])
                            S.op("dve", lambda e: e.tensor_tensor(out=Oo[:, :, :], in0=ov[:, :, 0:64], in1=rc[:, :].unsqueeze(2).to_broadcast([128, 4, 64]), op=ALU.mult), reads=[okey, rck], writes=[Ook])
                            S.dma("sp", lambda e: e.dma_start(out=Odr[qc * 512:(qc + 1) * 512, hcol:hcol + 64].rearrange("(q p) d -> p q d", p=128), in_=Oo[:, :, :]), reads=[Ook], writes=["Odr"])
                        return cbk

                    heads = []
                    for pr_ in range(4):
                        g_ = pr_ // 2

                        def ld(pr_=pr_, g_=g_):
                            QTb, qk_ = QTR.next(); KTb, kk_ = KTR.next()
                            S.dma("sp", lambda e: e.dma_start(out=QTb[:, :], in_=QgT[:, pr_, :]), reads=["QgT"], writes=[qk_])
                            S.dma("sp", lambda e: e.dma_start(out=KTb[:, :], in_=KgT[:, g_, :]), reads=["KgT"], writes=[kk_])
                            return (QTb, qk_, KTb, kk_)

                        def mk(bufs, pr_=pr_, g_=g_):
                            QTb, qk_, KTb, kk_ = bufs
                            st_ = []
                            for hh in range(2):
                                h = pr_ * 2 + hh
                                r0 = hh * 64
                                st_ += attn_steps(QTb[r0:r0 + 64, :], qk_, KTb[r0:r0 + 64, :], kk_, NT, lambda kt, g_=g_: Vg[:, kt, g_, :], "Vg", 64, 64 ** -0.5, None, make_cb(h * 64), Sring, PTR, Osets)
                            return st_
                        heads.append((ld, mk))
                    for h in range(8):
                        def ld(h=h):
                            QTb, qk_ = QTR.next(); KTb, kk_ = KTR.next()
                            S.dma("sp", lambda e: e.dma_start(out=QTb[0:96, :], in_=QmT[:, h, :]), reads=["QmT"], writes=[qk_])
                            S.dma("sp", lambda e: e.dma_start(out=KTb[0:96, :], in_=KmT[:, h, :]), reads=["KmT"], writes=[kk_])
                            return (QTb, qk_, KTb, kk_)

                        def mk(bufs, h=h):
                            QTb, qk_, KTb, kk_ = bufs
                            return attn_steps(QTb[0:96, :], qk_, KTb[0:96, :], kk_, NT, lambda kt, h=h: Vm[:, kt, h, :], "Vm", 64, 96 ** -0.5, None, make_cb(512 + h * 64), Sring, PTR, Osets)
                        heads.append((ld, mk))
                    allsteps = []
                    bufs = heads[0][0]()
                    for gi, (ld, mk) in enumerate(heads):
                        st_ = mk(bufs)
                        if gi + 1 < len(heads):
                            holder = {}
                            nxt = heads[gi + 1][0]
                            st_[0]["pre"] = (lambda holder=holder, nxt=nxt: holder.__setitem__("b", nxt()))
                            run_steps(st_, ATT_LA)
                            bufs = holder["b"]
                        else:
                            run_steps(st_, ATT_LA)
                S.barrier()
            if stop_after == "P2":
                d_ = dbgt("Odr", [S_LEN, 1024], F32)
                S.dma("sp", lambda e: e.dma_start(out=d_, in_=Odr), reads=["Odr"], writes=["dbg"])
                return False
            with ExitStack() as p3:
                b = lambda name, shape, dt: sb(name, shape, dt, p3)
                Wo = b("Wo", [128, 8, 1024], BF16)
                load_weight(Wo[:, 0:4, :], "Wo", w_o[l, 0:512, :], 4, 1024, gc_og[:, l, :], "gcB")
                load_weight(Wo[:, 4:8, :], "Wo", w_o[l, 512:1024, :], 4, 1024, gc_om[:, l, :], "gcB")
                otR = Ring(b, "ot", [128, D], F32, 2); hnR = Ring(b, "hn3", [128, D], BF16, 2); XTR = Ring(b, "XT3", [128, 8, 128], BF16, 2)
                xtR = Ring(b, "xt3", [128, D], F32, 2); yR = Ring(b, "y3", [128, D], F32, 2)
                scr = b("scr3", [128, D], F32); rsx = Ring(b, "rs3", [128, 2], F32, 2)
                for j in range(NT):
                    ot, otk = otR.next(); hn, hk = hnR.next(); XT, XTk = XTR.next(); xt, xk = xtR.next(); y, yk = yR.next(); rs, rsk = rsx.next()
                    S.dma("sp", lambda e, ot=ot, j=j: e.dma_start(out=ot[:], in_=Odr[j * 128:(j + 1) * 128, :]), reads=["Odr"], writes=[otk])
                    S.dma("sp", lambda e, xt=xt, j=j: e.dma_start(out=xt[:], in_=x_src[j * 128:(j + 1) * 128, :]), reads=[xs_key], writes=[xk])
                    norm_transpose(ot, otk, hn, hk, XT, XTk, 0, rs, rsk, scr, "scr3", halves=2)
                    for nb in range(2):
                        for kc in range(8):
                            S.op("pe", lambda e, nb=nb, kc=kc, XT=XT: e.matmul(FB[nb][:, :], lhsT=XT[:, kc, :], rhs=Wo[:, kc, nb * 512:(nb + 1) * 512], start=(kc == 0), stop=(kc == 7)), reads=[XTk, "Wo"], writes=[FK[nb]])
                        S.op("dve", lambda e, nb=nb, y=y, xt=xt: e.tensor_tensor(out=y[:, nb * 512:(nb + 1) * 512], in0=FB[nb][:, :], in1=xt[:, nb * 512:(nb + 1) * 512], op=ALU.add), reads=[FK[nb], xk], writes=[yk])
                    S.dma("sp", lambda e, y=y, j=j: e.dma_start(out=xres[j * 128:(j + 1) * 128, :], in_=y[:]), reads=[yk], writes=["xres"])
            S.barrier()
            if stop_after == "P3":
                return False

            with ExitStack() as p4:
                b = lambda name, shape, dt: sb(name, shape, dt, p4)
                Wq = b("Wmq", [128, 8, 512], BF16); Wkv = b("Wmkv", [128, 8, 1024], BF16); Wmo = b("Wmo", [128, 4, 1024], BF16)
                load_weight(Wq, "Wmq", w_mem_q[l], 8, 512, gc_mem[:, l, :], "gcA")
                load_weight(Wkv, "Wmkv", w_mem_kv[l], 8, 1024, gc_memkv[:, l, :], "gcA")
                load_weight(Wmo, "Wmo", w_mem_o[l], 4, 1024)
                xtR = Ring(b, "xt4", [128, D], F32, 8); hnR = Ring(b, "hn4", [128, D], BF16, 2)
                scr = b("scr4", [128, D], F32); rsx = Ring(b, "rs4", [128, 2], F32, 2)
                XTm = b("XTm", [128, 8, 256], BF16)
                KTm = b("KTm", [128, 4, 256], BF16); Vme = b("Vme", [128, 2, 4, 129], BF16)
                S.op("pool", lambda e: e.memset(Vme[:, :, :, 128:129], 1.0), writes=["Vme"])
                for mt in range(2):
                    xt, xk = xtR.next(); hn, hk = hnR.next(); rs, rsk = rsx.next()
                    S.dma("sp", lambda e, xt=xt, mt=mt: e.dma_start(out=xt[:], in_=mem_in[mt * 128:(mt + 1) * 128, :]), writes=[xk])
                    norm_transpose(xt, xk, hn, hk, XTm, "XTm", mt * 128, rs, rsk, scr, "scr4")
                for h in range(4):
                    for kc in range(8):
                        S.op("pe", lambda e, h=h, kc=kc: e.matmul(FB[2][:, 0:256], lhsT=Wkv[:, kc, h * 128:(h + 1) * 128], rhs=XTm[:, kc, :], start=(kc == 0), stop=(kc == 7)), reads=["Wmkv", "XTm"], writes=["F2"])
                    S.op("dve", lambda e, h=h: e.tensor_copy(out=KTm[:, h, :], in_=FB[2][:, 0:256]), reads=["F2"], writes=["KTm"])
                for mt in range(2):
                    for kc in range(8):
                        S.op("pe", lambda e, mt=mt, kc=kc: e.matmul(FB[3][:, :], lhsT=XTm[:, kc, mt * 128:(mt + 1) * 128], rhs=Wkv[:, kc, 512:1024], start=(kc == 0), stop=(kc == 7)), reads=["Wmkv", "XTm"], writes=["F3"])
                    S.op("dve", lambda e, mt=mt: e.tensor_copy(out=Vme[:, mt, :, 0:128], in_=FB[3][:, :].rearrange("p (h d) -> p h d", h=4)), reads=["F3"], writes=["Vme"])
                XTR = Ring(b, "XT4", [128, 8, 512], BF16, 2)
                QTR = Ring(b, "QTm", [128, 512], BF16, 2); PTR = Ring(b, "PT4", [128, 512], BF16, 4)
                ObR = Ring(b, "Obf", [128, 4, 512], BF16, 2); OTR = Ring(b, "OT4", [128, 4, 128], BF16, 2)
                rcR = Ring(b, "rc4", [128, 4], F32, 2); yR = Ring(b, "y4", [128, D], F32, 2)
                Sring = Ring(lambda n, s_, d: FB[int(n[-1])], "F", None, None, 2)
                Osets = [[(FB[2], "F2"), (FB[3], "F3")]]
                for c in range(8):
                    XT, XTk = XTR.next()
                    xts = []
                    for t in range(4):
                        j = c * 4 + t
                        xt, xk = xtR.next(); hn, hk = hnR.next(); rs, rsk = rsx.next()
                        xts.append((xt, xk))
                        S.dma("sp", lambda e, xt=xt, j=j: e.dma_start(out=xt[:], in_=xres[j * 128:(j + 1) * 128, :]), reads=["xres"], writes=[xk])
                        norm_transpose(xt, xk, hn, hk, XT, XTk, t * 128, rs, rsk, scr, "scr4")
                    Obf, Obk = ObR.next()
                    for h in range(4):
                        QTm, QTk = QTR.next()
                        for kc in range(8):
                            S.op("pe", lambda e, h=h, kc=kc, XT=XT: e.matmul(FB[4][:, :], lhsT=Wq[:, kc, h * 128:(h + 1) * 128], rhs=XT[:, kc, :], start=(kc == 0), stop=(kc == 7)), reads=["Wmq", XTk], writes=["F4"])
                        S.op("dve", lambda e, QTm=QTm: e.tensor_copy(out=QTm[:, :], in_=FB[4][:, :]), reads=["F4"], writes=[QTk])

                        def cbk(qc, oset, per_bank, h=h, Obf=Obf, Obk=Obk):
                            rc, rck = rcR.next()
                            for qs in range(4):
                                ob, okey = oset[qs // per_bank]
                                o0 = (qs % per_bank) * 129
                                S.op("dve", lambda e, ob=ob, o0=o0, qs=qs: e.reciprocal(out=rc[:, qs:qs + 1], in_=ob[:, o0 + 128:o0 + 129]), reads=[okey], writes=[rck])
                                S.op("dve", lambda e, ob=ob, o0=o0, qs=qs: e.tensor_scalar(out=Obf[:, qs, h * 128:(h + 1) * 128], in0=ob[:, o0:o0 + 128], scalar1=rc[:, qs:qs + 1], scalar2=None, op0=ALU.mult), reads=[okey, rck], writes=[Obk])
                        attention(QTm[:, :], QTk, KTm[:, h, :], "KTm", 2, lambda kt, h=h: Vme[:, kt, h, :], "Vme", 128, 128 ** -0.5, None, cbk, Sring, PTR, Osets, nqc=1)
                    for qs in range(4):
                        j = c * 4 + qs
                        xt, xk = xts[qs]
                        OT, OTk = OTR.next(); y, yk = yR.next()
                        for h in range(4):
                            S.op("pe", lambda e, h=h, qs=qs, Obf=Obf: e.transpose(out=T1[:, h * 128:(h + 1) * 128], in_=Obf[:, qs, h * 128:(h + 1) * 128], identity=identb[:]), reads=[Obk, "identb"], writes=["T1"])
                        S.op("dve", lambda e, OT=OT: e.tensor_copy(out=OT[:, :, :], in_=T1[:, 0:512].rearrange("p (k t) -> p k t", k=4)), reads=["T1"], writes=[OTk])
                        for nb in range(2):
                            for h in range(4):
                                S.op("pe", lambda e, nb=nb, h=h, OT=OT: e.matmul(FB[4 + nb][:, :], lhsT=OT[:, h, :], rhs=Wmo[:, h, nb * 512:(nb + 1) * 512], start=(h == 0), stop=(h == 3)), reads=[OTk, "Wmo"], writes=[FK[4 + nb]])
                            S.op("dve", lambda e, nb=nb, y=y, xt=xt: e.tensor_tensor(out=y[:, nb * 512:(nb + 1) * 512], in0=FB[4 + nb][:, :], in1=xt[:, nb * 512:(nb + 1) * 512], op=ALU.add), reads=[FK[4 + nb], xk], writes=[yk])
                        S.dma("sp", lambda e, y=y, j=j: e.dma_start(out=xres[j * 128:(j + 1) * 128, :], in_=y[:]), reads=[yk], writes=["xres"])
            S.barrier()
            if stop_after == "P4":
                return False
            with ExitStack() as p5:
                bo = lambda name, shape, dt: sb(name, shape, dt, p5)
                idx_i = bo("idx_i", [128, 64], I32)
                with ExitStack() as p5a:
                    b = lambda name, shape, dt: sb(name, shape, dt, p5a)
                    gfb = b("gfb", [128, D], F32)
                    S.dma("sp", lambda e: e.dma_start(out=gfb[:], in_=ln_ffn[l, :].partition_broadcast(128)), writes=["gfb"])
                    Wr = b("Wr", [128, 8, 16], F32)
                    for kc in range(8):
                        S.dma("sp", lambda e, kc=kc: e.dma_start(out=Wr[:, kc, :], in_=w_router[l, kc * 128:(kc + 1) * 128, :]), writes=["Wr"])
                    aff = b("aff", [128, NT, 16], F32)
                    xtR = Ring(b, "xt5", [128, D], F32, 2); h32R = Ring(b, "h32", [128, D], F32, 2); hbR = Ring(b, "hb5", [128, D], BF16, 2)
                    scr = b("scr5", [128, D], F32); rsx = Ring(b, "rs5", [128, 2], F32, 2)
                    XT32R = Ring(b, "XT32", [128, 8, 128], F32, 2)
                    smR = Ring(b, "sm5", [128, 4], F32, 2); exR = Ring(b, "ex5", [128, 16], F32, 2)
                    for j in range(NT):
                        xt, xk = xtR.next(); h32, h32k = h32R.next(); hb, hbk = hbR.next(); rs, rsk = rsx.next(); XT32, X32k = XT32R.next()
                        sm, smk = smR.next(); ex, exk = exR.next()
                        S.dma("sp", lambda e, xt=xt, j=j: e.dma_start(out=xt[:], in_=xres[j * 128:(j + 1) * 128, :]), reads=["xres"], writes=[xk])
                        norm_transpose(xt, xk, h32, h32k, None, None, 0, rs, rsk, scr, "scr5", gain_b=gfb, gbkey="gfb")
                        S.op("pool", lambda e, hb=hb, h32=h32: e.tensor_copy(out=hb[:, :], in_=h32[:, :]), reads=[h32k], writes=[hbk])
                        S.dma("sp", lambda e, hb=hb, j=j: e.dma_start(out=hdr[j * 128:(j + 1) * 128, :], in_=hb[:]), reads=[hbk], writes=["hdr"])
                        for kc in range(8):
                            S.op("pe", lambda e, kc=kc, h32=h32: e.transpose(out=FB[kc // 4][:, (kc % 4) * 128:(kc % 4 + 1) * 128], in_=h32[:, kc * 128:(kc + 1) * 128], identity=identf[:]), reads=[h32k, "identf"], writes=[FK[kc // 4]])
                        for hf in range(2):
                            S.op("dve", lambda e, hf=hf, XT32=XT32: e.tensor_copy(out=XT32[:, hf * 4:(hf + 1) * 4, :], in_=FB[hf][:, :].rearrange("p (k t) -> p k t", k=4)), reads=[FK[hf]], writes=[X32k])
                        for kc in range(8):
                            S.op("pe", lambda e, kc=kc, XT32=XT32: e.matmul(FB[2][:, 0:16], lhsT=XT32[:, kc, :], rhs=Wr[:, kc, :], start=(kc == 0), stop=(kc == 7)), reads=[X32k, "Wr"], writes=["F2"])
                        S.op("dve", lambda e, sm=sm: e.tensor_reduce(out=sm[:, 0:1], in_=FB[2][:, 0:16], axis=AX.X, op=ALU.max, negate=True), reads=["F2"], writes=[smk])
                        S.op("act", lambda e, sm=sm, ex=ex: e.activation(out=ex[:, :], in_=FB[2][:, 0:16], func=AF.Exp, bias=sm[:, 0:1], scale=1.0, accum_out=sm[:, 1:2]), reads=["F2", smk], writes=[exk, smk])
                        S.op("dve", lambda e, sm=sm: e.reciprocal(out=sm[:, 2:3], in_=sm[:, 1:2]), reads=[smk], writes=[smk])
                        S.op("dve", lambda e, sm=sm, ex=ex, j=j: e.tensor_scalar(out=aff[:, j, :], in0=ex[:, :], scalar1=sm[:, 2:3], scalar2=None, op0=ALU.mult), reads=[exk, smk], writes=["aff"])
                        S.dma("sp", lambda e, j=j: e.dma_start(out=affdr[j * 128:(j + 1) * 128, :], in_=aff[:, j, :]), reads=["aff"], writes=["affdr"])
                    affT = b("affT", [16, S_LEN], F32); junk = b("junk", [16, S_LEN], BF16); maskT = b("maskT", [16, S_LEN], BF16)
                    for rnd in range(2):
                        for jj in range(16):
                            j = rnd * 16 + jj
                            S.op("pe", lambda e, j=j, jj=jj: e.transpose(out=FB[jj // 4][0:16, (jj % 4) * 128:(jj % 4 + 1) * 128], in_=aff[:, j, :], identity=identf[:]), reads=["aff", "identf"], writes=[FK[jj // 4]])
                        for q4 in range(4):
                            S.op("dve", lambda e, rnd=rnd, q4=q4: e.tensor_copy(out=affT[:, (rnd * 4 + q4) * 512:(rnd * 4 + q4 + 1) * 512], in_=FB[q4][0:16, :]), reads=[FK[q4]], writes=["affT"])
                    bs = b("bs", [16, 8], F32)
                    S.op("dve", lambda e: e.memset(bs[:, 0:1], 0.0), writes=["bs"])
                    S.op("dve", lambda e: e.memset(bs[:, 1:2], 2.0), writes=["bs"])
                    for it in range(33):
                        S.op("dve", lambda e: e.tensor_tensor(out=bs[:, 2:3], in0=bs[:, 0:1], in1=bs[:, 1:2], op=ALU.add), reads=["bs"], writes=["bs"])
                        S.op("dve", lambda e: e.tensor_scalar(out=bs[:, 2:3], in0=bs[:, 2:3], scalar1=0.5, scalar2=None, op0=ALU.mult), reads=["bs"], writes=["bs"])
                        S.op("dve", lambda e: e.tensor_scalar(out=junk[:, :], in0=affT[:, :], scalar1=bs[:, 2:3], scalar2=0.0, op0=ALU.is_ge, op1=ALU.add, accum_out=bs[:, 3:4]), reads=["affT", "bs"], writes=["junk", "bs"])
                        S.op("dve", lambda e: e.tensor_scalar(out=bs[:, 4:5], in0=bs[:, 3:4], scalar1=511.5, scalar2=None, op0=ALU.is_ge), reads=["bs"], writes=["bs"])
                        S.op("dve", lambda e: e.tensor_tensor(out=bs[:, 5:6], in0=bs[:, 2:3], in1=bs[:, 0:1], op=ALU.subtract), reads=["bs"], writes=["bs"])
                        S.op("dve", lambda e: e.scalar_tensor_tensor(out=bs[:, 0:1], in0=bs[:, 5:6], scalar=bs[:, 4:5], in1=bs[:, 0:1], op0=ALU.mult, op1=ALU.add), reads=["bs"], writes=["bs"])
                        S.op("dve", lambda e: e.tensor_tensor(out=bs[:, 5:6], in0=bs[:, 1:2], in1=bs[:, 2:3], op=ALU.subtract), reads=["bs"], writes=["bs"])
                        S.op("dve", lambda e: e.scalar_tensor_tensor(out=bs[:, 1:2], in0=bs[:, 5:6], scalar=bs[:, 4:5], in1=bs[:, 2:3], op0=ALU.mult, op1=ALU.add), reads=["bs"], writes=["bs"])
                    S.op("dve", lambda e: e.tensor_scalar(out=maskT[:, :], in0=affT[:, :], scalar1=bs[:, 0:1], scalar2=None, op0=ALU.is_ge), reads=["affT", "bs"], writes=["maskT"])
                    mask_tok = b("mask_tok", [128, NT, 16], BF16)
                    for j in range(NT):
                        S.op("pe", lambda e, j=j: e.transpose(out=T0[:, j * 16:(j + 1) * 16], in_=maskT[:, j * 128:(j + 1) * 128], identity=identb[0:16, 0:16]), reads=["maskT", "identb"], writes=["T0"])
                    S.op("dve", lambda e: e.tensor_copy(out=mask_tok[:, :, :], in_=T0[:, 0:512].rearrange("p (j e) -> p j e", j=NT)), reads=["T0"], writes=["mask_tok"])
                    onesb = b("onesb", [128, 128], BF16); ustr = b("ustr", [128, 128], BF16)
                    S.op("pool", lambda e: e.memset(onesb[:], 1.0), writes=["onesb"])
                    S.op("pool", lambda e: e.memset(ustr[:], 1.0), writes=["ustr"])
                    S.op("pool", lambda e: e.affine_select(out=ustr[:], in_=ustr[:], pattern=[[1, 128]], compare_op=ALU.is_gt, fill=0.0, base=0, channel_multiplier=-1), reads=["ustr"], writes=["ustr"])
                    firstmm = True
                    for j in range(NT):
                        for i in range(j + 1):
                            lhs = ustr if i == j else onesb
                            S.op("pe", lambda e, j=j, i=i, lhs=lhs, firstmm=firstmm: e.matmul(FB[0][:, j * 16:(j + 1) * 16], lhsT=lhs[:, :], rhs=mask_tok[:, i, :], start=firstmm, stop=(i == j), skip_group_check=True), reads=["mask_tok", "onesb", "ustr"], writes=["F0"])
                            firstmm = False
                    posm = b("posm", [128, NT, 16], F32)
                    S.op("dve", lambda e: e.scalar_tensor_tensor(out=posm[:, :, :], in0=FB[0][:, :].rearrange("p (j e) -> p j e", j=NT), scalar=1.0, in1=mask_tok[:, :, :], op0=ALU.add, op1=ALU.mult), reads=["F0", "mask_tok"], writes=["posm"])
                    S.op("dve", lambda e: e.tensor_scalar(out=posm[:, :, :], in0=posm[:, :, :], scalar1=-1.0, scalar2=None, op0=ALU.add), reads=["posm"], writes=["posm"])
                    iota_s = b("iota_s", [128, 512], F32)
                    S.op("pool", lambda e: e.iota(iota_s[:, :], pattern=[[1, 512]], base=0, channel_multiplier=0, allow_small_or_imprecise_dtypes=True), writes=["iota_s"])
                    tv32 = b("tv32", [128, NT, 2], F32); tokval = b("tokval", [128, NT, 2], BF16)
                    S.op("pool", lambda e: e.iota(tv32[:, :, :], pattern=[[0, NT], [0, 2]], base=0, channel_multiplier=1, allow_small_or_imprecise_dtypes=True), writes=["tv32"])
                    S.op("pool", lambda e: e.iota(tv32[:, :, 1], pattern=[[1, NT]], base=0, channel_multiplier=0, allow_small_or_imprecise_dtypes=True), reads=["tv32"], writes=["tv32"])
                    S.op("pool", lambda e: e.tensor_copy(out=tokval[:, :, :], in_=tv32[:, :, :]), reads=["tv32"], writes=["tokval"])
                    OhR = Ring(b, "Oh", [128, 16, 512], BF16, 2)
                    firstmm = True
                    for j in range(NT):
                        Oh, Ohk = OhR.next()
                        eng = "dve"
                        S.op(eng, lambda e, Oh=Oh, j=j: e.tensor_tensor(out=Oh[:, :, :], in0=iota_s[:, :].unsqueeze(1).to_broadcast([128, 16, 512]), in1=posm[:, j, :].unsqueeze(2).to_broadcast([128, 16, 512]), op=ALU.is_equal), reads=["iota_s", "posm"], writes=[Ohk])
                        for ee in range(16):
                            for c in range(4):
                                col = (ee * 4 + c) * 2
                                S.op("pe", lambda e, Oh=Oh, ee=ee, c=c, col=col, j=j, firstmm=firstmm: e.matmul(FB[5][:, col:col + 2], lhsT=Oh[:, ee, c * 128:(c + 1) * 128], rhs=tokval[:, j, :], start=firstmm, stop=(j == NT - 1), skip_group_check=True), reads=[Ohk, "tokval"], writes=["F5"])
                                firstmm = False
                    idxf = b("idxf", [128, 64], F32)
                    f5s = b("f5s", [128, 128], F32)
                    S.op("dve", lambda e: e.tensor_copy(out=f5s[:, :], in_=FB[5][:, 0:128]), reads=["F5"], writes=["f5s"])
                    f5v = f5s[:, :].rearrange("p (s two) -> p s two", two=2)
                    S.op("dve", lambda e: e.scalar_tensor_tensor(out=idxf[:, :], in0=f5v[:, :, 1], scalar=128.0, in1=f5v[:, :, 0], op0=ALU.mult, op1=ALU.add), reads=["f5s"], writes=["idxf"])
                    S.op("dve", lambda e: e.tensor_copy(out=idx_i[:, :], in_=idxf[:, :]), reads=["idxf"], writes=["idx_i"])
                    if stop_after == "P5a":
                        d1 = dbgt("idx", [128, 64], I32); d2 = dbgt("aff", [S_LEN, 16], F32); d3 = dbgt("bs", [16, 8], F32)
                        S.dma("sp", lambda e: e.dma_start(out=d1, in_=idx_i[:, :]), reads=["idx_i"], writes=["dbg"])
                        S.dma("sp", lambda e: e.dma_start(out=d2, in_=affdr), reads=["affdr"], writes=["dbg"])
                        S.dma("sp", lambda e: e.dma_start(out=d3, in_=bs[:, :]), reads=["bs"], writes=["dbg"])
                S.barrier()
                if stop_after == "P5a":
                    return False
                with ExitStack() as p5b:
                    b = lambda name, shape, dt: sb(name, shape, dt, p5b)
                    WgR = Ring(b, "Wg", [128, 8, 512], BF16, 2); WuR = Ring(b, "Wu", [128, 8, 512], BF16, 2); WdR = Ring(b, "Wd", [128, 4, 1024], BF16, 2)
                    XgTR = Ring(b, "XgT", [128, 8, 512], BF16, 2); xgR = Ring(b, "xg", [128, D], BF16, 8); gaR = Ring(b, "ga", [128, 16], F32, 8)
                    GTR = Ring(b, "GT", [128, 4, 512], BF16, 2); sgR = Ring(b, "sg", [128, 512], F32, 2); yR = Ring(b, "y5", [128, D], F32, 2)
                    for ee in range(16):
                        Wg, Wgk = WgR.next(); Wu, Wuk = WuR.next(); Wd, Wdk = WdR.next()
                        load_weight(Wg, Wgk, w_gate[l, ee], 8, 512)
                        load_weight(Wu, Wuk, w_up[l, ee], 8, 512)
                        load_weight(Wd, Wdk, w_down[l, ee], 4, 1024)
                        XgT, XgTk = XgTR.next(); GT, GTk = GTR.next()
                        gas = []
                        for c in range(4):
                            xg, xgk = xgR.next(); ga, gak = gaR.next()
                            gas.append((ga, gak))
                            col = ee * 4 + c
                            S.dma("pool", lambda e, xg=xg, col=col: e.indirect_dma_start(out=xg[:, :], out_offset=None, in_=hdr[:, :], in_offset=bass.IndirectOffsetOnAxis(ap=idx_i[:, col:col + 1], axis=0)), reads=["idx_i", "hdr"], writes=[xgk])
                            S.dma("pool", lambda e, ga=ga, col=col: e.indirect_dma_start(out=ga[:, :], out_offset=None, in_=affdr[:, :], in_offset=bass.IndirectOffsetOnAxis(ap=idx_i[:, col:col + 1], axis=0)), reads=["idx_i", "affdr"], writes=[gak])
                            for kc in range(8):
                                S.op("pe", lambda e, kc=kc, xg=xg: e.transpose(out=T0[:, kc * 128:(kc + 1) * 128], in_=xg[:, kc * 128:(kc + 1) * 128], identity=identb[:]), reads=[xgk, "identb"], writes=["T0"])
                            S.op("dve", lambda e, XgT=XgT, c=c: e.tensor_copy(out=XgT[:, :, c * 128:(c + 1) * 128], in_=T0[:, :].rearrange("p (k t) -> p k t", k=8)), reads=["T0"], writes=[XgTk])
                        for fc in range(4):
                            pa = 2 * (fc % 2); pu = pa + 1
                            for kc in range(8):
                                S.op("pe", lambda e, fc=fc, kc=kc, pa=pa, Wg=Wg, XgT=XgT: e.matmul(FB[pa][:, :], lhsT=Wg[:, kc, fc * 128:(fc + 1) * 128], rhs=XgT[:, kc, :], start=(kc == 0), stop=(kc == 7)), reads=[Wgk, XgTk], writes=[FK[pa]])
                            for kc in range(8):
                                S.op("pe", lambda e, fc=fc, kc=kc, pu=pu, Wu=Wu, XgT=XgT: e.matmul(FB[pu][:, :], lhsT=Wu[:, kc, fc * 128:(fc + 1) * 128], rhs=XgT[:, kc, :], start=(kc == 0), stop=(kc == 7)), reads=[Wuk, XgTk], writes=[FK[pu]])
                            sg, sgk = sgR.next()
                            S.op("act", lambda e, sg=sg, pa=pa: e.activation(out=sg[:, :], in_=FB[pa][:, :], func=AF.Silu), reads=[FK[pa]], writes=[sgk])
                            S.op("dve", lambda e, sg=sg, pu=pu, GT=GT, fc=fc: e.tensor_tensor(out=GT[:, fc, :], in0=sg[:, :], in1=FB[pu][:, :], op=ALU.mult), reads=[sgk, FK[pu]], writes=[GTk])
                        for st_ in range(4):
                            y, yk = yR.next(); ga, gak = gas[st_]
                            col = ee * 4 + st_
                            for half in range(2):
                                for fc in range(4):
                                    S.op("pe", lambda e, half=half, fc=fc, GT=GT, Wd=Wd, st_=st_: e.matmul(FB[4 + half][:, :], lhsT=GT[:, fc, st_ * 128:(st_ + 1) * 128], rhs=Wd[:, fc, half * 512:(half + 1) * 512], start=(fc == 0), stop=(fc == 3)), reads=[GTk, Wdk], writes=[FK[4 + half]])
                                S.op("act", lambda e, half=half, y=y, ga=ga, ee=ee: e.activation(out=y[:, half * 512:(half + 1) * 512], in_=FB[4 + half][:, :], func=AF.Copy, scale=ga[:, ee:ee + 1]), reads=[FK[4 + half], gak], writes=[yk])
                            S.dma("pool", lambda e, y=y, col=col: e.indirect_dma_start(out=xres[:, :], out_offset=bass.IndirectOffsetOnAxis(ap=idx_i[:, col:col + 1], axis=0), in_=y[:, :], in_offset=None, compute_op=ALU.add), reads=[yk, "idx_i"], writes=["xres"])
            S.barrier()
            return True

        x_src, xs_key = x_in, "x_in"
        final_norm = (stop_after is None)
        for l in range(nlayers):
            cont = layer(l, x_src, xs_key)
            x_src, xs_key = xres, "xres"
            if not cont:
                final_norm = False
                break
        with ExitStack() as pf:
            b = lambda name, shape, dt: sb(name, shape, dt, pf)
            xtR = Ring(b, "xtf", [128, D], F32, 2); yR = Ring(b, "yf", [128, D], F32, 2)
            scr = b("scrf", [128, D], F32); rsx = Ring(b, "rsf", [128, 2], F32, 2)
            gfin = b("gfin", [128, D], F32)
            S.dma("sp", lambda e: e.dma_start(out=gfin[:], in_=ln_final.partition_broadcast(128)), writes=["gfin"])
            for j in range(NT):
                xt, xk = xtR.next(); y, yk = yR.next(); rs, rsk = rsx.next()
                S.dma("sp", lambda e, xt=xt, j=j: e.dma_start(out=xt[:], in_=xres[j * 128:(j + 1) * 128, :]), reads=["xres"], writes=[xk])
                if final_norm:
                    norm_transpose(xt, xk, y, yk, None, None, 0, rs, rsk, scr, "scrf", gain_b=gfin, gbkey="gfin")
                    S.dma("sp", lambda e, y=y, j=j: e.dma_start(out=out[j * 128:(j + 1) * 128, :], in_=y[:]), reads=[yk], writes=["out"])
                else:
                    S.dma("sp", lambda e, xt=xt, j=j: e.dma_start(out=out[j * 128:(j + 1) * 128, :], in_=xt[:]), reads=[xk], writes=["out"])
        S.barrier(engines=["sp"])
        print("ninstr", S.ninstr, "nsem", S.nsem, {k: v for k, v in S.cnt.items()})
    return nc, dbg_out


def rope_tables():
    def ang(rot_dim):
        rows = S_LEN // 64
        row = np.repeat(np.arange(rows, dtype=np.float32), 64)
        col = np.tile(np.arange(64, dtype=np.float32), rows)
        axis_dim = rot_dim // 2
        inv = (10000.0 ** (-np.arange(0, axis_dim, 2, dtype=np.float32) / axis_dim)).astype(np.float32)
        a = np.concatenate([row[:, None] * inv[None, :], col[:, None] * inv[None, :]], axis=-1).astype(np.float32)
        return np.concatenate([np.cos(a), np.sin(a)], axis=-1).astype(np.float32)
    return ang(64), ang(32)


def make_in_maps(inputs, ncores=8):
    rg, rm = rope_tables()
    maps = []
    for c in range(ncores):
        m = {k: np.ascontiguousarray(np.asarray(v, dtype=np.float32)) for k, v in inputs.items() if k not in ("x", "mem")}
        m["x"] = np.ascontiguousarray(np.asarray(inputs["x"][c], dtype=np.float32))
        m["mem"] = np.ascontiguousarray(np.asarray(inputs["mem"][c], dtype=np.float32))
        m["ropeg"] = rg
        m["ropem"] = rm
        maps.append(m)
    return maps


def kernel(**inputs):
    nc, _ = build_nc()
    maps = make_in_maps(inputs, 8)
    res = run_bass_kernel_spmd(nc, maps, core_ids=list(range(8)))
    return np.stack([np.asarray(r["out"], dtype=np.float32) for r in res.results], axis=0)
```

```python
from contextlib import ExitStack
import numpy as np
import ml_dtypes
import concourse.bass as bass
import concourse.mybir as mybir
from concourse.bass_utils import run_bass_kernel_spmd

F32 = mybir.dt.float32
BF16 = mybir.dt.bfloat16
I32 = mybir.dt.int32
ALU = mybir.AluOpType
AF = mybir.ActivationFunctionType
AX = mybir.AxisListType

S_LEN = 4096
D = 1024
NT = 32
DEPTH = 4
EPS = 1e-6
IN_W = 1184
EPOCH = 30000
import os
ATT_LA = int(os.environ.get('ATT_LA', '3'))


class Sched:
    def __init__(self, nc, es, dma_slots=8):
        self.nc = nc
        self.es = es
        self.engs = {"pe": nc.tensor, "act": nc.scalar, "dve": nc.vector, "pool": nc.gpsimd, "sp": nc.sync}
        self.cnt = {k: 0 for k in self.engs}
        self.esems = {k: [] for k in self.engs}
        self.waited = {k: {} for k in self.engs}
        self.lastw = {}
        self.readers = {}
        self.dma_pool = {}
        self.dma_slots = dma_slots
        self.nsem = 0
        self.ninstr = 0

    def _newsem(self, name):
        self.nsem += 1
        return self.es.enter_context(self.nc.semaphore(name))

    def _eng_event(self, eng):
        c = self.cnt[eng]
        ep, loc = divmod(c, EPOCH)
        while len(self.esems[eng]) <= ep:
            self.esems[eng].append(self._newsem(f"e_{eng}_{len(self.esems[eng])}"))
        self.cnt[eng] = c + 1
        return (self.esems[eng][ep], loc + 1, eng)

    def _dma_event(self, q):
        st = self.dma_pool.setdefault(q, {"sems": [], "uses": [], "i": 0})
        if len(st["sems"]) < self.dma_slots:
            st["sems"].append(self._newsem(f"d_{q}_{len(st['sems'])}"))
            st["uses"].append(0)
        i = st["i"] % self.dma_slots
        st["i"] += 1
        sem = st["sems"][i]
        if st["uses"][i] > 0:
            self._wait(q, (sem, 16 * st["uses"][i], None))
        st["uses"][i] += 1
        return (sem, 16 * st["uses"][i], None)

    def _wait(self, eng, ev):
        sem, val, src = ev
        if src == eng and eng == "pe":
            return
        w = self.waited[eng]
        k = id(sem)
        if w.get(k, 0) >= val:
            return
        w[k] = val
        self.engs[eng].wait_ge(sem, val)
        self.ninstr += 1

    def _deps(self, eng, reads, writes):
        for k in reads:
            ev = self.lastw.get(k)
            if ev is not None:
                self._wait(eng, ev)
        for k in writes:
            ev = self.lastw.get(k)
            if ev is not None:
                self._wait(eng, ev)
            for ev in self.readers.get(k, {}).values():
                self._wait(eng, ev)

    def _record(self, ev, reads, writes):
        for k in writes:
            self.lastw[k] = ev
            self.readers[k] = {}
        for k in reads:
            if k in writes:
                continue
            self.readers.setdefault(k, {})[id(ev[0])] = ev

    def op(self, eng, fn, reads=(), writes=()):
        self._deps(eng, reads, writes)
        ev = self._eng_event(eng)
        fn(self.engs[eng]).then_inc(ev[0], 1)
        self._record(ev, reads, writes)
        self.ninstr += 1
        return ev

    def dma(self, q, fn, reads=(), writes=()):
        self._deps(q, reads, writes)
        ev = self._dma_event(q)
        fn(self.engs[q]).then_inc(ev[0], 16)
        self._record(ev, reads, writes)
        self.ninstr += 1
        return ev

    def all_events(self):
        evs = []
        for eng in self.engs:
            c = self.cnt[eng]
            if c > 0:
                ep, loc = divmod(c - 1, EPOCH)
                evs.append((self.esems[eng][ep], loc + 1, eng))
        for q, st in self.dma_pool.items():
            for sem, u in zip(st["sems"], st["uses"]):
                if u:
                    evs.append((sem, 16 * u, None))
        return evs

    def barrier(self, engines=None):
        evs = self.all_events()
        for eng in (engines or list(self.engs)):
            for ev in evs:
                if ev[2] == eng:
                    continue
                self._wait(eng, ev)
        if engines is None:
            self.lastw = {}
            self.readers = {}


class Ring:
    def __init__(self, alloc, name, shape, dt, n):
        self.items = [(alloc(f"{name}{i}", shape, dt), f"{name}{i}") for i in range(n)]
        self.i = 0

    def next(self):
        it = self.items[self.i % len(self.items)]
        self.i += 1
        return it


def build_nc(nlayers=DEPTH, dbg=None, stop_after=None):
    dbg = dbg or {}
    nc = bass.Bass("TRN2", target_bir_lowering=False)
    L = DEPTH

    def din(name, shape, dt=F32):
        return nc.dram_tensor(name, list(shape), dt, kind="ExternalInput").ap()

    def dscr(name, shape, dt):
        return nc.dram_tensor(name, list(shape), dt, kind="Internal").ap()

    x_in = din("x", [S_LEN, D])
    mem_in = din("mem", [256, D])
    ln_mix = din("ln_mix", [L, D]); w_in = din("w_in", [L, D, IN_W])
    gqa_q_norm = din("gqa_q_norm", [L, 64]); gqa_k_norm = din("gqa_k_norm", [L, 64])
    mla_q_norm = din("mla_q_norm", [L, 256]); mla_kv_norm = din("mla_kv_norm", [L, 128])
    w_q_b = din("w_q_b", [L, 256, 768]); w_kv_b = din("w_kv_b", [L, 128, 1024])
    out_norm_gqa = din("out_norm_gqa", [L, 512]); out_norm_mla = din("out_norm_mla", [L, 512])
    w_o = din("w_o", [L, 1024, 1024])
    ln_mem = din("ln_mem", [L, D]); ln_mem_kv = din("ln_mem_kv", [L, D])
    w_mem_q = din("w_mem_q", [L, D, 512]); w_mem_kv = din("w_mem_kv", [L, D, 1024]); w_mem_o = din("w_mem_o", [L, 512, D])
    ln_ffn = din("ln_ffn", [L, D]); w_router = din("w_router", [L, D, 16])
    w_gate = din("w_gate", [L, 16, D, 512]); w_up = din("w_up", [L, 16, D, 512]); w_down = din("w_down", [L, 16, 512, D])
    ln_final = din("ln_final", [D])
    ropeg = din("ropeg", [S_LEN, 64])
    ropem = din("ropem", [S_LEN, 32])
    out = nc.dram_tensor("out", [S_LEN, D], F32, kind="ExternalOutput").ap()

    xres = dscr("xres", [S_LEN, D], F32)
    QgT = dscr("QgT", [128, 4, S_LEN], BF16)
    KgT = dscr("KgT", [128, 2, S_LEN], BF16)
    QmT = dscr("QmT", [96, 8, S_LEN], BF16)
    KmT = dscr("KmT", [96, 8, S_LEN], BF16)
    Odr = dscr("Odr", [S_LEN, 1024], F32)
    hdr = dscr("hdr", [S_LEN, D], BF16)
    affdr = dscr("affdr", [S_LEN, 16], F32)

    dbg_out = {}

    def dbgt(name, shape, dt=F32):
        t = nc.dram_tensor("dbg_" + name, list(shape), dt, kind="ExternalOutput").ap()
        dbg_out[name] = t
        return t

    with ExitStack() as es:
        S = Sched(nc, es)

        uid = [0]

        def sb(name, shape, dt, stack=es):
            uid[0] += 1
            return stack.enter_context(nc.sbuf_tensor(f"{name}_{uid[0]}", list(shape), dt))

        def ps(name, shape, dt, stack=es):
            return stack.enter_context(nc.psum_tensor(name, list(shape), dt))

        T0 = ps("T0", [128, 1024], BF16); T1 = ps("T1", [128, 1024], BF16)
        FB = [ps(f"F{i}", [128, 512], F32) for i in range(6)]
        FK = [f"F{i}" for i in range(6)]

        identb = sb("identb", [128, 128], BF16)
        identf = sb("identf", [128, 128], F32)
        for t_, k_ in ((identb, "identb"), (identf, "identf")):
            S.op("pool", lambda e, t_=t_: e.memset(t_[:], 0.0), writes=[k_])
            S.op("pool", lambda e, t_=t_: e.affine_select(out=t_[:], in_=t_[:], pattern=[[-1, 128]], compare_op=ALU.not_equal, fill=1.0, base=0, channel_multiplier=1), reads=[k_], writes=[k_])
        neghalf = sb("neghalf", [128, 16], F32)
        S.op("pool", lambda e: e.memset(neghalf[:], -0.5), writes=["neghalf"])
        ropeg_sb = sb("ropeg_sb", [128, NT, 64], F32)
        ropem_sb = sb("ropem_sb", [128, NT, 32], F32)
        S.dma("sp", lambda e: e.dma_start(out=ropeg_sb[:], in_=ropeg.rearrange("(j p) c -> p j c", p=128)), writes=["ropeg"])
        S.dma("sp", lambda e: e.dma_start(out=ropem_sb[:], in_=ropem.rearrange("(j p) c -> p j c", p=128)), writes=["ropem"])
        growA = sb("growA", [96, 128], F32); growB = sb("growB", [44, 128], F32)
        gcA = sb("gcA", [128, 96], F32); gcB = sb("gcB", [128, 44], F32)
        for r0, src in ((0, ln_mix), (32, ln_mem), (64, ln_mem_kv)):
            S.dma("sp", lambda e, r0=r0, src=src: e.dma_start(out=growA[r0:r0 + 32, :], in_=src.rearrange("l (kc p) -> (l kc) p", p=128)), writes=["growA"])
        for r0, n_, src in ((0, 8, mla_q_norm), (8, 4, mla_kv_norm), (12, 16, out_norm_gqa), (28, 16, out_norm_mla)):
            S.dma("sp", lambda e, r0=r0, n_=n_, src=src: e.dma_start(out=growB[r0:r0 + n_, :], in_=src.rearrange("l (kc p) -> (l kc) p", p=128)), writes=["growB"])
        S.op("pe", lambda e: e.transpose(out=FB[0][:, 0:96], in_=growA[:, :], identity=identf[0:96, 0:96]), reads=["growA", "identf"], writes=["F0"])
        S.op("pe", lambda e: e.transpose(out=FB[1][:, 0:44], in_=growB[:, :], identity=identf[0:44, 0:44]), reads=["growB", "identf"], writes=["F1"])
        S.op("dve", lambda e: e.tensor_copy(out=gcA[:, :], in_=FB[0][:, 0:96]), reads=["F0"], writes=["gcA"])
        S.op("dve", lambda e: e.tensor_copy(out=gcB[:, :], in_=FB[1][:, 0:44]), reads=["F1"], writes=["gcB"])
        gcAv = gcA[:, :].rearrange("p (g l k) -> p g l k", g=3, l=L)
        gc_mix, gc_mem, gc_memkv = gcAv[:, 0], gcAv[:, 1], gcAv[:, 2]
        gc_q = gcB[:, 0:8].rearrange("p (l k) -> p l k", l=L); gc_kv = gcB[:, 8:12].rearrange("p (l k) -> p l k", l=L)
        gc_og = gcB[:, 12:28].rearrange("p (l k) -> p l k", l=L); gc_om = gcB[:, 28:44].rearrange("p (l k) -> p l k", l=L)
        invn = sb("invn", [128, 12], F32)
        S.op("pool", lambda e: e.memset(invn[:, 0:10], 1.0 / 64), writes=["invn"])
        S.op("pool", lambda e: e.memset(invn[:, 10:11], 1.0 / 256), writes=["invn"])
        S.op("pool", lambda e: e.memset(invn[:, 11:12], 1.0 / 128), writes=["invn"])

        def rstd_from_ss(ss_ap, n, out_ap, key_in, key_out, ncols=1):
            S.op("dve", lambda e: e.tensor_scalar(out=out_ap, in0=ss_ap, scalar1=1.0 / n, scalar2=EPS, op0=ALU.mult, op1=ALU.add), reads=[key_in], writes=[key_out])
            S.op("pool", lambda e: e.tensor_tensor(out=out_ap, in0=out_ap, in1=neghalf[:, 0:ncols], op=ALU.pow), reads=[key_out, "neghalf"], writes=[key_out])

        wstage = Ring(sb, "wst", [128, IN_W], F32, 3)

        cast_rr = [0]

        def load_weight(dst, dkey, src2d, KC, N, gain=None, gkey=None, q="sp", engs=("pool",)):
            for kc in range(KC):
                n0 = 0
                while n0 < N:
                    nn = min(IN_W, N - n0)
                    st, sk = wstage.next()
                    S.dma(q, lambda e, st=st, kc=kc, n0=n0, nn=nn: e.dma_start(out=st[:, 0:nn], in_=src2d[kc * 128:(kc + 1) * 128, n0:n0 + nn]), writes=[sk])
                    eng = engs[cast_rr[0] % len(engs)]
                    cast_rr[0] += 1
                    rd = [sk] + ([gkey] if gain is not None else [])
                    if eng == "act":
                        if gain is not None:
                            S.op("act", lambda e, st=st, kc=kc, n0=n0, nn=nn: e.activation(out=dst[:, kc, n0:n0 + nn], in_=st[:, 0:nn], func=AF.Copy, scale=gain[:, kc:kc + 1]), reads=rd, writes=[dkey])
                        else:
                            S.op("act", lambda e, st=st, kc=kc, n0=n0, nn=nn: e.activation(out=dst[:, kc, n0:n0 + nn], in_=st[:, 0:nn], func=AF.Copy), reads=rd, writes=[dkey])
                    elif gain is not None:
                        S.op(eng, lambda e, st=st, kc=kc, n0=n0, nn=nn: e.tensor_scalar(out=dst[:, kc, n0:n0 + nn], in0=st[:, 0:nn], scalar1=gain[:, kc:kc + 1], scalar2=None, op0=ALU.mult), reads=rd, writes=[dkey])
                    else:
                        S.op(eng, lambda e, st=st, kc=kc, n0=n0, nn=nn: e.tensor_copy(out=dst[:, kc, n0:n0 + nn], in_=st[:, 0:nn]), reads=rd, writes=[dkey])
                    n0 += nn

        def norm_transpose(xt, xkey, hn, hkey, XTdst, XTkey, tcol, rstd, rkey, scr, scrkey, gain_b=None, gbkey=None, halves=1):
            w = D // halves
            for hf in range(halves):
                S.op("dve", lambda e, hf=hf: e.scalar_tensor_tensor(out=scr[:, hf * w:(hf + 1) * w], in0=xt[:, hf * w:(hf + 1) * w], scalar=1.0, in1=xt[:, hf * w:(hf + 1) * w], op0=ALU.mult, op1=ALU.mult, accum_out=rstd[:, hf:hf + 1]),
                     reads=[xkey], writes=[scrkey, rkey])
            rstd_from_ss(rstd[:, 0:halves], w, rstd[:, 0:halves], rkey, rkey, ncols=halves)
            for hf in range(halves):
                if gain_b is None:
                    S.op("dve", lambda e, hf=hf: e.tensor_scalar(out=hn[:, hf * w:(hf + 1) * w], in0=xt[:, hf * w:(hf + 1) * w], scalar1=rstd[:, hf:hf + 1], scalar2=None, op0=ALU.mult), reads=[xkey, rkey], writes=[hkey])
                else:
                    S.op("dve", lambda e, hf=hf: e.scalar_tensor_tensor(out=hn[:, hf * w:(hf + 1) * w], in0=xt[:, hf * w:(hf + 1) * w], scalar=rstd[:, hf:hf + 1], in1=gain_b[:, hf * w:(hf + 1) * w], op0=ALU.mult, op1=ALU.mult), reads=[xkey, rkey, gbkey], writes=[hkey])
            if XTdst is None:
                return
            for kc in range(8):
                S.op("pe", lambda e, kc=kc: e.transpose(out=T0[:, kc * 128:(kc + 1) * 128], in_=hn[:, kc * 128:(kc + 1) * 128], identity=identb[:]), reads=[hkey, "identb"], writes=["T0"])
            S.op("dve", lambda e: e.tensor_copy(out=XTdst[:, :, tcol:tcol + 128], in_=T0[:, :].rearrange("p (k t) -> p k t", k=8)), reads=["T0"], writes=[XTkey])

        def attn_steps(QT, qkey, KT, kkey, nk_tiles, Vfn, vkey, dv, scale, bias, out_cb, Sring, PTring, Oring_sets, nqc=8):
            per_bank = min(512 // (dv + 1), 4)
            steps = []
            for qc in range(nqc):
                oset = Oring_sets[qc % len(Oring_sets)]
                for kt in range(nk_tiles):
                    st = {}

                    def fS(st=st, kt=kt, qc=qc):
                        sp_, skey = Sring.next()
                        st["s"] = (sp_, skey)
                        S.op("pe", lambda e: e.matmul(sp_[:, :], lhsT=KT[:, kt * 128:(kt + 1) * 128], rhs=QT[:, qc * 512:(qc + 1) * 512], start=True, stop=True), reads=[qkey, kkey], writes=[skey])

                    def fE(st=st):
                        sp_, skey = st["s"]
                        pt_, pkey = PTring.next()
                        st["p"] = (pt_, pkey)
                        if bias is None:
                            S.op("act", lambda e: e.activation(out=pt_[:, :], in_=sp_[:, :], func=AF.Exp, scale=scale), reads=[skey], writes=[pkey])
                        else:
                            S.op("act", lambda e: e.activation(out=pt_[:, :], in_=sp_[:, :], func=AF.Exp, scale=scale, bias=bias[0]), reads=[skey, bias[1]], writes=[pkey])

                    def fPV(st=st, kt=kt, oset=oset):
                        pt_, pkey = st["p"]
                        for qs in range(4):
                            ob, okey = oset[qs // per_bank]
                            o0 = (qs % per_bank) * (dv + 1)
                            st_flag = (kt == 0 and (qs % per_bank) == 0)
                            S.op("pe", lambda e, ob=ob, o0=o0, qs=qs, st_flag=st_flag: e.matmul(ob[:, o0:o0 + dv + 1], lhsT=pt_[:, qs * 128:(qs + 1) * 128], rhs=Vfn(kt), start=st_flag, stop=(kt == nk_tiles - 1), skip_group_check=True),
                                 reads=[pkey, vkey], writes=[okey])

                    post = (lambda qc=qc, oset=oset: out_cb(qc, oset, per_bank)) if kt == nk_tiles - 1 else None
                    steps.append({"S": fS, "E": fE, "PV": fPV, "post": post, "pre": None})
            return steps

        def run_steps(steps, LA):
            n = len(steps)
            for i in range(n + LA):
                if i < n:
                    if steps[i]["pre"] is not None:
                        steps[i]["pre"]()
                    steps[i]["S"]()
                m = i - LA
                if m >= 0:
                    steps[m]["E"]()
                    steps[m]["PV"]()
                    if steps[m]["post"] is not None:
                        steps[m]["post"]()

        def attention(*args, LA=1, **kw):
            run_steps(attn_steps(*args, **kw), LA)

        def layer(l, x_src, xs_key):
            with ExitStack() as ph:
                a = lambda name, shape, dt: sb(name, shape, dt, ph)
                Win = a("Win", [128, 8, IN_W], BF16); Wqb = a("Wqb", [128, 2, 768], BF16); Wkvb = a("Wkvb", [128, 1, 1024], BF16)
                load_weight(Win, "Win", w_in[l], 8, IN_W, gc_mix[:, l, :], "gcA")
                load_weight(Wqb, "Wqb", w_q_b[l], 2, 768, gc_q[:, l, :], "gcB")
                load_weight(Wkvb, "Wkvb", w_kv_b[l], 1, 1024, gc_kv[:, l, :], "gcB")
                gq = a("gq", [128, 64], F32); gk = a("gk", [128, 64], F32)
                S.dma("sp", lambda e: e.dma_start(out=gq[:], in_=gqa_q_norm[l, :].partition_broadcast(128)), writes=["gq"])
                S.dma("sp", lambda e: e.dma_start(out=gk[:], in_=gqa_k_norm[l, :].partition_broadcast(128)), writes=["gk"])
                Vg = a("Vg", [128, NT, 2, 65], BF16); Vm = a("Vm", [128, NT, 8, 65], BF16)
                S.op("pool", lambda e: e.memset(Vg[:, :, :, 64:65], 1.0), writes=["Vg"])
                S.op("pool", lambda e: e.memset(Vm[:, :, :, 64:65], 1.0), writes=["Vm"])
                with ExitStack() as p1:
                    b = lambda name, shape, dt: sb(name, shape, dt, p1)
                    xtR = Ring(b, "xt", [128, D], F32, 2); hnR = Ring(b, "hn", [128, D], BF16, 2)
                    XTR = Ring(b, "XT", [128, 8, 128], BF16, 2)
                    scr = b("scr", [128, D], F32)
                    prR = Ring(b, "pr", [128, IN_W], F32, 2)
                    stR = Ring(b, "st", [128, 12], F32, 2)
                    rsx = Ring(b, "rsx", [128, 2], F32, 2)
                    qn = b("qn", [128, 10, 64], F32); tmpa = b("tmpa", [128, 10, 32], F32); tmpb = b("tmpb", [128, 10, 32], F32)
                    rot_f = b("rot", [128, 640], BF16); kdup_f = b("kdup", [128, 256], BF16)
                    rot = rot_f[:, :].rearrange("p (h d) -> p h d", h=10); kdup = kdup_f[:, :].rearrange("p (g u d) -> p g u d", g=2, u=2)
                    cb = b("cb", [128, 384], BF16); cT = b("cT", [128, 3, 128], BF16)
                    qm32_f = b("qm32", [128, 768], F32); qmb_f = b("qmb", [128, 768], BF16); km_f = b("km", [128, 768], BF16)
                    qm32 = qm32_f[:, :].rearrange("p (h d) -> p h d", h=8); qmb = qmb_f[:, :].rearrange("p (h d) -> p h d", h=8); km = km_f[:, :].rearrange("p (h d) -> p h d", h=8)
                    krot = b("krot", [128, 32], F32)
                    QKst = Ring(b, "QKst", [128, 6, 512], BF16, 2)
                    QmSt = Ring(b, "QmSt", [128, 8, 512], BF16, 1); KmSt = Ring(b, "KmSt", [128, 8, 512], BF16, 1)
                    for c in range(8):
                        qkst, qkk = QKst.next(); qmst, qmk = QmSt.next(); kmst, kmk = KmSt.next()
                        for t in range(4):
                            j = c * 4 + t
                            xt, xk = xtR.next(); hn, hk = hnR.next(); XT, XTk = XTR.next(); pr, prk = prR.next(); st, stk = stR.next(); rs, rsk = rsx.next()
                            S.dma("sp", lambda e, xt=xt, j=j: e.dma_start(out=xt[:], in_=x_src[j * 128:(j + 1) * 128, :]), reads=[xs_key], writes=[xk])
                            norm_transpose(xt, xk, hn, hk, XT, XTk, 0, rs, rsk, scr, "scr")
                            for nb, (n0, nn) in enumerate(((0, 512), (512, 512), (1024, 160))):
                                for kc in range(8):
                                    S.op("pe", lambda e, nb=nb, n0=n0, nn=nn, kc=kc, XT=XT: e.matmul(FB[nb][:, 0:nn], lhsT=XT[:, kc, :], rhs=Win[:, kc, n0:n0 + nn], start=(kc == 0), stop=(kc == 7)), reads=[XTk, "Win"], writes=[FK[nb]])
                                S.op("act", lambda e, nb=nb, n0=n0, nn=nn, pr=pr: e.activation(out=pr[:, n0:n0 + nn], in_=FB[nb][:, 0:nn], func=AF.Copy), reads=[FK[nb]], writes=[prk])
                            S.op("dve", lambda e, pr=pr: e.tensor_tensor(out=scr[:, 0:640], in0=pr[:, 0:640], in1=pr[:, 0:640], op=ALU.mult), reads=[prk], writes=["scr"])
                            S.op("dve", lambda e, st=st: e.tensor_reduce(out=st[:, 0:10], in_=scr[:, 0:640].rearrange("p (h d) -> p h d", h=10), axis=AX.X, op=ALU.add), reads=["scr"], writes=[stk])
                            S.op("dve", lambda e, pr=pr, st=st: e.scalar_tensor_tensor(out=scr[:, 0:256], in0=pr[:, 768:1024], scalar=1.0, in1=pr[:, 768:1024], op0=ALU.mult, op1=ALU.mult, accum_out=st[:, 10:11]), reads=[prk], writes=["scr", stk])
                            S.op("dve", lambda e, pr=pr, st=st: e.scalar_tensor_tensor(out=scr[:, 0:128], in0=pr[:, 1024:1152], scalar=1.0, in1=pr[:, 1024:1152], op0=ALU.mult, op1=ALU.mult, accum_out=st[:, 11:12]), reads=[prk], writes=["scr", stk])
                            S.op("dve", lambda e, st=st: e.tensor_tensor(out=st[:, :], in0=st[:, :], in1=invn[:, :], op=ALU.mult), reads=[stk, "invn"], writes=[stk])
                            S.op("dve", lambda e, st=st: e.tensor_scalar(out=st[:, :], in0=st[:, :], scalar1=EPS, scalar2=None, op0=ALU.add), reads=[stk], writes=[stk])
                            S.op("pool", lambda e, st=st: e.tensor_tensor(out=st[:, :], in0=st[:, :], in1=neghalf[:, 0:12], op=ALU.pow), reads=[stk, "neghalf"], writes=[stk])
                            prqk = pr[:, 0:640].rearrange("p (h d) -> p h d", h=10)
                            S.op("dve", lambda e, st=st, prqk=prqk: e.tensor_tensor(out=qn[:, :, :], in0=prqk, in1=st[:, 0:10].unsqueeze(2).to_broadcast([128, 10, 64]), op=ALU.mult), reads=[prk, stk], writes=["qn"])
                            S.op("dve", lambda e: e.tensor_tensor(out=qn[:, 0:8, :], in0=qn[:, 0:8, :], in1=gq[:, :].unsqueeze(1).to_broadcast([128, 8, 64]), op=ALU.mult), reads=["qn", "gq"], writes=["qn"])
                            S.op("dve", lambda e: e.tensor_tensor(out=qn[:, 8:10, :], in0=qn[:, 8:10, :], in1=gk[:, :].unsqueeze(1).to_broadcast([128, 2, 64]), op=ALU.mult), reads=["qn", "gk"], writes=["qn"])
                            cg = ropeg_sb[:, j, 0:32].unsqueeze(1).to_broadcast([128, 10, 32]); sg = ropeg_sb[:, j, 32:64].unsqueeze(1).to_broadcast([128, 10, 32])
                            S.op("dve", lambda e, cg=cg: e.tensor_tensor(out=tmpa[:, :, :], in0=qn[:, :, 0:32], in1=cg, op=ALU.mult), reads=["qn", "ropeg"], writes=["tmpa"])
                            S.op("dve", lambda e, sg=sg: e.tensor_tensor(out=tmpb[:, :, :], in0=qn[:, :, 32:64], in1=sg, op=ALU.mult), reads=["qn", "ropeg"], writes=["tmpb"])
                            S.op("dve", lambda e: e.tensor_tensor(out=rot[:, :, 0:32], in0=tmpa[:, :, :], in1=tmpb[:, :, :], op=ALU.subtract), reads=["tmpa", "tmpb"], writes=["rot"])
                            S.op("dve", lambda e, cg=cg: e.tensor_tensor(out=tmpa[:, :, :], in0=qn[:, :, 32:64], in1=cg, op=ALU.mult), reads=["qn", "ropeg"], writes=["tmpa"])
                            S.op("dve", lambda e, sg=sg: e.tensor_tensor(out=tmpb[:, :, :], in0=qn[:, :, 0:32], in1=sg, op=ALU.mult), reads=["qn", "ropeg"], writes=["tmpb"])
                            S.op("dve", lambda e: e.tensor_tensor(out=rot[:, :, 32:64], in0=tmpa[:, :, :], in1=tmpb[:, :, :], op=ALU.add), reads=["tmpa", "tmpb"], writes=["rot"])
                            S.op("dve", lambda e: e.tensor_copy(out=kdup[:, :, :, :], in_=rot[:, 8:10, :].unsqueeze(2).to_broadcast([128, 2, 2, 64])), reads=["rot"], writes=["kdup"])
                            for p_ in range(4):
                                S.op("pe", lambda e, p_=p_: e.transpose(out=T1[:, p_ * 128:(p_ + 1) * 128], in_=rot_f[:, p_ * 128:(p_ + 1) * 128], identity=identb[:]), reads=["rot", "identb"], writes=["T1"])
                            for g_ in range(2):
                                S.op("pe", lambda e, g_=g_: e.transpose(out=T1[:, (4 + g_) * 128:(5 + g_) * 128], in_=kdup_f[:, g_ * 128:(g_ + 1) * 128], identity=identb[:]), reads=["kdup", "identb"], writes=["T1"])
                            S.op("dve", lambda e, qkst=qkst, t=t: e.tensor_copy(out=qkst[:, :, t * 128:(t + 1) * 128], in_=T1[:, 0:768].rearrange("p (k t) -> p k t", k=6)), reads=["T1"], writes=[qkk])
                            S.op("act", lambda e, pr=pr, j=j: e.activation(out=Vg[:, j, :, 0:64], in_=pr[:, 640:768].rearrange("p (g d) -> p g d", g=2), func=AF.Copy), reads=[prk], writes=["Vg"])
                            S.op("dve", lambda e, pr=pr, st=st: e.tensor_scalar(out=cb[:, 0:256], in0=pr[:, 768:1024], scalar1=st[:, 10:11], scalar2=None, op0=ALU.mult), reads=[prk, stk], writes=["cb"])
                            S.op("dve", lambda e, pr=pr, st=st: e.tensor_scalar(out=cb[:, 256:384], in0=pr[:, 1024:1152], scalar1=st[:, 11:12], scalar2=None, op0=ALU.mult), reads=[prk, stk], writes=["cb"])
                            for i_ in range(3):
                                S.op("pe", lambda e, i_=i_: e.transpose(out=T1[:, i_ * 128:(i_ + 1) * 128], in_=cb[:, i_ * 128:(i_ + 1) * 128], identity=identb[:]), reads=["cb", "identb"], writes=["T1"])
                            S.op("dve", lambda e: e.tensor_copy(out=cT[:, :, :], in_=T1[:, 0:384].rearrange("p (k t) -> p k t", k=3)), reads=["T1"], writes=["cT"])
                            for nb, (n0, nn) in ((3, (0, 512)), (4, (512, 256))):
                                for kc in range(2):
                                    S.op("pe", lambda e, nb=nb, n0=n0, nn=nn, kc=kc: e.matmul(FB[nb][:, 0:nn], lhsT=cT[:, kc, :], rhs=Wqb[:, kc, n0:n0 + nn], start=(kc == 0), stop=(kc == 1)), reads=["cT", "Wqb"], writes=[FK[nb]])
                            qm32f = qm32_f
                            S.op("act", lambda e: e.activation(out=qm32f[:, 0:512], in_=FB[3][:, 0:512], func=AF.Copy), reads=[FK[3]], writes=["qm32"])
                            S.op("act", lambda e: e.activation(out=qm32f[:, 512:768], in_=FB[4][:, 0:256], func=AF.Copy), reads=[FK[4]], writes=["qm32"])
                            for nb in range(2):
                                S.op("pe", lambda e, nb=nb: e.matmul(FB[nb][:, 0:512], lhsT=cT[:, 2, :], rhs=Wkvb[:, 0, nb * 512:(nb + 1) * 512], start=True, stop=True), reads=["cT", "Wkvb"], writes=[FK[nb]])
                                kvv = FB[nb][:, 0:512].rearrange("p (h d) -> p h d", h=4)
                                S.op("act", lambda e, kvv=kvv, nb=nb, j=j: e.activation(out=Vm[:, j, nb * 4:(nb + 1) * 4, 0:64], in_=kvv[:, :, 64:128], func=AF.Copy), reads=[FK[nb]], writes=["Vm"])
                                S.op("dve", lambda e, kvv=kvv, nb=nb: e.tensor_copy(out=km[:, nb * 4:(nb + 1) * 4, 0:64], in_=kvv[:, :, 0:64]), reads=[FK[nb]], writes=["km"])
                            cm = ropem_sb[:, j, 0:16]; sm = ropem_sb[:, j, 16:32]
                            cm8 = cm.unsqueeze(1).to_broadcast([128, 8, 16]); sm8 = sm.unsqueeze(1).to_broadcast([128, 8, 16])
                            ta = tmpa[:, 0:8, 0:16]; tb = tmpb[:, 0:8, 0:16]
                            S.op("dve", lambda e: e.tensor_copy(out=qmb[:, :, 0:64], in_=qm32[:, :, 0:64]), reads=["qm32"], writes=["qmb"])
                            S.op("dve", lambda e, cm8=cm8, ta=ta: e.tensor_tensor(out=ta, in0=qm32[:, :, 64:80], in1=cm8, op=ALU.mult), reads=["qm32", "ropem"], writes=["tmpa"])
                            S.op("dve", lambda e, sm8=sm8, tb=tb: e.tensor_tensor(out=tb, in0=qm32[:, :, 80:96], in1=sm8, op=ALU.mult), reads=["qm32", "ropem"], writes=["tmpb"])
                            S.op("dve", lambda e, ta=ta, tb=tb: e.tensor_tensor(out=qmb[:, :, 64:80], in0=ta, in1=tb, op=ALU.subtract), reads=["tmpa", "tmpb"], writes=["qmb"])
                            S.op("dve", lambda e, cm8=cm8, ta=ta: e.tensor_tensor(out=ta, in0=qm32[:, :, 80:96], in1=cm8, op=ALU.mult), reads=["qm32", "ropem"], writes=["tmpa"])
                            S.op("dve", lambda e, sm8=sm8, tb=tb: e.tensor_tensor(out=tb, in0=qm32[:, :, 64:80], in1=sm8, op=ALU.mult), reads=["qm32", "ropem"], writes=["tmpb"])
                            S.op("dve", lambda e, ta=ta, tb=tb: e.tensor_tensor(out=qmb[:, :, 80:96], in0=ta, in1=tb, op=ALU.add), reads=["tmpa", "tmpb"], writes=["qmb"])
                            ta1 = tmpa[:, 8, 0:16]; tb1 = tmpb[:, 8, 0:16]
                            S.op("dve", lambda e, pr=pr, cm=cm, ta1=ta1: e.tensor_tensor(out=ta1, in0=pr[:, 1152:1168], in1=cm, op=ALU.mult), reads=[prk, "ropem"], writes=["tmpa"])
                            S.op("dve", lambda e, pr=pr, sm=sm, tb1=tb1: e.tensor_tensor(out=tb1, in0=pr[:, 1168:1184], in1=sm, op=ALU.mult), reads=[prk, "ropem"], writes=["tmpb"])
                            S.op("dve", lambda e, ta1=ta1, tb1=tb1: e.tensor_tensor(out=krot[:, 0:16], in0=ta1, in1=tb1, op=ALU.subtract), reads=["tmpa", "tmpb"], writes=["krot"])
                            S.op("dve", lambda e, pr=pr, cm=cm, ta1=ta1: e.tensor_tensor(out=ta1, in0=pr[:, 1168:1184], in1=cm, op=ALU.mult), reads=[prk, "ropem"], writes=["tmpa"])
                            S.op("dve", lambda e, pr=pr, sm=sm, tb1=tb1: e.tensor_tensor(out=tb1, in0=pr[:, 1152:1168], in1=sm, op=ALU.mult), reads=[prk, "ropem"], writes=["tmpb"])
                            S.op("dve", lambda e, ta1=ta1, tb1=tb1: e.tensor_tensor(out=krot[:, 16:32], in0=ta1, in1=tb1, op=ALU.add), reads=["tmpa", "tmpb"], writes=["krot"])
                            S.op("dve", lambda e: e.tensor_copy(out=km[:, :, 64:96], in_=krot[:, :].unsqueeze(1).to_broadcast([128, 8, 32])), reads=["krot"], writes=["km"])
                            for h in range(8):
                                S.op("pe", lambda e, h=h: e.transpose(out=T0[0:96, h * 128:(h + 1) * 128], in_=qmb_f[:, h * 96:(h + 1) * 96], identity=identb[:]), reads=["qmb", "identb"], writes=["T0"])
                            S.op("dve", lambda e, qmst=qmst, t=t: e.tensor_copy(out=qmst[0:96, :, t * 128:(t + 1) * 128], in_=T0[0:96, :].rearrange("p (k t) -> p k t", k=8)), reads=["T0"], writes=[qmk])
                            for h in range(8):
                                S.op("pe", lambda e, h=h: e.transpose(out=T1[0:96, h * 128:(h + 1) * 128], in_=km_f[:, h * 96:(h + 1) * 96], identity=identb[:]), reads=["km", "identb"], writes=["T1"])
                            S.op("dve", lambda e, kmst=kmst, t=t: e.tensor_copy(out=kmst[0:96, :, t * 128:(t + 1) * 128], in_=T1[0:96, :].rearrange("p (k t) -> p k t", k=8)), reads=["T1"], writes=[kmk])
                        csl = slice(c * 512, (c + 1) * 512)
                        S.dma("sp", lambda e, qkst=qkst, csl=csl: e.dma_start(out=QgT[:, :, csl], in_=qkst[:, 0:4, :]), reads=[qkk], writes=["QgT"])
                        S.dma("sp", lambda e, qkst=qkst, csl=csl: e.dma_start(out=KgT[:, :, csl], in_=qkst[:, 4:6, :]), reads=[qkk], writes=["KgT"])
                        S.dma("sp", lambda e, qmst=qmst, csl=csl: e.dma_start(out=QmT[:, :, csl], in_=qmst[0:96, :, :]), reads=[qmk], writes=["QmT"])
                        S.dma("sp", lambda e, kmst=kmst, csl=csl: e.dma_start(out=KmT[:, :, csl], in_=kmst[0:96, :, :]), reads=[kmk], writes=["KmT"])
                S.barrier()
                if stop_after == "P1":
                    for nm, t_ in (("QgT", QgT), ("KgT", KgT), ("QmT", QmT), ("KmT", KmT)):
                        d_ = dbgt(nm, t_.shape, BF16)
                        S.dma("sp", lambda e, d_=d_, t_=t_: e.dma_start(out=d_, in_=t_), reads=[nm], writes=["dbg"])
                    return False
                with ExitStack() as p2:
                    b = lambda name, shape, dt: sb(name, shape, dt, p2)
                    QTR = Ring(b, "QTb", [128, S_LEN], BF16, 2); KTR = Ring(b, "KTb", [128, S_LEN], BF16, 2)
                    PTR = Ring(b, "PT", [128, 512], BF16, 4)
                    OoR = Ring(b, "Oo", [128, 4, 64], F32, 2); rcR = Ring(b, "rc", [128, 4], F32, 2)
                    Sring = Ring(lambda n, s, d: FB[(0, 1, 2, 5)[int(n[-1])]], "SR", None, None, 4)
                    Sring.items = [(FB[i], FK[i]) for i in (0, 1, 2, 5)]
                    Osets = [[(FB[3], "F3")], [(FB[4], "F4")]]

                    def make_cb(hcol):
                        def cbk(qc, oset, per_bank):
                            ob, okey = oset[0]
                            ov = ob[:, 0:260].rearrange("p (q d) -> p q d", q=4)
                            rc, rck = rcR.next(); Oo, Ook = OoR.next()
                            S.op("dve", lambda e: e.reciprocal(out=rc[:, :], in_=ov[:, :, 64]), reads=[okey], writes=[rck])
                            S.op("dve", lambda e: e.tensor_tensor(out=Oo[:, :, :], in0=ov[:, :, 0:64], in1=rc[:, :].unsqueeze(2).to_broadcast([128, 4, 64]), op=ALU.mult), reads=[okey, rck], writes=[Ook])
                            S.dma("sp", lambda e: e.dma_start(out=Odr[qc * 512:(qc + 1) * 512, hcol:hcol + 64].rearrange("(q p) d -> p q d", p=128), in_=Oo[:, :, :]), reads=[Ook], writes=["Odr"])
                        return cbk

                    heads = []
                    for pr_ in range(4):
                        g_ = pr_ // 2

                        def ld(pr_=pr_, g_=g_):
                            QTb, qk_ = QTR.next(); KTb, kk_ = KTR.next()
                            S.dma("sp", lambda e: e.dma_start(out=QTb[:, :], in_=QgT[:, pr_, :]), reads=["QgT"], writes=[qk_])
                            S.dma("sp", lambda e: e.dma_start(out=KTb[:, :], in_=KgT[:, g_, :]), reads=["KgT"], writes=[kk_])
                            return (QTb, qk_, KTb, kk_)

                        def mk(bufs, pr_=pr_, g_=g_):
                            QTb, qk_, KTb, kk_ = bufs
                            st_ = []
                            for hh in range(2):
                                h = pr_ * 2 + hh
                                r0 = hh * 64
                                st_ += attn_steps(QTb[r0:r0 + 64, :], qk_, KTb[r0:r0 + 64, :], kk_, NT, lambda kt, g_=g_: Vg[:, kt, g_, :], "Vg", 64, 64 ** -0.5, None, make_cb(h * 64), Sring, PTR, Osets)
                            return st_
                        heads.append((ld, mk))
                    for h in range(8):
                        def ld(h=h):
                            QTb, qk_ = QTR.next(); KTb, kk_ = KTR.next()
                            S.dma("sp", lambda e: e.dma_start(out=QTb[0:96, :], in_=QmT[:, h, :]), reads=["QmT"], writes=[qk_])
                            S.dma("sp", lambda e: e.dma_start(out=KTb[0:96, :], in_=KmT[:, h, :]), reads=["KmT"], writes=[kk_])
                            return (QTb, qk_, KTb, kk_)

                        def mk(bufs, h=h):
                            QTb, qk_, KTb, kk_ = bufs
                            return attn_steps(QTb[0:96, :], qk_, KTb[0:96, :], kk_, NT, lambda kt, h=h: Vm[:, kt, h, :], "Vm", 64, 96 ** -0.5, None, make_cb(512 + h * 64), Sring, PTR, Osets)
                        heads.append((ld, mk))
                    allsteps = []
                    bufs = heads[0][0]()
                    for gi, (ld, mk) in enumerate(heads):
                        st_ = mk(bufs)
                        if gi + 1 < len(heads):
                            holder = {}
                            nxt = heads[gi + 1][0]
                            st_[0]["pre"] = (lambda holder=holder, nxt=nxt: holder.__setitem__("b", nxt()))
                            run_steps(st_, ATT_LA)
                            bufs = holder["b"]
                        else:
                            run_steps(st_, ATT_LA)
                S.barrier()
            if stop_after == "P2":
                d_ = dbgt("Odr", [S_LEN, 1024], F32)
                S.dma("sp", lambda e: e.dma_start(out=d_, in_=Odr), reads=["Odr"], writes=["dbg"])
                return False
            with ExitStack() as p3:
                b = lambda name, shape, dt: sb(name, shape, dt, p3)
                Wo = b("Wo", [128, 8, 1024], BF16)
                load_weight(Wo[:, 0:4, :], "Wo", w_o[l, 0:512, :], 4, 1024, gc_og[:, l, :], "gcB")
                load_weight(Wo[:, 4:8, :], "Wo", w_o[l, 512:1024, :], 4, 1024, gc_om[:, l, :], "gcB")
                otR = Ring(b, "ot", [128, D], F32, 2); hnR = Ring(b, "hn3", [128, D], BF16, 2); XTR = Ring(b, "XT3", [128, 8, 128], BF16, 2)
                xtR = Ring(b, "xt3", [128, D], F32, 2); yR = Ring(b, "y3", [128, D], F32, 2)
                scr = b("scr3", [128, D], F32); rsx = Ring(b, "rs3", [128, 2], F32, 2)
                for j in range(NT):
                    ot, otk = otR.next(); hn, hk = hnR.next(); XT, XTk = XTR.next(); xt, xk = xtR.next(); y, yk = yR.next(); rs, rsk = rsx.next()
                    S.dma("sp", lambda e, ot=ot, j=j: e.dma_start(out=ot[:], in_=Odr[j * 128:(j + 1) * 128, :]), reads=["Odr"], writes=[otk])
                    S.dma("sp", lambda e, xt=xt, j=j: e.dma_start(out=xt[:], in_=x_src[j * 128:(j + 1) * 128, :]), reads=[xs_key], writes=[xk])
                    norm_transpose(ot, otk, hn, hk, XT, XTk, 0, rs, rsk, scr, "scr3", halves=2)
                    for nb in range(2):
                        for kc in range(8):
                            S.op("pe", lambda e, nb=nb, kc=kc, XT=XT: e.matmul(FB[nb][:, :], lhsT=XT[:, kc, :], rhs=Wo[:, kc, nb * 512:(nb + 1) * 512], start=(kc == 0), stop=(kc == 7)), reads=[XTk, "Wo"], writes=[FK[nb]])
                        S.op("dve", lambda e, nb=nb, y=y, xt=xt: e.tensor_tensor(out=y[:, nb * 512:(nb + 1) * 512], in0=FB[nb][:, :], in1=xt[:, nb * 512:(nb + 1) * 512], op=ALU.add), reads=[FK[nb], xk], writes=[yk])
                    S.dma("sp", lambda e, y=y, j=j: e.dma_start(out=xres[j * 128:(j + 1) * 128, :], in_=y[:]), reads=[yk], writes=["xres"])
            S.barrier()
            if stop_after == "P3":
                return False

            with ExitStack() as p4:
                b = lambda name, shape, dt: sb(name, shape, dt, p4)
                Wq = b("Wmq", [128, 8, 512], BF16); Wkv = b("Wmkv", [128, 8, 1024], BF16); Wmo = b("Wmo", [128, 4, 1024], BF16)
                load_weight(Wq, "Wmq", w_mem_q[l], 8, 512, gc_mem[:, l, :], "gcA")
                load_weight(Wkv, "Wmkv", w_mem_kv[l], 8, 1024, gc_memkv[:, l, :], "gcA")
                load_weight(Wmo, "Wmo", w_mem_o[l], 4, 1024)
                xtR = Ring(b, "xt4", [128, D], F32, 8); hnR = Ring(b, "hn4", [128, D], BF16, 2)
                scr = b("scr4", [128, D], F32); rsx = Ring(b, "rs4", [128, 2], F32, 2)
                XTm = b("XTm", [128, 8, 256], BF16)
                KTm = b("KTm", [128, 4, 256], BF16); Vme = b("Vme", [128, 2, 4, 129], BF16)
                S.op("pool", lambda e: e.memset(Vme[:, :, :, 128:129], 1.0), writes=["Vme"])
                for mt in range(2):
                    xt, xk = xtR.next(); hn, hk = hnR.next(); rs, rsk = rsx.next()
                    S.dma("sp", lambda e, xt=xt, mt=mt: e.dma_start(out=xt[:], in_=mem_in[mt * 128:(mt + 1) * 128, :]), writes=[xk])
                    norm_transpose(xt, xk, hn, hk, XTm, "XTm", mt * 128, rs, rsk, scr, "scr4")
                for h in range(4):
                    for kc in range(8):
                        S.op("pe", lambda e, h=h, kc=kc: e.matmul(FB[2][:, 0:256], lhsT=Wkv[:, kc, h * 128:(h + 1) * 128], rhs=XTm[:, kc, :], start=(kc == 0), stop=(kc == 7)), reads=["Wmkv", "XTm"], writes=["F2"])
                    S.op("dve", lambda e, h=h: e.tensor_copy(out=KTm[:, h, :], in_=FB[2][:, 0:256]), reads=["F2"], writes=["KTm"])
                for mt in range(2):
                    for kc in range(8):
                        S.op("pe", lambda e, mt=mt, kc=kc: e.matmul(FB[3][:, :], lhsT=XTm[:, kc, mt * 128:(mt + 1) * 128], rhs=Wkv[:, kc, 512:1024], start=(kc == 0), stop=(kc == 7)), reads=["Wmkv", "XTm"], writes=["F3"])
                    S.op("dve", lambda e, mt=mt: e.tensor_copy(out=Vme[:, mt, :, 0:128], in_=FB[3][:, :].rearrange("p (h d) -> p h d", h=4)), reads=["F3"], writes=["Vme"])
                XTR = Ring(b, "XT4", [128, 8, 512], BF16, 2)
                QTR = Ring(b, "QTm", [128, 512], BF16, 2); PTR = Ring(b, "PT4", [128, 512], BF16, 4)
                ObR = Ring(b, "Obf", [128, 4, 512], BF16, 2); OTR = Ring(b, "OT4", [128, 4, 128], BF16, 2)
                rcR = Ring(b, "rc4", [128, 4], F32, 2); yR = Ring(b, "y4", [128, D], F32, 2)
                Sring = Ring(lambda n, s_, d: FB[int(n[-1])], "F", None, None, 2)
                Osets = [[(FB[2], "F2"), (FB[3], "F3")]]
                for c in range(8):
                    XT, XTk = XTR.next()
                    xts = []
                    for t in range(4):
                        j = c * 4 + t
                        xt, xk = xtR.next(); hn, hk = hnR.next(); rs, rsk = rsx.next()
                        xts.append((xt, xk))
                        S.dma("sp", lambda e, xt=xt, j=j: e.dma_start(out=xt[:], in_=xres[j * 128:(j + 1) * 128, :]), reads=["xres"], writes=[xk])
                        norm_transpose(xt, xk, hn, hk, XT, XTk, t * 128, rs, rsk, scr, "scr4")
                    Obf, Obk = ObR.next()
                    for h in range(4):
                        QTm, QTk = QTR.next()
                        for kc in range(8):
                            S.op("pe", lambda e, h=h, kc=kc, XT=XT: e.matmul(FB[4][:, :], lhsT=Wq[:, kc, h * 128:(h + 1) * 128], rhs=XT[:, kc, :], start=(kc == 0), stop=(kc == 7)), reads=["Wmq", XTk], writes=["F4"])
                        S.op("dve", lambda e, QTm=QTm: e.tensor_copy(out=QTm[:, :], in_=FB[4][:, :]), reads=["F4"], writes=[QTk])

                        def cbk(qc, oset, per_bank, h=h, Obf=Obf, Obk=Obk):
                            rc, rck = rcR.next()
                            for qs in range(4):
                                ob, okey = oset[qs // per_bank]
                                o0 = (qs % per_bank) * 129
                                S.op("dve", lambda e, ob=ob, o0=o0, qs=qs: e.reciprocal(out=rc[:, qs:qs + 1], in_=ob[:, o0 + 128:o0 + 129]), reads=[okey], writes=[rck])
                                S.op("dve", lambda e, ob=ob, o0=o0, qs=qs: e.tensor_scalar(out=Obf[:, qs, h * 128:(h + 1) * 128], in0=ob[:, o0:o0 + 128], scalar1=rc[:, qs:qs + 1], scalar2=None, op0=ALU.mult), reads=[okey, rck], writes=[Obk])
                        attention(QTm[:, :], QTk, KTm[:, h, :], "KTm", 2, lambda kt, h=h: Vme[:, kt, h, :], "Vme", 128, 128 ** -0.5, None, cbk, Sring, PTR, Osets, nqc=1)
                    for qs in range(4):
                        j = c * 4 + qs
                        xt, xk = xts[qs]
                        OT, OTk = OTR.next(); y, yk = yR.next()
                        for h in range(4):
                            S.op("pe", lambda e, h=h, qs=qs, Obf=Obf: e.transpose(out=T1[:, h * 128:(h + 1) * 128], in_=Obf[:, qs, h * 128:(h + 1) * 128], identity=identb[:]), reads=[Obk, "identb"], writes=["T1"])
                        S.op("dve", lambda e, OT=OT: e.tensor_copy(out=OT[:, :, :], in_=T1[:, 0:512].rearrange("p (k t) -> p k t", k=4)), reads=["T1"], writes=[OTk])
                        for nb in range(2):
                            for h in range(4):
                                S.op("pe", lambda e, nb=nb, h=h, OT=OT: e.matmul(FB[4 + nb][:, :], lhsT=OT[:, h, :], rhs=Wmo[:, h, nb * 512:(nb + 1) * 512], start=(h == 0), stop=(h == 3)), reads=[OTk, "Wmo"], writes=[FK[4 + nb]])
                            S.op("dve", lambda e, nb=nb, y=y, xt=xt: e.tensor_tensor(out=y[:, nb * 512:(nb + 1) * 512], in0=FB[4 + nb][:, :], in1=xt[:, nb * 512:(nb + 1) * 512], op=ALU.add), reads=[FK[4 + nb], xk], writes=[yk])
                        S.dma("sp", lambda e, y=y, j=j: e.dma_start(out=xres[j * 128:(j + 1) * 128, :], in_=y[:]), reads=[yk], writes=["xres"])
            S.barrier()
            if stop_after == "P4":
                return False
            with ExitStack() as p5:
                bo = lambda name, shape, dt: sb(name, shape, dt, p5)
                idx_i = bo("idx_i", [128, 64], I32)
                with ExitStack() as p5a:
                    b = lambda name, shape, dt: sb(name, shape, dt, p5a)
                    gfb = b("gfb", [128, D], F32)
                    S.dma("sp", lambda e: e.dma_start(out=gfb[:], in_=ln_ffn[l, :].partition_broadcast(128)), writes=["gfb"])
                    Wr = b("Wr", [128, 8, 16], F32)
                    for kc in range(8):
                        S.dma("sp", lambda e, kc=kc: e.dma_start(out=Wr[:, kc, :], in_=w_router[l, kc * 128:(kc + 1) * 128, :]), writes=["Wr"])
                    aff = b("aff", [128, NT, 16], F32)
                    xtR = Ring(b, "xt5", [128, D], F32, 2); h32R = Ring(b, "h32", [128, D], F32, 2); hbR = Ring(b, "hb5", [128, D], BF16, 2)
                    scr = b("scr5", [128, D], F32); rsx = Ring(b, "rs5", [128, 2], F32, 2)
                    XT32R = Ring(b, "XT32", [128, 8, 128], F32, 2)
                    smR = Ring(b, "sm5", [128, 4], F32, 2); exR = Ring(b, "ex5", [128, 16], F32, 2)
                    for j in range(NT):
                        xt, xk = xtR.next(); h32, h32k = h32R.next(); hb, hbk = hbR.next(); rs, rsk = rsx.next(); XT32, X32k = XT32R.next()
                        sm, smk = smR.next(); ex, exk = exR.next()
                        S.dma("sp", lambda e, xt=xt, j=j: e.dma_start(out=xt[:], in_=xres[j * 128:(j + 1) * 128, :]), reads=["xres"], writes=[xk])
                        norm_transpose(xt, xk, h32, h32k, None, None, 0, rs, rsk, scr, "scr5", gain_b=gfb, gbkey="gfb")
                        S.op("pool", lambda e, hb=hb, h32=h32: e.tensor_copy(out=hb[:, :], in_=h32[:, :]), reads=[h32k], writes=[hbk])
                        S.dma("sp", lambda e, hb=hb, j=j: e.dma_start(out=hdr[j * 128:(j + 1) * 128, :], in_=hb[:]), reads=[hbk], writes=["hdr"])
                        for kc in range(8):
                            S.op("pe", lambda e, kc=kc, h32=h32: e.transpose(out=FB[kc // 4][:, (kc % 4) * 128:(kc % 4 + 1) * 128], in_=h32[:, kc * 128:(kc + 1) * 128], identity=identf[:]), reads=[h32k, "identf"], writes=[FK[kc // 4]])
                        for hf in range(2):
                            S.op("dve", lambda e, hf=hf, XT32=XT32: e.tensor_copy(out=XT32[:, hf * 4:(hf + 1) * 4, :], in_=FB[hf][:, :].rearrange("p (k t) -> p k t", k=4)), reads=[FK[hf]], writes=[X32k])
                        for kc in range(8):
                            S.op("pe", lambda e, kc=kc, XT32=XT32: e.matmul(FB[2][:, 0:16], lhsT=XT32[:, kc, :], rhs=Wr[:, kc, :], start=(kc == 0), stop=(kc == 7)), reads=[X32k, "Wr"], writes=["F2"])
                        S.op("dve", lambda e, sm=sm: e.tensor_reduce(out=sm[:, 0:1], in_=FB[2][:, 0:16], axis=AX.X, op=ALU.max, negate=True), reads=["F2"], writes=[smk])
                        S.op("act", lambda e, sm=sm, ex=ex: e.activation(out=ex[:, :], in_=FB[2][:, 0:16], func=AF.Exp, bias=sm[:, 0:1], scale=1.0, accum_out=sm[:, 1:2]), reads=["F2", smk], writes=[exk, smk])
                        S.op("dve", lambda e, sm=sm: e.reciprocal(out=sm[:, 2:3], in_=sm[:, 1:2]), reads=[smk], writes=[smk])
                        S.op("dve", lambda e, sm=sm, ex=ex, j=j: e.tensor_scalar(out=aff[:, j, :], in0=ex[:, :], scalar1=sm[:, 2:3], scalar2=None, op0=ALU.mult), reads=[exk, smk], writes=["aff"])
                        S.dma("sp", lambda e, j=j: e.dma_start(out=affdr[j * 128:(j + 1) * 128, :], in_=aff[:, j, :]), reads=["aff"], writes=["affdr"])
                    affT = b("affT", [16, S_LEN], F32); junk = b("junk", [16, S_LEN], BF16); maskT = b("maskT", [16, S_LEN], BF16)
                    for rnd in range(2):
                        for jj in range(16):
                            j = rnd * 16 + jj
                            S.op("pe", lambda e, j=j, jj=jj: e.transpose(out=FB[jj // 4][0:16, (jj % 4) * 128:(jj % 4 + 1) * 128], in_=aff[:, j, :], identity=identf[:]), reads=["aff", "identf"], writes=[FK[jj // 4]])
                        for q4 in range(4):
                            S.op("dve", lambda e, rnd=rnd, q4=q4: e.tensor_copy(out=affT[:, (rnd * 4 + q4) * 512:(rnd * 4 + q4 + 1) * 512], in_=FB[q4][0:16, :]), reads=[FK[q4]], writes=["affT"])
                    bs = b("bs", [16, 8], F32)
                    S.op("dve", lambda e: e.memset(bs[:, 0:1], 0.0), writes=["bs"])
                    S.op("dve", lambda e: e.memset(bs[:, 1:2], 2.0), writes=["bs"])
                    for it in range(33):
                        S.op("dve", lambda e: e.tensor_tensor(out=bs[:, 2:3], in0=bs[:, 0:1], in1=bs[:, 1:2], op=ALU.add), reads=["bs"], writes=["bs"])
                        S.op("dve", lambda e: e.tensor_scalar(out=bs[:, 2:3], in0=bs[:, 2:3], scalar1=0.5, scalar2=None, op0=ALU.mult), reads=["bs"], writes=["bs"])
                        S.op("dve", lambda e: e.tensor_scalar(out=junk[:, :], in0=affT[:, :], scalar1=bs[:, 2:3], scalar2=0.0, op0=ALU.is_ge, op1=ALU.add, accum_out=bs[:, 3:4]), reads=["affT", "bs"], writes=["junk", "bs"])
                        S.op("dve", lambda e: e.tensor_scalar(out=bs[:, 4:5], in0=bs[:, 3:4], scalar1=511.5, scalar2=None, op0=ALU.is_ge), reads=["bs"], writes=["bs"])
                        S.op("dve", lambda e: e.tensor_tensor(out=bs[:, 5:6], in0=bs[:, 2:3], in1=bs[:, 0:1], op=ALU.subtract), reads=["bs"], writes=["bs"])
                        S.op("dve", lambda e: e.scalar_tensor_tensor(out=bs[:, 0:1], in0=bs[:, 5:6], scalar=bs[:, 4:5], in1=bs[:, 0:1], op0=ALU.mult, op1=ALU.add), reads=["bs"], writes=["bs"])
                        S.op("dve", lambda e: e.tensor_tensor(out=bs[:, 5:6], in0=bs[:, 1:2], in1=bs[:, 2:3], op=ALU.subtract), reads=["bs"], writes=["bs"])
                        S.op("dve", lambda e: e.scalar_tensor_tensor(out=bs[:, 1:2], in0=bs[:, 5:6], scalar=bs[:, 4:5], in1=bs[:, 2:3], op0=ALU.mult, op1=ALU.add), reads=["bs"], writes=["bs"])
                    S.op("dve", lambda e: e.tensor_scalar(out=maskT[:, :], in0=affT[:, :], scalar1=bs[:, 0:1], scalar2=None, op0=ALU.is_ge), reads=["affT", "bs"], writes=["maskT"])
                    mask_tok = b("mask_tok", [128, NT, 16], BF16)
                    for j in range(NT):
                        S.op("pe", lambda e, j=j: e.transpose(out=T0[:, j * 16:(j + 1) * 16], in_=maskT[:, j * 128:(j + 1) * 128], identity=identb[0:16, 0:16]), reads=["maskT", "identb"], writes=["T0"])
                    S.op("dve", lambda e: e.tensor_copy(out=mask_tok[:, :, :], in_=T0[:, 0:512].rearrange("p (j e) -> p j e", j=NT)), reads=["T0"], writes=["mask_tok"])
                    onesb = b("onesb", [128, 128], BF16); ustr = b("ustr", [128, 128], BF16)
                    S.op("pool", lambda e: e.memset(onesb[:], 1.0), writes=["onesb"])
                    S.op("pool", lambda e: e.memset(ustr[:], 1.0), writes=["ustr"])
                    S.op("pool", lambda e: e.affine_select(out=ustr[:], in_=ustr[:], pattern=[[1, 128]], compare_op=ALU.is_gt, fill=0.0, base=0, channel_multiplier=-1), reads=["ustr"], writes=["ustr"])
                    firstmm = True
                    for j in range(NT):
                        for i in range(j + 1):
                            lhs = ustr if i == j else onesb
                            S.op("pe", lambda e, j=j, i=i, lhs=lhs, firstmm=firstmm: e.matmul(FB[0][:, j * 16:(j + 1) * 16], lhsT=lhs[:, :], rhs=mask_tok[:, i, :], start=firstmm, stop=(i == j), skip_group_check=True), reads=["mask_tok", "onesb", "ustr"], writes=["F0"])
                            firstmm = False
                    posm = b("posm", [128, NT, 16], F32)
                    S.op("dve", lambda e: e.scalar_tensor_tensor(out=posm[:, :, :], in0=FB[0][:, :].rearrange("p (j e) -> p j e", j=NT), scalar=1.0, in1=mask_tok[:, :, :], op0=ALU.add, op1=ALU.mult), reads=["F0", "mask_tok"], writes=["posm"])
                    S.op("dve", lambda e: e.tensor_scalar(out=posm[:, :, :], in0=posm[:, :, :], scalar1=-1.0, scalar2=None, op0=ALU.add), reads=["posm"], writes=["posm"])
                    iota_s = b("iota_s", [128, 512], F32)
                    S.op("pool", lambda e: e.iota(iota_s[:, :], pattern=[[1, 512]], base=0, channel_multiplier=0, allow_small_or_imprecise_dtypes=True), writes=["iota_s"])
                    tv32 = b("tv32", [128, NT, 2], F32); tokval = b("tokval", [128, NT, 2], BF16)
                    S.op("pool", lambda e: e.iota(tv32[:, :, :], pattern=[[0, NT], [0, 2]], base=0, channel_multiplier=1, allow_small_or_imprecise_dtypes=True), writes=["tv32"])
                    S.op("pool", lambda e: e.iota(tv32[:, :, 1], pattern=[[1, NT]], base=0, channel_multiplier=0, allow_small_or_imprecise_dtypes=True), reads=["tv32"], writes=["tv32"])
                    S.op("pool", lambda e: e.tensor_copy(out=tokval[:, :, :], in_=tv32[:, :, :]), reads=["tv32"], writes=["tokval"])
                    OhR = Ring(b, "Oh", [128, 16, 512], BF16, 2)
                    firstmm = True
                    for j in range(NT):
                        Oh, Ohk = OhR.next()
                        eng = "dve"
                        S.op(eng, lambda e, Oh=Oh, j=j: e.tensor_tensor(out=Oh[:, :, :], in0=iota_s[:, :].unsqueeze(1).to_broadcast([128, 16, 512]), in1=posm[:, j, :].unsqueeze(2).to_broadcast([128, 16, 512]), op=ALU.is_equal), reads=["iota_s", "posm"], writes=[Ohk])
                        for ee in range(16):
                            for c in range(4):
                                col = (ee * 4 + c) * 2
                                S.op("pe", lambda e, Oh=Oh, ee=ee, c=c, col=col, j=j, firstmm=firstmm: e.matmul(FB[5][:, col:col + 2], lhsT=Oh[:, ee, c * 128:(c + 1) * 128], rhs=tokval[:, j, :], start=firstmm, stop=(j == NT - 1), skip_group_check=True), reads=[Ohk, "tokval"], writes=["F5"])
                                firstmm = False
                    idxf = b("idxf", [128, 64], F32)
                    f5s = b("f5s", [128, 128], F32)
                    S.op("dve", lambda e: e.tensor_copy(out=f5s[:, :], in_=FB[5][:, 0:128]), reads=["F5"], writes=["f5s"])
                    f5v = f5s[:, :].rearrange("p (s two) -> p s two", two=2)
                    S.op("dve", lambda e: e.scalar_tensor_tensor(out=idxf[:, :], in0=f5v[:, :, 1], scalar=128.0, in1=f5v[:, :, 0], op0=ALU.mult, op1=ALU.add), reads=["f5s"], writes=["idxf"])
                    S.op("dve", lambda e: e.tensor_copy(out=idx_i[:, :], in_=idxf[:, :]), reads=["idxf"], writes=["idx_i"])
                    if stop_after == "P5a":
                        d1 = dbgt("idx", [128, 64], I32); d2 = dbgt("aff", [S_LEN, 16], F32); d3 = dbgt("bs", [16, 8], F32)
                        S.dma("sp", lambda e: e.dma_start(out=d1, in_=idx_i[:, :]), reads=["idx_i"], writes=["dbg"])
                        S.dma("sp", lambda e: e.dma_start(out=d2, in_=affdr), reads=["affdr"], writes=["dbg"])
                        S.dma("sp", lambda e: e.dma_start(out=d3, in_=bs[:, :]), reads=["bs"], writes=["dbg"])
                S.barrier()
                if stop_after == "P5a":
                    return False
                with ExitStack() as p5b:
                    b = lambda name, shape, dt: sb(name, shape, dt, p5b)
                    WgR = Ring(b, "Wg", [128, 8, 512], BF16, 2); WuR = Ring(b, "Wu", [128, 8, 512], BF16, 2); WdR = Ring(b, "Wd", [128, 4, 1024], BF16, 2)
                    XgTR = Ring(b, "XgT", [128, 8, 512], BF16, 2); xgR = Ring(b, "xg", [128, D], BF16, 8); gaR = Ring(b, "ga", [128, 16], F32, 8)
                    GTR = Ring(b, "GT", [128, 4, 512], BF16, 2); sgR = Ring(b, "sg", [128, 512], F32, 2); yR = Ring(b, "y5", [128, D], F32, 2)
                    def prefetch(ee):
                        Wg, Wgk = WgR.next(); Wu, Wuk = WuR.next(); Wd, Wdk = WdR.next()
                        load_weight(Wg, Wgk, w_gate[l, ee], 8, 512)
                        load_weight(Wu, Wuk, w_up[l, ee], 8, 512)
                        load_weight(Wd, Wdk, w_down[l, ee], 4, 1024)
                        xgs = []; gas = []
                        for c in range(4):
                            xg, xgk = xgR.next(); ga, gak = gaR.next()
                            xgs.append((xg, xgk)); gas.append((ga, gak))
                            col = ee * 4 + c
                            S.dma("pool", lambda e, xg=xg, col=col: e.indirect_dma_start(out=xg[:, :], out_offset=None, in_=hdr[:, :], in_offset=bass.IndirectOffsetOnAxis(ap=idx_i[:, col:col + 1], axis=0)), reads=["idx_i", "hdr"], writes=[xgk])
                            S.dma("pool", lambda e, ga=ga, col=col: e.indirect_dma_start(out=ga[:, :], out_offset=None, in_=affdr[:, :], in_offset=bass.IndirectOffsetOnAxis(ap=idx_i[:, col:col + 1], axis=0)), reads=["idx_i", "affdr"], writes=[gak])
                        return (Wg, Wgk, Wu, Wuk, Wd, Wdk, xgs, gas)

                    nxt = prefetch(0)
                    for ee in range(16):
                        Wg, Wgk, Wu, Wuk, Wd, Wdk, xgs, gas = nxt
                        if ee + 1 < 16:
                            nxt = prefetch(ee + 1)
                        XgT, XgTk = XgTR.next(); GT, GTk = GTR.next()
                        for c in range(4):
                            xg, xgk = xgs[c]
                            for kc in range(8):
                                S.op("pe", lambda e, kc=kc, xg=xg: e.transpose(out=T0[:, kc * 128:(kc + 1) * 128], in_=xg[:, kc * 128:(kc + 1) * 128], identity=identb[:]), reads=[xgk, "identb"], writes=["T0"])
                            S.op("dve", lambda e, XgT=XgT, c=c: e.tensor_copy(out=XgT[:, :, c * 128:(c + 1) * 128], in_=T0[:, :].rearrange("p (k t) -> p k t", k=8)), reads=["T0"], writes=[XgTk])
                        for fc in range(4):
                            pa = 2 * (fc % 2); pu = pa + 1
                            for kc in range(8):
                                S.op("pe", lambda e, fc=fc, kc=kc, pa=pa, Wg=Wg, XgT=XgT: e.matmul(FB[pa][:, :], lhsT=Wg[:, kc, fc * 128:(fc + 1) * 128], rhs=XgT[:, kc, :], start=(kc == 0), stop=(kc == 7)), reads=[Wgk, XgTk], writes=[FK[pa]])
                            for kc in range(8):
                                S.op("pe", lambda e, fc=fc, kc=kc, pu=pu, Wu=Wu, XgT=XgT: e.matmul(FB[pu][:, :], lhsT=Wu[:, kc, fc * 128:(fc + 1) * 128], rhs=XgT[:, kc, :], start=(kc == 0), stop=(kc == 7)), reads=[Wuk, XgTk], writes=[FK[pu]])
                            sg, sgk = sgR.next()
                            S.op("act", lambda e, sg=sg, pa=pa: e.activation(out=sg[:, :], in_=FB[pa][:, :], func=AF.Silu), reads=[FK[pa]], writes=[sgk])
                            S.op("dve", lambda e, sg=sg, pu=pu, GT=GT, fc=fc: e.tensor_tensor(out=GT[:, fc, :], in0=sg[:, :], in1=FB[pu][:, :], op=ALU.mult), reads=[sgk, FK[pu]], writes=[GTk])
                        for st_ in range(4):
                            y, yk = yR.next(); ga, gak = gas[st_]
                            col = ee * 4 + st_
                            for half in range(2):
                                for fc in range(4):
                                    S.op("pe", lambda e, half=half, fc=fc, GT=GT, Wd=Wd, st_=st_: e.matmul(FB[4 + half][:, :], lhsT=GT[:, fc, st_ * 128:(st_ + 1) * 128], rhs=Wd[:, fc, half * 512:(half + 1) * 512], start=(fc == 0), stop=(fc == 3)), reads=[GTk, Wdk], writes=[FK[4 + half]])
                                S.op("act", lambda e, half=half, y=y, ga=ga, ee=ee: e.activation(out=y[:, half * 512:(half + 1) * 512], in_=FB[4 + half][:, :], func=AF.Copy, scale=ga[:, ee:ee + 1]), reads=[FK[4 + half], gak], writes=[yk])
                            S.dma("pool", lambda e, y=y, col=col: e.indirect_dma_start(out=xres[:, :], out_offset=bass.IndirectOffsetOnAxis(ap=idx_i[:, col:col + 1], axis=0), in_=y[:, :], in_offset=None, compute_op=ALU.add), reads=[yk, "idx_i"], writes=["xres"])
            S.barrier()
            return True

        x_src, xs_key = x_in, "x_in"
        final_norm = (stop_after is None)
        for l in range(nlayers):
            cont = layer(l, x_src, xs_key)
            x_src, xs_key = xres, "xres"
            if not cont:
                final_norm = False
                break
        with ExitStack() as pf:
            b = lambda name, shape, dt: sb(name, shape, dt, pf)
            xtR = Ring(b, "xtf", [128, D], F32, 2); yR = Ring(b, "yf", [128, D], F32, 2)
            scr = b("scrf", [128, D], F32); rsx = Ring(b, "rsf", [128, 2], F32, 2)
            gfin = b("gfin", [128, D], F32)
            S.dma("sp", lambda e: e.dma_start(out=gfin[:], in_=ln_final.partition_broadcast(128)), writes=["gfin"])
            for j in range(NT):
                xt, xk = xtR.next(); y, yk = yR.next(); rs, rsk = rsx.next()
                S.dma("sp", lambda e, xt=xt, j=j: e.dma_start(out=xt[:], in_=xres[j * 128:(j + 1) * 128, :]), reads=["xres"], writes=[xk])
                if final_norm:
                    norm_transpose(xt, xk, y, yk, None, None, 0, rs, rsk, scr, "scrf", gain_b=gfin, gbkey="gfin")
                    S.dma("sp", lambda e, y=y, j=j: e.dma_start(out=out[j * 128:(j + 1) * 128, :], in_=y[:]), reads=[yk], writes=["out"])
                else:
                    S.dma("sp", lambda e, xt=xt, j=j: e.dma_start(out=out[j * 128:(j + 1) * 128, :], in_=xt[:]), reads=[xk], writes=["out"])
        S.barrier(engines=["sp"])
        print("ninstr", S.ninstr, "nsem", S.nsem, {k: v for k, v in S.cnt.items()})
    return nc, dbg_out


def rope_tables():
    def ang(rot_dim):
        rows = S_LEN // 64
        row = np.repeat(np.arange(rows, dtype=np.float32), 64)
        col = np.tile(np.arange(64, dtype=np.float32), rows)
        axis_dim = rot_dim // 2
        inv = (10000.0 ** (-np.arange(0, axis_dim, 2, dtype=np.float32) / axis_dim)).astype(np.float32)
        a = np.concatenate([row[:, None] * inv[None, :], col[:, None] * inv[None, :]], axis=-1).astype(np.float32)
        return np.concatenate([np.cos(a), np.sin(a)], axis=-1).astype(np.float32)
    return ang(64), ang(32)


def make_in_maps(inputs, ncores=8):
    rg, rm = rope_tables()
    maps = []
    for c in range(ncores):
        m = {k: np.ascontiguousarray(np.asarray(v, dtype=np.float32)) for k, v in inputs.items() if k not in ("x", "mem")}
        m["x"] = np.ascontiguousarray(np.asarray(inputs["x"][c], dtype=np.float32))
        m["mem"] = np.ascontiguousarray(np.asarray(inputs["mem"][c], dtype=np.float32))
        m["ropeg"] = rg
        m["ropem"] = rm
        maps.append(m)
    return maps


def kernel(**inputs):
    nc, _ = build_nc()
    maps = make_in_maps(inputs, 8)
    res = run_bass_kernel_spmd(nc, maps, core_ids=list(range(8)))
    return np.stack([np.asarray(r["out"], dtype=np.float32) for r in res.results], axis=0)
```

```python
from contextlib import ExitStack
import numpy as np
import ml_dtypes
import concourse.bass as bass
import concourse.mybir as mybir
from concourse.bass_utils import run_bass_kernel_spmd

F32 = mybir.dt.float32
BF16 = mybir.dt.bfloat16
I32 = mybir.dt.int32
ALU = mybir.AluOpType
AF = mybir.ActivationFunctionType
AX = mybir.AxisListType

S_LEN = 4096
D = 1024
NT = 32
DEPTH = 4
EPS = 1e-6
IN_W = 1184
EPOCH = 30000
import os
ATT_LA = int(os.environ.get('ATT_LA', '3'))


class Sched:
    def __init__(self, nc, es, dma_slots=8):
        self.nc = nc
        self.es = es
        self.engs = {"pe": nc.tensor, "act": nc.scalar, "dve": nc.vector, "pool": nc.gpsimd, "sp": nc.sync}
        self.cnt = {k: 0 for k in self.engs}
        self.esems = {k: [] for k in self.engs}
        self.waited = {k: {} for k in self.engs}
        self.lastw = {}
        self.readers = {}
        self.dma_pool = {}
        self.dma_slots = dma_slots
        self.nsem = 0
        self.ninstr = 0

    def _newsem(self, name):
        self.nsem += 1
        return self.es.enter_context(self.nc.semaphore(name))

    def _eng_event(self, eng):
        c = self.cnt[eng]
        ep, loc = divmod(c, EPOCH)
        while len(self.esems[eng]) <= ep:
            self.esems[eng].append(self._newsem(f"e_{eng}_{len(self.esems[eng])}"))
        self.cnt[eng] = c + 1
        return (self.esems[eng][ep], loc + 1, eng)

    def _dma_event(self, q):
        st = self.dma_pool.setdefault(q, {"sems": [], "uses": [], "i": 0})
        if len(st["sems"]) < self.dma_slots:
            st["sems"].append(self._newsem(f"d_{q}_{len(st['sems'])}"))
            st["uses"].append(0)
        i = st["i"] % self.dma_slots
        st["i"] += 1
        sem = st["sems"][i]
        if st["uses"][i] > 0:
            self._wait(q, (sem, 16 * st["uses"][i], None))
        st["uses"][i] += 1
        return (sem, 16 * st["uses"][i], None)

    def _wait(self, eng, ev):
        sem, val, src = ev
        if src == eng and eng == "pe":
            return
        w = self.waited[eng]
        k = id(sem)
        if w.get(k, 0) >= val:
            return
        w[k] = val
        self.engs[eng].wait_ge(sem, val)
        self.ninstr += 1

    def _deps(self, eng, reads, writes):
        for k in reads:
            ev = self.lastw.get(k)
            if ev is not None:
                self._wait(eng, ev)
        for k in writes:
            ev = self.lastw.get(k)
            if ev is not None:
                self._wait(eng, ev)
            for ev in self.readers.get(k, {}).values():
                self._wait(eng, ev)

    def _record(self, ev, reads, writes):
        for k in writes:
            self.lastw[k] = ev
            self.readers[k] = {}
        for k in reads:
            if k in writes:
                continue
            self.readers.setdefault(k, {})[id(ev[0])] = ev

    def op(self, eng, fn, reads=(), writes=()):
        self._deps(eng, reads, writes)
        ev = self._eng_event(eng)
        fn(self.engs[eng]).then_inc(ev[0], 1)
        self._record(ev, reads, writes)
        self.ninstr += 1
        return ev

    def dma(self, q, fn, reads=(), writes=()):
        self._deps(q, reads, writes)
        ev = self._dma_event(q)
        fn(self.engs[q]).then_inc(ev[0], 16)
        self._record(ev, reads, writes)
        self.ninstr += 1
        return ev

    def all_events(self):
        evs = []
        for eng in self.engs:
            c = self.cnt[eng]
            if c > 0:
                ep, loc = divmod(c - 1, EPOCH)
                evs.append((self.esems[eng][ep], loc + 1, eng))
        for q, st in self.dma_pool.items():
            for sem, u in zip(st["sems"], st["uses"]):
                if u:
                    evs.append((sem, 16 * u, None))
        return evs

    def barrier(self, engines=None):
        evs = self.all_events()
        for eng in (engines or list(self.engs)):
            for ev in evs:
                if ev[2] == eng:
                    continue
                self._wait(eng, ev)
        if engines is None:
            self.lastw = {}
            self.readers = {}


class Ring:
    def __init__(self, alloc, name, shape, dt, n):
        self.items = [(alloc(f"{name}{i}", shape, dt), f"{name}{i}") for i in range(n)]
        self.i = 0

    def next(self):
        it = self.items[self.i % len(self.items)]
        self.i += 1
        return it


def build_nc(nlayers=DEPTH, dbg=None, stop_after=None):
    dbg = dbg or {}
    nc = bass.Bass("TRN2", target_bir_lowering=False)
    L = DEPTH

    def din(name, shape, dt=F32):
        return nc.dram_tensor(name, list(shape), dt, kind="ExternalInput").ap()

    def dscr(name, shape, dt):
        return nc.dram_tensor(name, list(shape), dt, kind="Internal").ap()

    x_in = din("x", [S_LEN, D])
    mem_in = din("mem", [256, D])
    ln_mix = din("ln_mix", [L, D]); w_in = din("w_in", [L, D, IN_W])
    gqa_q_norm = din("gqa_q_norm", [L, 64]); gqa_k_norm = din("gqa_k_norm", [L, 64])
    mla_q_norm = din("mla_q_norm", [L, 256]); mla_kv_norm = din("mla_kv_norm", [L, 128])
    w_q_b = din("w_q_b", [L, 256, 768]); w_kv_b = din("w_kv_b", [L, 128, 1024])
    out_norm_gqa = din("out_norm_gqa", [L, 512]); out_norm_mla = din("out_norm_mla", [L, 512])
    w_o = din("w_o", [L, 1024, 1024])
    ln_mem = din("ln_mem", [L, D]); ln_mem_kv = din("ln_mem_kv", [L, D])
    w_mem_q = din("w_mem_q", [L, D, 512]); w_mem_kv = din("w_mem_kv", [L, D, 1024]); w_mem_o = din("w_mem_o", [L, 512, D])
    ln_ffn = din("ln_ffn", [L, D]); w_router = din("w_router", [L, D, 16])
    w_gate = din("w_gate", [L, 16, D, 512]); w_up = din("w_up", [L, 16, D, 512]); w_down = din("w_down", [L, 16, 512, D])
    ln_final = din("ln_final", [D])
    ropeg = din("ropeg", [S_LEN, 64])
    ropem = din("ropem", [S_LEN, 32])
    out = nc.dram_tensor("out", [S_LEN, D], F32, kind="ExternalOutput").ap()

    xres = dscr("xres", [S_LEN, D], F32)
    QgT = dscr("QgT", [128, 4, S_LEN], BF16)
    KgT = dscr("KgT", [128, 2, S_LEN], BF16)
    QmT = dscr("QmT", [96, 8, S_LEN], BF16)
    KmT = dscr("KmT", [96, 8, S_LEN], BF16)
    Odr = dscr("Odr", [S_LEN, 1024], F32)
    hdr = dscr("hdr", [S_LEN, D], BF16)
    affdr = dscr("affdr", [S_LEN, 16], F32)

    dbg_out = {}

    def dbgt(name, shape, dt=F32):
        t = nc.dram_tensor("dbg_" + name, list(shape), dt, kind="ExternalOutput").ap()
        dbg_out[name] = t
        return t

    with ExitStack() as es:
        S = Sched(nc, es)

        uid = [0]

        def sb(name, shape, dt, stack=es):
            uid[0] += 1
            return stack.enter_context(nc.sbuf_tensor(f"{name}_{uid[0]}", list(shape), dt))

        def ps(name, shape, dt, stack=es):
            return stack.enter_context(nc.psum_tensor(name, list(shape), dt))

        T0 = ps("T0", [128, 1024], BF16); T1 = ps("T1", [128, 1024], BF16)
        FB = [ps(f"F{i}", [128, 512], F32) for i in range(6)]
        FK = [f"F{i}" for i in range(6)]

        identb = sb("identb", [128, 128], BF16)
        identf = sb("identf", [128, 128], F32)
        for t_, k_ in ((identb, "identb"), (identf, "identf")):
            S.op("pool", lambda e, t_=t_: e.memset(t_[:], 0.0), writes=[k_])
            S.op("pool", lambda e, t_=t_: e.affine_select(out=t_[:], in_=t_[:], pattern=[[-1, 128]], compare_op=ALU.not_equal, fill=1.0, base=0, channel_multiplier=1), reads=[k_], writes=[k_])
        neghalf = sb("neghalf", [128, 16], F32)
        S.op("pool", lambda e: e.memset(neghalf[:], -0.5), writes=["neghalf"])
        ropeg_sb = sb("ropeg_sb", [128, NT, 64], F32)
        ropem_sb = sb("ropem_sb", [128, NT, 32], F32)
        S.dma("sp", lambda e: e.dma_start(out=ropeg_sb[:], in_=ropeg.rearrange("(j p) c -> p j c", p=128)), writes=["ropeg"])
        S.dma("sp", lambda e: e.dma_start(out=ropem_sb[:], in_=ropem.rearrange("(j p) c -> p j c", p=128)), writes=["ropem"])
        growA = sb("growA", [96, 128], F32); growB = sb("growB", [44, 128], F32)
        gcA = sb("gcA", [128, 96], F32); gcB = sb("gcB", [128, 44], F32)
        for r0, src in ((0, ln_mix), (32, ln_mem), (64, ln_mem_kv)):
            S.dma("sp", lambda e, r0=r0, src=src: e.dma_start(out=growA[r0:r0 + 32, :], in_=src.rearrange("l (kc p) -> (l kc) p", p=128)), writes=["growA"])
        for r0, n_, src in ((0, 8, mla_q_norm), (8, 4, mla_kv_norm), (12, 16, out_norm_gqa), (28, 16, out_norm_mla)):
            S.dma("sp", lambda e, r0=r0, n_=n_, src=src: e.dma_start(out=growB[r0:r0 + n_, :], in_=src.rearrange("l (kc p) -> (l kc) p", p=128)), writes=["growB"])
        S.op("pe", lambda e: e.transpose(out=FB[0][:, 0:96], in_=growA[:, :], identity=identf[0:96, 0:96]), reads=["growA", "identf"], writes=["F0"])
        S.op("pe", lambda e: e.transpose(out=FB[1][:, 0:44], in_=growB[:, :], identity=identf[0:44, 0:44]), reads=["growB", "identf"], writes=["F1"])
        S.op("dve", lambda e: e.tensor_copy(out=gcA[:, :], in_=FB[0][:, 0:96]), reads=["F0"], writes=["gcA"])
        S.op("dve", lambda e: e.tensor_copy(out=gcB[:, :], in_=FB[1][:, 0:44]), reads=["F1"], writes=["gcB"])
        gcAv = gcA[:, :].rearrange("p (g l k) -> p g l k", g=3, l=L)
        gc_mix, gc_mem, gc_memkv = gcAv[:, 0], gcAv[:, 1], gcAv[:, 2]
        gc_q = gcB[:, 0:8].rearrange("p (l k) -> p l k", l=L); gc_kv = gcB[:, 8:12].rearrange("p (l k) -> p l k", l=L)
        gc_og = gcB[:, 12:28].rearrange("p (l k) -> p l k", l=L); gc_om = gcB[:, 28:44].rearrange("p (l k) -> p l k", l=L)
        invn = sb("invn", [128, 12], F32)
        S.op("pool", lambda e: e.memset(invn[:, 0:10], 1.0 / 64), writes=["invn"])
        S.op("pool", lambda e: e.memset(invn[:, 10:11], 1.0 / 256), writes=["invn"])
        S.op("pool", lambda e: e.memset(invn[:, 11:12], 1.0 / 128), writes=["invn"])

        def rstd_from_ss(ss_ap, n, out_ap, key_in, key_out, ncols=1):
            S.op("dve", lambda e: e.tensor_scalar(out=out_ap, in0=ss_ap, scalar1=1.0 / n, scalar2=EPS, op0=ALU.mult, op1=ALU.add), reads=[key_in], writes=[key_out])
            S.op("pool", lambda e: e.tensor_tensor(out=out_ap, in0=out_ap, in1=neghalf[:, 0:ncols], op=ALU.pow), reads=[key_out, "neghalf"], writes=[key_out])

        wstage = Ring(sb, "wst", [128, IN_W], F32, 3)

        cast_rr = [0]

        def load_weight(dst, dkey, src2d, KC, N, gain=None, gkey=None, q="sp", engs=("pool",)):
            for kc in range(KC):
                n0 = 0
                while n0 < N:
                    nn = min(IN_W, N - n0)
                    st, sk = wstage.next()
                    S.dma(q, lambda e, st=st, kc=kc, n0=n0, nn=nn: e.dma_start(out=st[:, 0:nn], in_=src2d[kc * 128:(kc + 1) * 128, n0:n0 + nn]), writes=[sk])
                    eng = engs[cast_rr[0] % len(engs)]
                    cast_rr[0] += 1
                    rd = [sk] + ([gkey] if gain is not None else [])
                    if eng == "act":
                        if gain is not None:
                            S.op("act", lambda e, st=st, kc=kc, n0=n0, nn=nn: e.activation(out=dst[:, kc, n0:n0 + nn], in_=st[:, 0:nn], func=AF.Copy, scale=gain[:, kc:kc + 1]), reads=rd, writes=[dkey])
                        else:
                            S.op("act", lambda e, st=st, kc=kc, n0=n0, nn=nn: e.activation(out=dst[:, kc, n0:n0 + nn], in_=st[:, 0:nn], func=AF.Copy), reads=rd, writes=[dkey])
                    elif gain is not None:
                        S.op(eng, lambda e, st=st, kc=kc, n0=n0, nn=nn: e.tensor_scalar(out=dst[:, kc, n0:n0 + nn], in0=st[:, 0:nn], scalar1=gain[:, kc:kc + 1], scalar2=None, op0=ALU.mult), reads=rd, writes=[dkey])
                    else:
                        S.op(eng, lambda e, st=st, kc=kc, n0=n0, nn=nn: e.tensor_copy(out=dst[:, kc, n0:n0 + nn], in_=st[:, 0:nn]), reads=rd, writes=[dkey])
                    n0 += nn

        def norm_transpose(xt, xkey, hn, hkey, XTdst, XTkey, tcol, rstd, rkey, scr, scrkey, gain_b=None, gbkey=None, halves=1):
            w = D // halves
            for hf in range(halves):
                S.op("dve", lambda e, hf=hf: e.scalar_tensor_tensor(out=scr[:, hf * w:(hf + 1) * w], in0=xt[:, hf * w:(hf + 1) * w], scalar=1.0, in1=xt[:, hf * w:(hf + 1) * w], op0=ALU.mult, op1=ALU.mult, accum_out=rstd[:, hf:hf + 1]),
                     reads=[xkey], writes=[scrkey, rkey])
            rstd_from_ss(rstd[:, 0:halves], w, rstd[:, 0:halves], rkey, rkey, ncols=halves)
            for hf in range(halves):
                if gain_b is None:
                    S.op("dve", lambda e, hf=hf: e.tensor_scalar(out=hn[:, hf * w:(hf + 1) * w], in0=xt[:, hf * w:(hf + 1) * w], scalar1=rstd[:, hf:hf + 1], scalar2=None, op0=ALU.mult), reads=[xkey, rkey], writes=[hkey])
                else:
                    S.op("dve", lambda e, hf=hf: e.scalar_tensor_tensor(out=hn[:, hf * w:(hf + 1) * w], in0=xt[:, hf * w:(hf + 1) * w], scalar=rstd[:, hf:hf + 1], in1=gain_b[:, hf * w:(hf + 1) * w], op0=ALU.mult, op1=ALU.mult), reads=[xkey, rkey, gbkey], writes=[hkey])
            if XTdst is None:
                return
            for kc in range(8):
                S.op("pe", lambda e, kc=kc: e.transpose(out=T0[:, kc * 128:(kc + 1) * 128], in_=hn[:, kc * 128:(kc + 1) * 128], identity=identb[:]), reads=[hkey, "identb"], writes=["T0"])
            S.op("dve", lambda e: e.tensor_copy(out=XTdst[:, :, tcol:tcol + 128], in_=T0[:, :].rearrange("p (k t) -> p k t", k=8)), reads=["T0"], writes=[XTkey])

        def attn_steps(QT, qkey, KT, kkey, nk_tiles, Vfn, vkey, dv, scale, bias, out_cb, Sring, PTring, Oring_sets, nqc=8):
            per_bank = min(512 // (dv + 1), 4)
            steps = []
            for qc in range(nqc):
                oset = Oring_sets[qc % len(Oring_sets)]
                for kt in range(nk_tiles):
                    st = {}

                    def fS(st=st, kt=kt, qc=qc):
                        sp_, skey = Sring.next()
                        st["s"] = (sp_, skey)
                        S.op("pe", lambda e: e.matmul(sp_[:, :], lhsT=KT[:, kt * 128:(kt + 1) * 128], rhs=QT[:, qc * 512:(qc + 1) * 512], start=True, stop=True), reads=[qkey, kkey], writes=[skey])

                    def fE(st=st):
                        sp_, skey = st["s"]
                        pt_, pkey = PTring.next()
                        st["p"] = (pt_, pkey)
                        if bias is None:
                            S.op("act", lambda e: e.activation(out=pt_[:, :], in_=sp_[:, :], func=AF.Exp, scale=scale), reads=[skey], writes=[pkey])
                        else:
                            S.op("act", lambda e: e.activation(out=pt_[:, :], in_=sp_[:, :], func=AF.Exp, scale=scale, bias=bias[0]), reads=[skey, bias[1]], writes=[pkey])

                    def fPV(st=st, kt=kt, oset=oset):
                        pt_, pkey = st["p"]
                        for qs in range(4):
                            ob, okey = oset[qs // per_bank]
                            o0 = (qs % per_bank) * (dv + 1)
                            st_flag = (kt == 0 and (qs % per_bank) == 0)
                            S.op("pe", lambda e, ob=ob, o0=o0, qs=qs, st_flag=st_flag: e.matmul(ob[:, o0:o0 + dv + 1], lhsT=pt_[:, qs * 128:(qs + 1) * 128], rhs=Vfn(kt), start=st_flag, stop=(kt == nk_tiles - 1), skip_group_check=True),
                                 reads=[pkey, vkey], writes=[okey])

                    post = (lambda qc=qc, oset=oset: out_cb(qc, oset, per_bank)) if kt == nk_tiles - 1 else None
                    steps.append({"S": fS, "E": fE, "PV": fPV, "post": post, "pre": None})
            return steps

        def run_steps(steps, LA):
            n = len(steps)
            for i in range(n + LA):
                if i < n:
                    if steps[i]["pre"] is not None:
                        steps[i]["pre"]()
                    steps[i]["S"]()
                m = i - LA
                if m >= 0:
                    steps[m]["E"]()
                    steps[m]["PV"]()
                    if steps[m]["post"] is not None:
                        steps[m]["post"]()

        def attention(*args, LA=1, **kw):
            run_steps(attn_steps(*args, **kw), LA)

        def layer(l, x_src, xs_key):
            with ExitStack() as ph:
                a = lambda name, shape, dt: sb(name, shape, dt, ph)
                Win = a("Win", [128, 8, IN_W], BF16); Wqb = a("Wqb", [128, 2, 768], BF16); Wkvb = a("Wkvb", [128, 1, 1024], BF16)
                load_weight(Win, "Win", w_in[l], 8, IN_W, gc_mix[:, l, :], "gcA")
                load_weight(Wqb, "Wqb", w_q_b[l], 2, 768, gc_q[:, l, :], "gcB")
                load_weight(Wkvb, "Wkvb", w_kv_b[l], 1, 1024, gc_kv[:, l, :], "gcB")
                gq = a("gq", [128, 64], F32); gk = a("gk", [128, 64], F32)
                S.dma("sp", lambda e: e.dma_start(out=gq[:], in_=gqa_q_norm[l, :].partition_broadcast(128)), writes=["gq"])
                S.dma("sp", lambda e: e.dma_start(out=gk[:], in_=gqa_k_norm[l, :].partition_broadcast(128)), writes=["gk"])
                Vg = a("Vg", [128, NT, 2, 65], BF16); Vm = a("Vm", [128, NT, 8, 65], BF16)
                S.op("pool", lambda e: e.memset(Vg[:, :, :, 64:65], 1.0), writes=["Vg"])
                S.op("pool", lambda e: e.memset(Vm[:, :, :, 64:65], 1.0), writes=["Vm"])
                with ExitStack() as p1:
                    b = lambda name, shape, dt: sb(name, shape, dt, p1)
                    xtR = Ring(b, "xt", [128, D], F32, 2); hnR = Ring(b, "hn", [128, D], BF16, 2)
                    XTR = Ring(b, "XT", [128, 8, 128], BF16, 2)
                    scr = b("scr", [128, D], F32)
                    prR = Ring(b, "pr", [128, IN_W], F32, 2)
                    stR = Ring(b, "st", [128, 12], F32, 2)
                    rsx = Ring(b, "rsx", [128, 2], F32, 2)
                    qn = b("qn", [128, 10, 64], F32); tmpa = b("tmpa", [128, 10, 32], F32); tmpb = b("tmpb", [128, 10, 32], F32)
                    rot_f = b("rot", [128, 640], BF16); kdup_f = b("kdup", [128, 256], BF16)
                    rot = rot_f[:, :].rearrange("p (h d) -> p h d", h=10); kdup = kdup_f[:, :].rearrange("p (g u d) -> p g u d", g=2, u=2)
                    cb = b("cb", [128, 384], BF16); cT = b("cT", [128, 3, 128], BF16)
                    qm32_f = b("qm32", [128, 768], F32); qmb_f = b("qmb", [128, 768], BF16); km_f = b("km", [128, 768], BF16)
                    qm32 = qm32_f[:, :].rearrange("p (h d) -> p h d", h=8); qmb = qmb_f[:, :].rearrange("p (h d) -> p h d", h=8); km = km_f[:, :].rearrange("p (h d) -> p h d", h=8)
                    krot = b("krot", [128, 32], F32)
                    QKst = Ring(b, "QKst", [128, 6, 512], BF16, 2)
                    QmSt = Ring(b, "QmSt", [128, 8, 512], BF16, 1); KmSt = Ring(b, "KmSt", [128, 8, 512], BF16, 1)
                    for c in range(8):
                        qkst, qkk = QKst.next(); qmst, qmk = QmSt.next(); kmst, kmk = KmSt.next()
                        for t in range(4):
                            j = c * 4 + t
                            xt, xk = xtR.next(); hn, hk = hnR.next(); XT, XTk = XTR.next(); pr, prk = prR.next(); st, stk = stR.next(); rs, rsk = rsx.next()
                            S.dma("sp", lambda e, xt=xt, j=j: e.dma_start(out=xt[:], in_=x_src[j * 128:(j + 1) * 128, :]), reads=[xs_key], writes=[xk])
                            norm_transpose(xt, xk, hn, hk, XT, XTk, 0, rs, rsk, scr, "scr")
                            for nb, (n0, nn) in enumerate(((0, 512), (512, 512), (1024, 160))):
                                for kc in range(8):
                                    S.op("pe", lambda e, nb=nb, n0=n0, nn=nn, kc=kc, XT=XT: e.matmul(FB[nb][:, 0:nn], lhsT=XT[:, kc, :], rhs=Win[:, kc, n0:n0 + nn], start=(kc == 0), stop=(kc == 7)), reads=[XTk, "Win"], writes=[FK[nb]])
                                S.op("act", lambda e, nb=nb, n0=n0, nn=nn, pr=pr: e.activation(out=pr[:, n0:n0 + nn], in_=FB[nb][:, 0:nn], func=AF.Copy), reads=[FK[nb]], writes=[prk])
                            S.op("dve", lambda e, pr=pr: e.tensor_tensor(out=scr[:, 0:640], in0=pr[:, 0:640], in1=pr[:, 0:640], op=ALU.mult), reads=[prk], writes=["scr"])
                            S.op("dve", lambda e, st=st: e.tensor_reduce(out=st[:, 0:10], in_=scr[:, 0:640].rearrange("p (h d) -> p h d", h=10), axis=AX.X, op=ALU.add), reads=["scr"], writes=[stk])
                            S.op("dve", lambda e, pr=pr, st=st: e.scalar_tensor_tensor(out=scr[:, 0:256], in0=pr[:, 768:1024], scalar=1.0, in1=pr[:, 768:1024], op0=ALU.mult, op1=ALU.mult, accum_out=st[:, 10:11]), reads=[prk], writes=["scr", stk])
                            S.op("dve", lambda e, pr=pr, st=st: e.scalar_tensor_tensor(out=scr[:, 0:128], in0=pr[:, 1024:1152], scalar=1.0, in1=pr[:, 1024:1152], op0=ALU.mult, op1=ALU.mult, accum_out=st[:, 11:12]), reads=[prk], writes=["scr", stk])
                            S.op("dve", lambda e, st=st: e.tensor_tensor(out=st[:, :], in0=st[:, :], in1=invn[:, :], op=ALU.mult), reads=[stk, "invn"], writes=[stk])
                            S.op("dve", lambda e, st=st: e.tensor_scalar(out=st[:, :], in0=st[:, :], scalar1=EPS, scalar2=None, op0=ALU.add), reads=[stk], writes=[stk])
                            S.op("pool", lambda e, st=st: e.tensor_tensor(out=st[:, :], in0=st[:, :], in1=neghalf[:, 0:12], op=ALU.pow), reads=[stk, "neghalf"], writes=[stk])
                            prqk = pr[:, 0:640].rearrange("p (h d) -> p h d", h=10)
                            S.op("dve", lambda e, st=st, prqk=prqk: e.tensor_tensor(out=qn[:, :, :], in0=prqk, in1=st[:, 0:10].unsqueeze(2).to_broadcast([128, 10, 64]), op=ALU.mult), reads=[prk, stk], writes=["qn"])
                            S.op("dve", lambda e: e.tensor_tensor(out=qn[:, 0:8, :], in0=qn[:, 0:8, :], in1=gq[:, :].unsqueeze(1).to_broadcast([128, 8, 64]), op=ALU.mult), reads=["qn", "gq"], writes=["qn"])
                            S.op("dve", lambda e: e.tensor_tensor(out=qn[:, 8:10, :], in0=qn[:, 8:10, :], in1=gk[:, :].unsqueeze(1).to_broadcast([128, 2, 64]), op=ALU.mult), reads=["qn", "gk"], writes=["qn"])
                            cg = ropeg_sb[:, j, 0:32].unsqueeze(1).to_broadcast([128, 10, 32]); sg = ropeg_sb[:, j, 32:64].unsqueeze(1).to_broadcast([128, 10, 32])
                            S.op("dve", lambda e, cg=cg: e.tensor_tensor(out=tmpa[:, :, :], in0=qn[:, :, 0:32], in1=cg, op=ALU.mult), reads=["qn", "ropeg"], writes=["tmpa"])
                            S.op("dve", lambda e, sg=sg: e.tensor_tensor(out=tmpb[:, :, :], in0=qn[:, :, 32:64], in1=sg, op=ALU.mult), reads=["qn", "ropeg"], writes=["tmpb"])
                            S.op("dve", lambda e: e.tensor_tensor(out=rot[:, :, 0:32], in0=tmpa[:, :, :], in1=tmpb[:, :, :], op=ALU.subtract), reads=["tmpa", "tmpb"], writes=["rot"])
                            S.op("dve", lambda e, cg=cg: e.tensor_tensor(out=tmpa[:, :, :], in0=qn[:, :, 32:64], in1=cg, op=ALU.mult), reads=["qn", "ropeg"], writes=["tmpa"])
                            S.op("dve", lambda e, sg=sg: e.tensor_tensor(out=tmpb[:, :, :], in0=qn[:, :, 0:32], in1=sg, op=ALU.mult), reads=["qn", "ropeg"], writes=["tmpb"])
                            S.op("dve", lambda e: e.tensor_tensor(out=rot[:, :, 32:64], in0=tmpa[:, :, :], in1=tmpb[:, :, :], op=ALU.add), reads=["tmpa", "tmpb"], writes=["rot"])
                            S.op("dve", lambda e: e.tensor_copy(out=kdup[:, :, :, :], in_=rot[:, 8:10, :].unsqueeze(2).to_broadcast([128, 2, 2, 64])), reads=["rot"], writes=["kdup"])
                            for p_ in range(4):
                                S.op("pe", lambda e, p_=p_: e.transpose(out=T1[:, p_ * 128:(p_ + 1) * 128], in_=rot_f[:, p_ * 128:(p_ + 1) * 128], identity=identb[:]), reads=["rot", "identb"], writes=["T1"])
                            for g_ in range(2):
                                S.op("pe", lambda e, g_=g_: e.transpose(out=T1[:, (4 + g_) * 128:(5 + g_) * 128], in_=kdup_f[:, g_ * 128:(g_ + 1) * 128], identity=identb[:]), reads=["kdup", "identb"], writes=["T1"])
                            S.op("dve", lambda e, qkst=qkst, t=t: e.tensor_copy(out=qkst[:, :, t * 128:(t + 1) * 128], in_=T1[:, 0:768].rearrange("p (k t) -> p k t", k=6)), reads=["T1"], writes=[qkk])
                            S.op("act", lambda e, pr=pr, j=j: e.activation(out=Vg[:, j, :, 0:64], in_=pr[:, 640:768].rearrange("p (g d) -> p g d", g=2), func=AF.Copy), reads=[prk], writes=["Vg"])
                            S.op("dve", lambda e, pr=pr, st=st: e.tensor_scalar(out=cb[:, 0:256], in0=pr[:, 768:1024], scalar1=st[:, 10:11], scalar2=None, op0=ALU.mult), reads=[prk, stk], writes=["cb"])
                            S.op("dve", lambda e, pr=pr, st=st: e.tensor_scalar(out=cb[:, 256:384], in0=pr[:, 1024:1152], scalar1=st[:, 11:12], scalar2=None, op0=ALU.mult), reads=[prk, stk], writes=["cb"])
                            for i_ in range(3):
                                S.op("pe", lambda e, i_=i_: e.transpose(out=T1[:, i_ * 128:(i_ + 1) * 128], in_=cb[:, i_ * 128:(i_ + 1) * 128], identity=identb[:]), reads=["cb", "identb"], writes=["T1"])
                            S.op("dve", lambda e: e.tensor_copy(out=cT[:, :, :], in_=T1[:, 0:384].rearrange("p (k t) -> p k t", k=3)), reads=["T1"], writes=["cT"])
                            for nb, (n0, nn) in ((3, (0, 512)), (4, (512, 256))):
                                for kc in range(2):
                                    S.op("pe", lambda e, nb=nb, n0=n0, nn=nn, kc=kc: e.matmul(FB[nb][:, 0:nn], lhsT=cT[:, kc, :], rhs=Wqb[:, kc, n0:n0 + nn], start=(kc == 0), stop=(kc == 1)), reads=["cT", "Wqb"], writes=[FK[nb]])
                            qm32f = qm32_f
                            S.op("act", lambda e: e.activation(out=qm32f[:, 0:512], in_=FB[3][:, 0:512], func=AF.Copy), reads=[FK[3]], writes=["qm32"])
                            S.op("act", lambda e: e.activation(out=qm32f[:, 512:768], in_=FB[4][:, 0:256], func=AF.Copy), reads=[FK[4]], writes=["qm32"])
                            for nb in range(2):
                                S.op("pe", lambda e, nb=nb: e.matmul(FB[nb][:, 0:512], lhsT=cT[:, 2, :], rhs=Wkvb[:, 0, nb * 512:(nb + 1) * 512], start=True, stop=True), reads=["cT", "Wkvb"], writes=[FK[nb]])
                                kvv = FB[nb][:, 0:512].rearrange("p (h d) -> p h d", h=4)
                                S.op("act", lambda e, kvv=kvv, nb=nb, j=j: e.activation(out=Vm[:, j, nb * 4:(nb + 1) * 4, 0:64], in_=kvv[:, :, 64:128], func=AF.Copy), reads=[FK[nb]], writes=["Vm"])
                                S.op("dve", lambda e, kvv=kvv, nb=nb: e.tensor_copy(out=km[:, nb * 4:(nb + 1) * 4, 0:64], in_=kvv[:, :, 0:64]), reads=[FK[nb]], writes=["km"])
                            cm = ropem_sb[:, j, 0:16]; sm = ropem_sb[:, j, 16:32]
                            cm8 = cm.unsqueeze(1).to_broadcast([128, 8, 16]); sm8 = sm.unsqueeze(1).to_broadcast([128, 8, 16])
                            ta = tmpa[:, 0:8, 0:16]; tb = tmpb[:, 0:8, 0:16]
                            S.op("dve", lambda e: e.tensor_copy(out=qmb[:, :, 0:64], in_=qm32[:, :, 0:64]), reads=["qm32"], writes=["qmb"])
                            S.op("dve", lambda e, cm8=cm8, ta=ta: e.tensor_tensor(out=ta, in0=qm32[:, :, 64:80], in1=cm8, op=ALU.mult), reads=["qm32", "ropem"], writes=["tmpa"])
                            S.op("dve", lambda e, sm8=sm8, tb=tb: e.tensor_tensor(out=tb, in0=qm32[:, :, 80:96], in1=sm8, op=ALU.mult), reads=["qm32", "ropem"], writes=["tmpb"])
                            S.op("dve", lambda e, ta=ta, tb=tb: e.tensor_tensor(out=qmb[:, :, 64:80], in0=ta, in1=tb, op=ALU.subtract), reads=["tmpa", "tmpb"], writes=["qmb"])
                            S.op("dve", lambda e, cm8=cm8, ta=ta: e.tensor_tensor(out=ta, in0=qm32[:, :, 80:96], in1=cm8, op=ALU.mult), reads=["qm32", "ropem"], writes=["tmpa"])
                            S.op("dve", lambda e, sm8=sm8, tb=tb: e.tensor_tensor(out=tb, in0=qm32[:, :, 64:80], in1=sm8, op=ALU.mult), reads=["qm32", "ropem"], writes=["tmpb"])
                            S.op("dve", lambda e, ta=ta, tb=tb: e.tensor_tensor(out=qmb[:, :, 80:96], in0=ta, in1=tb, op=ALU.add), reads=["tmpa", "tmpb"], writes=["qmb"])
                            ta1 = tmpa[:, 8, 0:16]; tb1 = tmpb[:, 8, 0:16]
                            S.op("dve", lambda e, pr=pr, cm=cm, ta1=ta1: e.tensor_tensor(out=ta1, in0=pr[:, 1152:1168], in1=cm, op=ALU.mult), reads=[prk, "ropem"], writes=["tmpa"])
                            S.op("dve", lambda e, pr=pr, sm=sm, tb1=tb1: e.tensor_tensor(out=tb1, in0=pr[:, 1168:1184], in1=sm, op=ALU.mult), reads=[prk, "ropem"], writes=["tmpb"])
                            S.op("dve", lambda e, ta1=ta1, tb1=tb1: e.tensor_tensor(out=krot[:, 0:16], in0=ta1, in1=tb1, op=ALU.subtract), reads=["tmpa", "tmpb"], writes=["krot"])
                            S.op("dve", lambda e, pr=pr, cm=cm, ta1=ta1: e.tensor_tensor(out=ta1, in0=pr[:, 1168:1184], in1=cm, op=ALU.mult), reads=[prk, "ropem"], writes=["tmpa"])
                            S.op("dve", lambda e, pr=pr, sm=sm, tb1=tb1: e.tensor_tensor(out=tb1, in0=pr[:, 1152:1168], in1=sm, op=ALU.mult), reads=[prk, "ropem"], writes=["tmpb"])
                            S.op("dve", lambda e, ta1=ta1, tb1=tb1: e.tensor_tensor(out=krot[:, 16:32], in0=ta1, in1=tb1, op=ALU.add), reads=["tmpa", "tmpb"], writes=["krot"])
                            S.op("dve", lambda e: e.tensor_copy(out=km[:, :, 64:96], in_=krot[:, :].unsqueeze(1).to_broadcast([128, 8, 32])), reads=["krot"], writes=["km"])
                            for h in range(8):
                                S.op("pe", lambda e, h=h: e.transpose(out=T0[0:96, h * 128:(h + 1) * 128], in_=qmb_f[:, h * 96:(h + 1) * 96], identity=identb[:]), reads=["qmb", "identb"], writes=["T0"])
                            S.op("dve", lambda e, qmst=qmst, t=t: e.tensor_copy(out=qmst[0:96, :, t * 128:(t + 1) * 128], in_=T0[0:96, :].rearrange("p (k t) -> p k t", k=8)), reads=["T0"], writes=[qmk])
                            for h in range(8):
                                S.op("pe", lambda e, h=h: e.transpose(out=T1[0:96, h * 128:(h + 1) * 128], in_=km_f[:, h * 96:(h + 1) * 96], identity=identb[:]), reads=["km", "identb"], writes=["T1"])
                            S.op("dve", lambda e, kmst=kmst, t=t: e.tensor_copy(out=kmst[0:96, :, t * 128:(t + 1) * 128], in_=T1[0:96, :].rearrange("p (k t) -> p k t", k=8)), reads=["T1"], writes=[kmk])
                        csl = slice(c * 512, (c + 1) * 512)
                        S.dma("sp", lambda e, qkst=qkst, csl=csl: e.dma_start(out=QgT[:, :, csl], in_=qkst[:, 0:4, :]), reads=[qkk], writes=["QgT"])
                        S.dma("sp", lambda e, qkst=qkst, csl=csl: e.dma_start(out=KgT[:, :, csl], in_=qkst[:, 4:6, :]), reads=[qkk], writes=["KgT"])
                        S.dma("sp", lambda e, qmst=qmst, csl=csl: e.dma_start(out=QmT[:, :, csl], in_=qmst[0:96, :, :]), reads=[qmk], writes=["QmT"])
                        S.dma("sp", lambda e, kmst=kmst, csl=csl: e.dma_start(out=KmT[:, :, csl], in_=kmst[0:96, :, :]), reads=[kmk], writes=["KmT"])
                S.barrier()
                if stop_after == "P1":
                    for nm, t_ in (("QgT", QgT), ("KgT", KgT), ("QmT", QmT), ("KmT", KmT)):
                        d_ = dbgt(nm, t_.shape, BF16)
                        S.dma("sp", lambda e, d_=d_, t_=t_: e.dma_start(out=d_, in_=t_), reads=[nm], writes=["dbg"])
                    return False
                with ExitStack() as p2:
                    b = lambda name, shape, dt: sb(name, shape, dt, p2)
                    QTR = Ring(b, "QTb", [128, S_LEN], BF16, 2); KTR = Ring(b, "KTb", [128, S_LEN], BF16, 2)
                    PTR = Ring(b, "PT", [128, 512], BF16, 4)
                    OoR = Ring(b, "Oo", [128, 4, 64], F32, 2); rcR = Ring(b, "rc", [128, 4], F32, 2)
                    Sring = Ring(lambda n, s, d: FB[(0, 1, 2, 5)[int(n[-1])]], "SR", None, None, 4)
                    Sring.items = [(FB[i], FK[i]) for i in (0, 1, 2, 5)]
                    Osets = [[(FB[3], "F3")], [(FB[4], "F4")]]

                    def make_cb(hcol):
                        def cbk(qc, oset, per_bank):
                            ob, okey = oset[0]
                            ov = ob[:, 0:260].rearrange("p (q d) -> p q d", q=4)
                            rc, rck = rcR.next(); Oo, Ook = OoR.next()
                            S.op("dve", lambda e: e.reciprocal(out=rc[:, :], in_=ov[:, :, 64]), reads=[okey], writes=[rck])
                            S.op("dve", lambda e: e.tensor_tensor(out=Oo[:, :, :], in0=ov[:, :, 0:64], in1=rc[:, :].unsqueeze(2).to_broadcast([128, 4, 64]), op=ALU.mult), reads=[okey, rck], writes=[Ook])
                            S.dma("sp", lambda e: e.dma_start(out=Odr[qc * 512:(qc + 1) * 512, hcol:hcol + 64].rearrange("(q p) d -> p q d", p=128), in_=Oo[:, :, :]), reads=[Ook], writes=["Odr"])
                        return cbk

                    heads = []
                    for pr_ in range(4):
                        g_ = pr_ // 2

                        def ld(pr_=pr_, g_=g_):
                            QTb, qk_ = QTR.next(); KTb, kk_ = KTR.next()
                            S.dma("sp", lambda e: e.dma_start(out=QTb[:, :], in_=QgT[:, pr_, :]), reads=["QgT"], writes=[qk_])
                            S.dma("sp", lambda e: e.dma_start(out=KTb[:, :], in_=KgT[:, g_, :]), reads=["KgT"], writes=[kk_])
                            return (QTb, qk_, KTb, kk_)

                        def mk(bufs, pr_=pr_, g_=g_):
                            QTb, qk_, KTb, kk_ = bufs
                            st_ = []
                            for hh in range(2):
                                h = pr_ * 2 + hh
                                r0 = hh * 64
                                st_ += attn_steps(QTb[r0:r0 + 64, :], qk_, KTb[r0:r0 + 64, :], kk_, NT, lambda kt, g_=g_: Vg[:, kt, g_, :], "Vg", 64, 64 ** -0.5, None, make_cb(h * 64), Sring, PTR, Osets)
                            return st_
                        heads.append((ld, mk))
                    for h in range(8):
                        def ld(h=h):
                            QTb, qk_ = QTR.next(); KTb, kk_ = KTR.next()
                            S.dma("sp", lambda e: e.dma_start(out=QTb[0:96, :], in_=QmT[:, h, :]), reads=["QmT"], writes=[qk_])
                            S.dma("sp", lambda e: e.dma_start(out=KTb[0:96, :], in_=KmT[:, h, :]), reads=["KmT"], writes=[kk_])
                            return (QTb, qk_, KTb, kk_)

                        def mk(bufs, h=h):
                            QTb, qk_, KTb, kk_ = bufs
                            return attn_steps(QTb[0:96, :], qk_, KTb[0:96, :], kk_, NT, lambda kt, h=h: Vm[:, kt, h, :], "Vm", 64, 96 ** -0.5, None, make_cb(512 + h * 64), Sring, PTR, Osets)
                        heads.append((ld, mk))
                    allsteps = []
                    bufs = heads[0][0]()
                    for gi, (ld, mk) in enumerate(heads):
                        st_ = mk(bufs)
                        if gi + 1 < len(heads):
                            holder = {}
                            nxt = heads[gi + 1][0]
                            st_[0]["pre"] = (lambda holder=holder, nxt=nxt: holder.__setitem__("b", nxt()))
                            run_steps(st_, ATT_LA)
                            bufs = holder["b"]
                        else:
                            run_steps(st_, ATT_LA)
                S.barrier()
            if stop_after == "P2":
                d_ = dbgt("Odr", [S_LEN, 1024], F32)
                S.dma("sp", lambda e: e.dma_start(out=d_, in_=Odr), reads=["Odr"], writes=["dbg"])
                return False
            with ExitStack() as p3:
                b = lambda name, shape, dt: sb(name, shape, dt, p3)
                Wo = b("Wo", [128, 8, 1024], BF16)
                load_weight(Wo[:, 0:4, :], "Wo", w_o[l, 0:512, :], 4, 1024, gc_og[:, l, :], "gcB")
                load_weight(Wo[:, 4:8, :], "Wo", w_o[l, 512:1024, :], 4, 1024, gc_om[:, l, :], "gcB")
                otR = Ring(b, "ot", [128, D], F32, 2); hnR = Ring(b, "hn3", [128, D], BF16, 2); XTR = Ring(b, "XT3", [128, 8, 128], BF16, 2)
                xtR = Ring(b, "xt3", [128, D], F32, 2); yR = Ring(b, "y3", [128, D], F32, 2)
                scr = b("scr3", [128, D], F32); rsx = Ring(b, "rs3", [128, 2], F32, 2)
                def stageA(j):
                    ot, otk = otR.next(); hn, hk = hnR.next(); XT, XTk = XTR.next(); xt, xk = xtR.next(); rs, rsk = rsx.next()
                    S.dma("sp", lambda e: e.dma_start(out=ot[:], in_=Odr[j * 128:(j + 1) * 128, :]), reads=["Odr"], writes=[otk])
                    S.dma("sp", lambda e: e.dma_start(out=xt[:], in_=x_src[j * 128:(j + 1) * 128, :]), reads=[xs_key], writes=[xk])
                    norm_transpose(ot, otk, hn, hk, XT, XTk, 0, rs, rsk, scr, "scr3", halves=2)
                    return (XT, XTk, xt, xk)

                def stageB(j, XT, XTk, xt, xk):
                    y, yk = yR.next()
                    for nb in range(2):
                        for kc in range(8):
                            S.op("pe", lambda e, nb=nb, kc=kc: e.matmul(FB[nb][:, :], lhsT=XT[:, kc, :], rhs=Wo[:, kc, nb * 512:(nb + 1) * 512], start=(kc == 0), stop=(kc == 7)), reads=[XTk, "Wo"], writes=[FK[nb]])
                        S.op("dve", lambda e, nb=nb: e.tensor_tensor(out=y[:, nb * 512:(nb + 1) * 512], in0=FB[nb][:, :], in1=xt[:, nb * 512:(nb + 1) * 512], op=ALU.add), reads=[FK[nb], xk], writes=[yk])
                    S.dma("sp", lambda e: e.dma_start(out=xres[j * 128:(j + 1) * 128, :], in_=y[:]), reads=[yk], writes=["xres"])

                cur = stageA(0)
                for j in range(NT):
                    nxt = stageA(j + 1) if j + 1 < NT else None
                    stageB(j, *cur)
                    cur = nxt
            S.barrier()
            if stop_after == "P3":
                return False

            with ExitStack() as p4:
                b = lambda name, shape, dt: sb(name, shape, dt, p4)
                Wq = b("Wmq", [128, 8, 512], BF16); Wkv = b("Wmkv", [128, 8, 1024], BF16); Wmo = b("Wmo", [128, 4, 1024], BF16)
                load_weight(Wq, "Wmq", w_mem_q[l], 8, 512, gc_mem[:, l, :], "gcA")
                load_weight(Wkv, "Wmkv", w_mem_kv[l], 8, 1024, gc_memkv[:, l, :], "gcA")
                load_weight(Wmo, "Wmo", w_mem_o[l], 4, 1024)
                xtR = Ring(b, "xt4", [128, D], F32, 8); hnR = Ring(b, "hn4", [128, D], BF16, 2)
                scr = b("scr4", [128, D], F32); rsx = Ring(b, "rs4", [128, 2], F32, 2)
                XTm = b("XTm", [128, 8, 256], BF16)
                KTm = b("KTm", [128, 4, 256], BF16); Vme = b("Vme", [128, 2, 4, 129], BF16)
                S.op("pool", lambda e: e.memset(Vme[:, :, :, 128:129], 1.0), writes=["Vme"])
                for mt in range(2):
                    xt, xk = xtR.next(); hn, hk = hnR.next(); rs, rsk = rsx.next()
                    S.dma("sp", lambda e, xt=xt, mt=mt: e.dma_start(out=xt[:], in_=mem_in[mt * 128:(mt + 1) * 128, :]), writes=[xk])
                    norm_transpose(xt, xk, hn, hk, XTm, "XTm", mt * 128, rs, rsk, scr, "scr4")
                for h in range(4):
                    for kc in range(8):
                        S.op("pe", lambda e, h=h, kc=kc: e.matmul(FB[2][:, 0:256], lhsT=Wkv[:, kc, h * 128:(h + 1) * 128], rhs=XTm[:, kc, :], start=(kc == 0), stop=(kc == 7)), reads=["Wmkv", "XTm"], writes=["F2"])
                    S.op("dve", lambda e, h=h: e.tensor_copy(out=KTm[:, h, :], in_=FB[2][:, 0:256]), reads=["F2"], writes=["KTm"])
                for mt in range(2):
                    for kc in range(8):
                        S.op("pe", lambda e, mt=mt, kc=kc: e.matmul(FB[3][:, :], lhsT=XTm[:, kc, mt * 128:(mt + 1) * 128], rhs=Wkv[:, kc, 512:1024], start=(kc == 0), stop=(kc == 7)), reads=["Wmkv", "XTm"], writes=["F3"])
                    S.op("dve", lambda e, mt=mt: e.tensor_copy(out=Vme[:, mt, :, 0:128], in_=FB[3][:, :].rearrange("p (h d) -> p h d", h=4)), reads=["F3"], writes=["Vme"])
                XTR = Ring(b, "XT4", [128, 8, 512], BF16, 2)
                QTR = Ring(b, "QTm", [128, 512], BF16, 2); PTR = Ring(b, "PT4", [128, 512], BF16, 4)
                ObR = Ring(b, "Obf", [128, 4, 512], BF16, 2); OTR = Ring(b, "OT4", [128, 4, 128], BF16, 2)
                rcR = Ring(b, "rc4", [128, 4], F32, 2); yR = Ring(b, "y4", [128, D], F32, 2)
                Sring = Ring(lambda n, s_, d: FB[int(n[-1])], "F", None, None, 2)
                Osets = [[(FB[2], "F2"), (FB[3], "F3")]]
                for c in range(8):
                    XT, XTk = XTR.next()
                    xts = []
                    for t in range(4):
                        j = c * 4 + t
                        xt, xk = xtR.next(); hn, hk = hnR.next(); rs, rsk = rsx.next()
                        xts.append((xt, xk))
                        S.dma("sp", lambda e, xt=xt, j=j: e.dma_start(out=xt[:], in_=xres[j * 128:(j + 1) * 128, :]), reads=["xres"], writes=[xk])
                        norm_transpose(xt, xk, hn, hk, XT, XTk, t * 128, rs, rsk, scr, "scr4")
                    Obf, Obk = ObR.next()
                    for h in range(4):
                        QTm, QTk = QTR.next()
                        for kc in range(8):
                            S.op("pe", lambda e, h=h, kc=kc, XT=XT: e.matmul(FB[4][:, :], lhsT=Wq[:, kc, h * 128:(h + 1) * 128], rhs=XT[:, kc, :], start=(kc == 0), stop=(kc == 7)), reads=["Wmq", XTk], writes=["F4"])
                        S.op("dve", lambda e, QTm=QTm: e.tensor_copy(out=QTm[:, :], in_=FB[4][:, :]), reads=["F4"], writes=[QTk])

                        def cbk(qc, oset, per_bank, h=h, Obf=Obf, Obk=Obk):
                            rc, rck = rcR.next()
                            for qs in range(4):
                                ob, okey = oset[qs // per_bank]
                                o0 = (qs % per_bank) * 129
                                S.op("dve", lambda e, ob=ob, o0=o0, qs=qs: e.reciprocal(out=rc[:, qs:qs + 1], in_=ob[:, o0 + 128:o0 + 129]), reads=[okey], writes=[rck])
                                S.op("dve", lambda e, ob=ob, o0=o0, qs=qs: e.tensor_scalar(out=Obf[:, qs, h * 128:(h + 1) * 128], in0=ob[:, o0:o0 + 128], scalar1=rc[:, qs:qs + 1], scalar2=None, op0=ALU.mult), reads=[okey, rck], writes=[Obk])
                        attention(QTm[:, :], QTk, KTm[:, h, :], "KTm", 2, lambda kt, h=h: Vme[:, kt, h, :], "Vme", 128, 128 ** -0.5, None, cbk, Sring, PTR, Osets, nqc=1)
                    for qs in range(4):
                        j = c * 4 + qs
                        xt, xk = xts[qs]
                        OT, OTk = OTR.next(); y, yk = yR.next()
                        for h in range(4):
                            S.op("pe", lambda e, h=h, qs=qs, Obf=Obf: e.transpose(out=T1[:, h * 128:(h + 1) * 128], in_=Obf[:, qs, h * 128:(h + 1) * 128], identity=identb[:]), reads=[Obk, "identb"], writes=["T1"])
                        S.op("dve", lambda e, OT=OT: e.tensor_copy(out=OT[:, :, :], in_=T1[:, 0:512].rearrange("p (k t) -> p k t", k=4)), reads=["T1"], writes=[OTk])
                        for nb in range(2):
                            for h in range(4):
                                S.op("pe", lambda e, nb=nb, h=h, OT=OT: e.matmul(FB[4 + nb][:, :], lhsT=OT[:, h, :], rhs=Wmo[:, h, nb * 512:(nb + 1) * 512], start=(h == 0), stop=(h == 3)), reads=[OTk, "Wmo"], writes=[FK[4 + nb]])
                            S.op("dve", lambda e, nb=nb, y=y, xt=xt: e.tensor_tensor(out=y[:, nb * 512:(nb + 1) * 512], in0=FB[4 + nb][:, :], in1=xt[:, nb * 512:(nb + 1) * 512], op=ALU.add), reads=[FK[4 + nb], xk], writes=[yk])
                        S.dma("sp", lambda e, y=y, j=j: e.dma_start(out=xres[j * 128:(j + 1) * 128, :], in_=y[:]), reads=[yk], writes=["xres"])
            S.barrier()
            if stop_after == "P4":
                return False
            with ExitStack() as p5:
                bo = lambda name, shape, dt: sb(name, shape, dt, p5)
                idx_i = bo("idx_i", [128, 64], I32)
                with ExitStack() as p5a:
                    b = lambda name, shape, dt: sb(name, shape, dt, p5a)
                    gfb = b("gfb", [128, D], F32)
                    S.dma("sp", lambda e: e.dma_start(out=gfb[:], in_=ln_ffn[l, :].partition_broadcast(128)), writes=["gfb"])
                    Wr = b("Wr", [128, 8, 16], F32)
                    for kc in range(8):
                        S.dma("sp", lambda e, kc=kc: e.dma_start(out=Wr[:, kc, :], in_=w_router[l, kc * 128:(kc + 1) * 128, :]), writes=["Wr"])
                    aff = b("aff", [128, NT, 16], F32)
                    xtR = Ring(b, "xt5", [128, D], F32, 2); h32R = Ring(b, "h32", [128, D], F32, 2); hbR = Ring(b, "hb5", [128, D], BF16, 2)
                    scr = b("scr5", [128, D], F32); rsx = Ring(b, "rs5", [128, 2], F32, 2)
                    XT32R = Ring(b, "XT32", [128, 8, 128], F32, 2)
                    smR = Ring(b, "sm5", [128, 4], F32, 2); exR = Ring(b, "ex5", [128, 16], F32, 2)
                    for j in range(NT):
                        xt, xk = xtR.next(); h32, h32k = h32R.next(); hb, hbk = hbR.next(); rs, rsk = rsx.next(); XT32, X32k = XT32R.next()
                        sm, smk = smR.next(); ex, exk = exR.next()
                        S.dma("sp", lambda e, xt=xt, j=j: e.dma_start(out=xt[:], in_=xres[j * 128:(j + 1) * 128, :]), reads=["xres"], writes=[xk])
                        norm_transpose(xt, xk, h32, h32k, None, None, 0, rs, rsk, scr, "scr5", gain_b=gfb, gbkey="gfb")
                        S.op("pool", lambda e, hb=hb, h32=h32: e.tensor_copy(out=hb[:, :], in_=h32[:, :]), reads=[h32k], writes=[hbk])
                        S.dma("sp", lambda e, hb=hb, j=j: e.dma_start(out=hdr[j * 128:(j + 1) * 128, :], in_=hb[:]), reads=[hbk], writes=["hdr"])
                        for kc in range(8):
                            S.op("pe", lambda e, kc=kc, h32=h32: e.transpose(out=FB[kc // 4][:, (kc % 4) * 128:(kc % 4 + 1) * 128], in_=h32[:, kc * 128:(kc + 1) * 128], identity=identf[:]), reads=[h32k, "identf"], writes=[FK[kc // 4]])
                        for hf in range(2):
                            S.op("dve", lambda e, hf=hf, XT32=XT32: e.tensor_copy(out=XT32[:, hf * 4:(hf + 1) * 4, :], in_=FB[hf][:, :].rearrange("p (k t) -> p k t", k=4)), reads=[FK[hf]], writes=[X32k])
                        for kc in range(8):
                            S.op("pe", lambda e, kc=kc, XT32=XT32: e.matmul(FB[2][:, 0:16], lhsT=XT32[:, kc, :], rhs=Wr[:, kc, :], start=(kc == 0), stop=(kc == 7)), reads=[X32k, "Wr"], writes=["F2"])
                        S.op("dve", lambda e, sm=sm: e.tensor_reduce(out=sm[:, 0:1], in_=FB[2][:, 0:16], axis=AX.X, op=ALU.max, negate=True), reads=["F2"], writes=[smk])
                        S.op("act", lambda e, sm=sm, ex=ex: e.activation(out=ex[:, :], in_=FB[2][:, 0:16], func=AF.Exp, bias=sm[:, 0:1], scale=1.0, accum_out=sm[:, 1:2]), reads=["F2", smk], writes=[exk, smk])
                        S.op("dve", lambda e, sm=sm: e.reciprocal(out=sm[:, 2:3], in_=sm[:, 1:2]), reads=[smk], writes=[smk])
                        S.op("dve", lambda e, sm=sm, ex=ex, j=j: e.tensor_scalar(out=aff[:, j, :], in0=ex[:, :], scalar1=sm[:, 2:3], scalar2=None, op0=ALU.mult), reads=[exk, smk], writes=["aff"])
                        S.dma("sp", lambda e, j=j: e.dma_start(out=affdr[j * 128:(j + 1) * 128, :], in_=aff[:, j, :]), reads=["aff"], writes=["affdr"])
                    affT = b("affT", [16, S_LEN], F32); junk = b("junk", [16, S_LEN], BF16); maskT = b("maskT", [16, S_LEN], BF16)
                    for rnd in range(2):
                        for jj in range(16):
                            j = rnd * 16 + jj
                            S.op("pe", lambda e, j=j, jj=jj: e.transpose(out=FB[jj // 4][0:16, (jj % 4) * 128:(jj % 4 + 1) * 128], in_=aff[:, j, :], identity=identf[:]), reads=["aff", "identf"], writes=[FK[jj // 4]])
                        for q4 in range(4):
                            S.op("dve", lambda e, rnd=rnd, q4=q4: e.tensor_copy(out=affT[:, (rnd * 4 + q4) * 512:(rnd * 4 + q4 + 1) * 512], in_=FB[q4][0:16, :]), reads=[FK[q4]], writes=["affT"])
                    bs = b("bs", [16, 8], F32)
                    S.op("dve", lambda e: e.memset(bs[:, 0:1], 0.0), writes=["bs"])
                    S.op("dve", lambda e: e.memset(bs[:, 1:2], 2.0), writes=["bs"])
                    for it in range(33):
                        S.op("dve", lambda e: e.tensor_tensor(out=bs[:, 2:3], in0=bs[:, 0:1], in1=bs[:, 1:2], op=ALU.add), reads=["bs"], writes=["bs"])
                        S.op("dve", lambda e: e.tensor_scalar(out=bs[:, 2:3], in0=bs[:, 2:3], scalar1=0.5, scalar2=None, op0=ALU.mult), reads=["bs"], writes=["bs"])
                        S.op("dve", lambda e: e.tensor_scalar(out=junk[:, :], in0=affT[:, :], scalar1=bs[:, 2:3], scalar2=0.0, op0=ALU.is_ge, op1=ALU.add, accum_out=bs[:, 3:4]), reads=["affT", "bs"], writes=["junk", "bs"])
                        S.op("dve", lambda e: e.tensor_scalar(out=bs[:, 4:5], in0=bs[:, 3:4], scalar1=511.5, scalar2=None, op0=ALU.is_ge), reads=["bs"], writes=["bs"])
                        S.op("dve", lambda e: e.tensor_tensor(out=bs[:, 5:6], in0=bs[:, 2:3], in1=bs[:, 0:1], op=ALU.subtract), reads=["bs"], writes=["bs"])
                        S.op("dve", lambda e: e.scalar_tensor_tensor(out=bs[:, 0:1], in0=bs[:, 5:6], scalar=bs[:, 4:5], in1=bs[:, 0:1], op0=ALU.mult, op1=ALU.add), reads=["bs"], writes=["bs"])
                        S.op("dve", lambda e: e.tensor_tensor(out=bs[:, 5:6], in0=bs[:, 1:2], in1=bs[:, 2:3], op=ALU.subtract), reads=["bs"], writes=["bs"])
                        S.op("dve", lambda e: e.scalar_tensor_tensor(out=bs[:, 1:2], in0=bs[:, 5:6], scalar=bs[:, 4:5], in1=bs[:, 2:3], op0=ALU.mult, op1=ALU.add), reads=["bs"], writes=["bs"])
                    S.op("dve", lambda e: e.tensor_scalar(out=maskT[:, :], in0=affT[:, :], scalar1=bs[:, 0:1], scalar2=None, op0=ALU.is_ge), reads=["affT", "bs"], writes=["maskT"])
                    mask_tok = b("mask_tok", [128, NT, 16], BF16)
                    for j in range(NT):
                        S.op("pe", lambda e, j=j: e.transpose(out=T0[:, j * 16:(j + 1) * 16], in_=maskT[:, j * 128:(j + 1) * 128], identity=identb[0:16, 0:16]), reads=["maskT", "identb"], writes=["T0"])
                    S.op("dve", lambda e: e.tensor_copy(out=mask_tok[:, :, :], in_=T0[:, 0:512].rearrange("p (j e) -> p j e", j=NT)), reads=["T0"], writes=["mask_tok"])
                    onesb = b("onesb", [128, 128], BF16); ustr = b("ustr", [128, 128], BF16)
                    S.op("pool", lambda e: e.memset(onesb[:], 1.0), writes=["onesb"])
                    S.op("pool", lambda e: e.memset(ustr[:], 1.0), writes=["ustr"])
                    S.op("pool", lambda e: e.affine_select(out=ustr[:], in_=ustr[:], pattern=[[1, 128]], compare_op=ALU.is_gt, fill=0.0, base=0, channel_multiplier=-1), reads=["ustr"], writes=["ustr"])
                    firstmm = True
                    for j in range(NT):
                        for i in range(j + 1):
                            lhs = ustr if i == j else onesb
                            S.op("pe", lambda e, j=j, i=i, lhs=lhs, firstmm=firstmm: e.matmul(FB[0][:, j * 16:(j + 1) * 16], lhsT=lhs[:, :], rhs=mask_tok[:, i, :], start=firstmm, stop=(i == j), skip_group_check=True), reads=["mask_tok", "onesb", "ustr"], writes=["F0"])
                            firstmm = False
                    posm = b("posm", [128, NT, 16], F32)
                    S.op("dve", lambda e: e.scalar_tensor_tensor(out=posm[:, :, :], in0=FB[0][:, :].rearrange("p (j e) -> p j e", j=NT), scalar=1.0, in1=mask_tok[:, :, :], op0=ALU.add, op1=ALU.mult), reads=["F0", "mask_tok"], writes=["posm"])
                    S.op("dve", lambda e: e.tensor_scalar(out=posm[:, :, :], in0=posm[:, :, :], scalar1=-1.0, scalar2=None, op0=ALU.add), reads=["posm"], writes=["posm"])
                    iota_s = b("iota_s", [128, 512], F32)
                    S.op("pool", lambda e: e.iota(iota_s[:, :], pattern=[[1, 512]], base=0, channel_multiplier=0, allow_small_or_imprecise_dtypes=True), writes=["iota_s"])
                    tv32 = b("tv32", [128, NT, 2], F32); tokval = b("tokval", [128, NT, 2], BF16)
                    S.op("pool", lambda e: e.iota(tv32[:, :, :], pattern=[[0, NT], [0, 2]], base=0, channel_multiplier=1, allow_small_or_imprecise_dtypes=True), writes=["tv32"])
                    S.op("pool", lambda e: e.iota(tv32[:, :, 1], pattern=[[1, NT]], base=0, channel_multiplier=0, allow_small_or_imprecise_dtypes=True), reads=["tv32"], writes=["tv32"])
                    S.op("pool", lambda e: e.tensor_copy(out=tokval[:, :, :], in_=tv32[:, :, :]), reads=["tv32"], writes=["tokval"])
                    OhR = Ring(b, "Oh", [128, 16, 512], BF16, 2)
                    firstmm = True
                    for j in range(NT):
                        Oh, Ohk = OhR.next()
                        eng = "dve"
                        S.op(eng, lambda e, Oh=Oh, j=j: e.tensor_tensor(out=Oh[:, :, :], in0=iota_s[:, :].unsqueeze(1).to_broadcast([128, 16, 512]), in1=posm[:, j, :].unsqueeze(2).to_broadcast([128, 16, 512]), op=ALU.is_equal), reads=["iota_s", "posm"], writes=[Ohk])
                        for ee in range(16):
                            for c in range(4):
                                col = (ee * 4 + c) * 2
                                S.op("pe", lambda e, Oh=Oh, ee=ee, c=c, col=col, j=j, firstmm=firstmm: e.matmul(FB[5][:, col:col + 2], lhsT=Oh[:, ee, c * 128:(c + 1) * 128], rhs=tokval[:, j, :], start=firstmm, stop=(j == NT - 1), skip_group_check=True), reads=[Ohk, "tokval"], writes=["F5"])
                                firstmm = False
                    idxf = b("idxf", [128, 64], F32)
                    f5s = b("f5s", [128, 128], F32)
                    S.op("dve", lambda e: e.tensor_copy(out=f5s[:, :], in_=FB[5][:, 0:128]), reads=["F5"], writes=["f5s"])
                    f5v = f5s[:, :].rearrange("p (s two) -> p s two", two=2)
                    S.op("dve", lambda e: e.scalar_tensor_tensor(out=idxf[:, :], in0=f5v[:, :, 1], scalar=128.0, in1=f5v[:, :, 0], op0=ALU.mult, op1=ALU.add), reads=["f5s"], writes=["idxf"])
                    S.op("dve", lambda e: e.tensor_copy(out=idx_i[:, :], in_=idxf[:, :]), reads=["idxf"], writes=["idx_i"])
                    if stop_after == "P5a":
                        d1 = dbgt("idx", [128, 64], I32); d2 = dbgt("aff", [S_LEN, 16], F32); d3 = dbgt("bs", [16, 8], F32)
                        S.dma("sp", lambda e: e.dma_start(out=d1, in_=idx_i[:, :]), reads=["idx_i"], writes=["dbg"])
                        S.dma("sp", lambda e: e.dma_start(out=d2, in_=affdr), reads=["affdr"], writes=["dbg"])
                        S.dma("sp", lambda e: e.dma_start(out=d3, in_=bs[:, :]), reads=["bs"], writes=["dbg"])
                S.barrier()
                if stop_after == "P5a":
                    return False
                with ExitStack() as p5b:
                    b = lambda name, shape, dt: sb(name, shape, dt, p5b)
                    WgR = Ring(b, "Wg", [128, 8, 512], BF16, 2); WuR = Ring(b, "Wu", [128, 8, 512], BF16, 2); WdR = Ring(b, "Wd", [128, 4, 1024], BF16, 2)
                    XgTR = Ring(b, "XgT", [128, 8, 512], BF16, 2); xgR = Ring(b, "xg", [128, D], BF16, 8); gaR = Ring(b, "ga", [128, 16], F32, 8)
                    GTR = Ring(b, "GT", [128, 4, 512], BF16, 2); sgR = Ring(b, "sg", [128, 512], F32, 2); yR = Ring(b, "y5", [128, D], F32, 2)
                    def prefetch(ee):
                        Wg, Wgk = WgR.next(); Wu, Wuk = WuR.next(); Wd, Wdk = WdR.next()
                        load_weight(Wg, Wgk, w_gate[l, ee], 8, 512)
                        load_weight(Wu, Wuk, w_up[l, ee], 8, 512)
                        load_weight(Wd, Wdk, w_down[l, ee], 4, 1024)
                        xgs = []; gas = []
                        for c in range(4):
                            xg, xgk = xgR.next(); ga, gak = gaR.next()
                            xgs.append((xg, xgk)); gas.append((ga, gak))
                            col = ee * 4 + c
                            S.dma("pool", lambda e, xg=xg, col=col: e.indirect_dma_start(out=xg[:, :], out_offset=None, in_=hdr[:, :], in_offset=bass.IndirectOffsetOnAxis(ap=idx_i[:, col:col + 1], axis=0)), reads=["idx_i", "hdr"], writes=[xgk])
                            S.dma("pool", lambda e, ga=ga, col=col: e.indirect_dma_start(out=ga[:, :], out_offset=None, in_=affdr[:, :], in_offset=bass.IndirectOffsetOnAxis(ap=idx_i[:, col:col + 1], axis=0)), reads=["idx_i", "affdr"], writes=[gak])
                        return (Wg, Wgk, Wu, Wuk, Wd, Wdk, xgs, gas)

                    nxt = prefetch(0)
                    for ee in range(16):
                        Wg, Wgk, Wu, Wuk, Wd, Wdk, xgs, gas = nxt
                        if ee + 1 < 16:
                            nxt = prefetch(ee + 1)
                        XgT, XgTk = XgTR.next(); GT, GTk = GTR.next()
                        for c in range(4):
                            xg, xgk = xgs[c]
                            for kc in range(8):
                                S.op("pe", lambda e, kc=kc, xg=xg: e.transpose(out=T0[:, kc * 128:(kc + 1) * 128], in_=xg[:, kc * 128:(kc + 1) * 128], identity=identb[:]), reads=[xgk, "identb"], writes=["T0"])
                            S.op("dve", lambda e, XgT=XgT, c=c: e.tensor_copy(out=XgT[:, :, c * 128:(c + 1) * 128], in_=T0[:, :].rearrange("p (k t) -> p k t", k=8)), reads=["T0"], writes=[XgTk])
                        for fc in range(4):
                            pa = 2 * (fc % 2); pu = pa + 1
                            for kc in range(8):
                                S.op("pe", lambda e, fc=fc, kc=kc, pa=pa, Wg=Wg, XgT=XgT: e.matmul(FB[pa][:, :], lhsT=Wg[:, kc, fc * 128:(fc + 1) * 128], rhs=XgT[:, kc, :], start=(kc == 0), stop=(kc == 7)), reads=[Wgk, XgTk], writes=[FK[pa]])
                            for kc in range(8):
                                S.op("pe", lambda e, fc=fc, kc=kc, pu=pu, Wu=Wu, XgT=XgT: e.matmul(FB[pu][:, :], lhsT=Wu[:, kc, fc * 128:(fc + 1) * 128], rhs=XgT[:, kc, :], start=(kc == 0), stop=(kc == 7)), reads=[Wuk, XgTk], writes=[FK[pu]])
                            sg, sgk = sgR.next()
                            S.op("act", lambda e, sg=sg, pa=pa: e.activation(out=sg[:, :], in_=FB[pa][:, :], func=AF.Silu), reads=[FK[pa]], writes=[sgk])
                            S.op("dve", lambda e, sg=sg, pu=pu, GT=GT, fc=fc: e.tensor_tensor(out=GT[:, fc, :], in0=sg[:, :], in1=FB[pu][:, :], op=ALU.mult), reads=[sgk, FK[pu]], writes=[GTk])
                        for st_ in range(4):
                            y, yk = yR.next(); ga, gak = gas[st_]
                            col = ee * 4 + st_
                            for half in range(2):
                                for fc in range(4):
                                    S.op("pe", lambda e, half=half, fc=fc, GT=GT, Wd=Wd, st_=st_: e.matmul(FB[4 + half][:, :], lhsT=GT[:, fc, st_ * 128:(st_ + 1) * 128], rhs=Wd[:, fc, half * 512:(half + 1) * 512], start=(fc == 0), stop=(fc == 3)), reads=[GTk, Wdk], writes=[FK[4 + half]])
                                S.op("act", lambda e, half=half, y=y, ga=ga, ee=ee: e.activation(out=y[:, half * 512:(half + 1) * 512], in_=FB[4 + half][:, :], func=AF.Copy, scale=ga[:, ee:ee + 1]), reads=[FK[4 + half], gak], writes=[yk])
                            S.dma("pool", lambda e, y=y, col=col: e.indirect_dma_start(out=xres[:, :], out_offset=bass.IndirectOffsetOnAxis(ap=idx_i[:, col:col + 1], axis=0), in_=y[:, :], in_offset=None, compute_op=ALU.add), reads=[yk, "idx_i"], writes=["xres"])
            S.barrier()
            return True

        x_src, xs_key = x_in, "x_in"
        final_norm = (stop_after is None)
        for l in range(nlayers):
            cont = layer(l, x_src, xs_key)
            x_src, xs_key = xres, "xres"
            if not cont:
                final_norm = False
                break
        with ExitStack() as pf:
            b = lambda name, shape, dt: sb(name, shape, dt, pf)
            xtR = Ring(b, "xtf", [128, D], F32, 2); yR = Ring(b, "yf", [128, D], F32, 2)
            scr = b("scrf", [128, D], F32); rsx = Ring(b, "rsf", [128, 2], F32, 2)
            gfin = b("gfin", [128, D], F32)
            S.dma("sp", lambda e: e.dma_start(out=gfin[:], in_=ln_final.partition_broadcast(128)), writes=["gfin"])
            for j in range(NT):
                xt, xk = xtR.next(); y, yk = yR.next(); rs, rsk = rsx.next()
                S.dma("sp", lambda e, xt=xt, j=j: e.dma_start(out=xt[:], in_=xres[j * 128:(j + 1) * 128, :]), reads=["xres"], writes=[xk])
                if final_norm:
                    norm_transpose(xt, xk, y, yk, None, None, 0, rs, rsk, scr, "scrf", gain_b=gfin, gbkey="gfin")
                    S.dma("sp", lambda e, y=y, j=j: e.dma_start(out=out[j * 128:(j + 1) * 128, :], in_=y[:]), reads=[yk], writes=["out"])
                else:
                    S.dma("sp", lambda e, xt=xt, j=j: e.dma_start(out=out[j * 128:(j + 1) * 128, :], in_=xt[:]), reads=[xk], writes=["out"])
        S.barrier(engines=["sp"])
        print("ninstr", S.ninstr, "nsem", S.nsem, {k: v for k, v in S.cnt.items()})
    return nc, dbg_out


def rope_tables():
    def ang(rot_dim):
        rows = S_LEN // 64
        row = np.repeat(np.arange(rows, dtype=np.float32), 64)
        col = np.tile(np.arange(64, dtype=np.float32), rows)
        axis_dim = rot_dim // 2
        inv = (10000.0 ** (-np.arange(0, axis_dim, 2, dtype=np.float32) / axis_dim)).astype(np.float32)
        a = np.concatenate([row[:, None] * inv[None, :], col[:, None] * inv[None, :]], axis=-1).astype(np.float32)
        return np.concatenate([np.cos(a), np.sin(a)], axis=-1).astype(np.float32)
    return ang(64), ang(32)


def make_in_maps(inputs, ncores=8):
    rg, rm = rope_tables()
    maps = []
    for c in range(ncores):
        m = {k: np.ascontiguousarray(np.asarray(v, dtype=np.float32)) for k, v in inputs.items() if k not in ("x", "mem")}
        m["x"] = np.ascontiguousarray(np.asarray(inputs["x"][c], dtype=np.float32))
        m["mem"] = np.ascontiguousarray(np.asarray(inputs["mem"][c], dtype=np.float32))
        m["ropeg"] = rg
        m["ropem"] = rm
        maps.append(m)
    return maps


def kernel(**inputs):
    nc, _ = build_nc()
    maps = make_in_maps(inputs, 8)
    res = run_bass_kernel_spmd(nc, maps, core_ids=list(range(8)))
    return np.stack([np.asarray(r["out"], dtype=np.float32) for r in res.results], axis=0)
```
